# Optimizing a Trainium2 kernel written in Bass

```python
import math
import jax, jax.numpy as jnp
from jax import lax
import numpy as np

D_MODEL = 1024
BATCH = 16
SEQ = 4096
DEPTH = 1

CHUNK = 64
D_MIX = D_MODEL
CONV_CH = D_MIX // 2
CONV_WIDTH = 31
ATT_HEADS = 8
HEAD_DIM = (D_MIX - CONV_CH) // ATT_HEADS
ATT_W = ATT_HEADS * HEAD_DIM
IDX_HEADS = 4
IDX_DIM = 64
IDX_SCALE = (IDX_HEADS * IDX_DIM) ** -0.5
TOPK_MAX = 256
QBLOCK = 32
REL_BUCKETS = 32
REL_MAX_DIST = 128
PEER_HEADS = 8
N_KEYS = 128
N_EXPERTS = N_KEYS * N_KEYS
PEER_TOPK = 16
PEER_DKEY = 256
PEER_TOKBLOCK = 128
EPS = 1e-6
IN_WIDTHS = (CONV_CH, CONV_CH, ATT_W, ATT_W, ATT_W, IDX_HEADS * IDX_DIM, IDX_DIM, IDX_HEADS)
IN_COLS = 2 * CONV_CH + 3 * ATT_W + IDX_HEADS * IDX_DIM + IDX_DIM + IDX_HEADS

kernel_name = "hybrid_conv_dsa_peer_block"


def rmsnorm(x, g):
    xf = x.astype(jnp.float32)
    y = xf * lax.rsqrt(jnp.mean(xf * xf, axis=-1, keepdims=True) + EPS)
    return (y * g.astype(jnp.float32)).astype(x.dtype)


def layernorm(x, g, b):
    xf = x.astype(jnp.float32)
    mu = jnp.mean(xf, axis=-1, keepdims=True)
    xc = xf - mu
    y = xc * lax.rsqrt(jnp.mean(xc * xc, axis=-1, keepdims=True) + EPS)
    return (y * g.astype(jnp.float32) + b.astype(jnp.float32)).astype(x.dtype)


def split_cols(a):
    outs = []
    off = 0
    for w in IN_WIDTHS:
        outs.append(a[..., off:off + w])
        off += w
    return outs


def t5_bucket(rel):
    half = REL_BUCKETS // 2
    max_exact = half // 2
    ret = jnp.where(rel > 0, half, 0)
    n = jnp.abs(rel)
    nf = jnp.maximum(n, 1).astype(jnp.float32)
    large = max_exact + (jnp.log(nf / max_exact) / math.log(REL_MAX_DIST / max_exact)
                         * (half - max_exact)).astype(jnp.int32)
    large = jnp.minimum(large, half - 1)
    return ret + jnp.where(n < max_exact, n, large)


def conformer_conv(val, gate, w_dw, b_dw, ln_g, ln_b):
    u = val * jax.nn.sigmoid(gate)
    y = lax.conv_general_dilated(u, w_dw, window_strides=(1,),
                                 padding=[(CONV_WIDTH - 1, 0)],
                                 dimension_numbers=('NWC', 'WIO', 'NWC'),
                                 feature_group_count=CONV_CH) + b_dw
    return jax.nn.silu(layernorm(y, ln_g, ln_b))


def dsa_attention(q, k, v, qi, ki, wi, rel_bias):
    B_, S_ = q.shape[0], q.shape[1]
    n_sel = min(TOPK_MAX, S_ // 4)
    nblk = S_ // QBLOCK
    key_pos = jnp.arange(S_, dtype=jnp.int32)
    t_blocks = key_pos.reshape(nblk, QBLOCK)
    scale = HEAD_DIM ** -0.5

    def to_blocks(a):
        return a.reshape((B_, nblk, QBLOCK) + a.shape[2:]).swapaxes(0, 1)

    def one_block(args):
        qb, qib, wib, tb = args
        idx_logits = jnp.einsum('bqhd,bsd->bqhs', qib, ki)
        index = jnp.einsum('bqh,bqhs->bqs', wib, jax.nn.relu(idx_logits)) * IDX_SCALE
        limit = (tb // CHUNK + 1) * CHUNK
        admissible = key_pos[None, :] < limit[:, None]
        index = jnp.where(admissible[None], index.astype(jnp.float32), -jnp.inf)
        _, sel = lax.top_k(index, n_sel)
        valid = sel < limit[None, :, None]
        k_sel = jax.vmap(lambda kb, ib: kb[ib])(k, sel)
        v_sel = jax.vmap(lambda vb, ib: vb[ib])(v, sel)
        bias = rel_bias[t5_bucket(sel - tb[None, :, None])]
        logits = (jnp.einsum('bqhd,bqkhd->bqhk', qb, k_sel).astype(jnp.float32) * scale
                  + jnp.swapaxes(bias, -1, -2).astype(jnp.float32))
        logits = jnp.where(valid[:, :, None, :], logits, -jnp.inf)
        p = jax.nn.softmax(logits, axis=-1).astype(v.dtype)
        return jnp.einsum('bqhk,bqkhd->bqhd', p, v_sel)

    out = lax.map(one_block, (to_blocks(q), to_blocks(qi), to_blocks(wi), t_blocks))
    return out.swapaxes(0, 1).reshape(B_, S_, ATT_W)


def peer(h, w_pq, sub_k1, sub_k2, u_tab, v_tab):
    B_, S_, D_ = h.shape
    hb = h.reshape(-1, PEER_TOKBLOCK, D_)

    def one(xb):
        qq = (xb @ w_pq).reshape(PEER_TOKBLOCK, PEER_HEADS, 2, PEER_DKEY // 2)
        s1 = jnp.einsum('thd,hnd->thn', qq[:, :, 0], sub_k1)
        s2 = jnp.einsum('thd,hnd->thn', qq[:, :, 1], sub_k2)
        v1, i1 = lax.top_k(s1, PEER_TOPK)
        v2, i2 = lax.top_k(s2, PEER_TOPK)
        cand = (v1[..., :, None] + v2[..., None, :]).reshape(PEER_TOKBLOCK, PEER_HEADS, -1)
        cand_idx = (i1[..., :, None] * N_KEYS + i2[..., None, :]).reshape(PEER_TOKBLOCK, PEER_HEADS, -1)
        best, pos = lax.top_k(cand, PEER_TOPK)
        experts = jnp.take_along_axis(cand_idx, pos, axis=-1)
        g = jax.nn.softmax(best.astype(jnp.float32), axis=-1).astype(xb.dtype)
        act = jax.nn.gelu(jnp.einsum('td,thkd->thk', xb, u_tab[experts]))
        return jnp.einsum('thk,thkd->td', g * act, v_tab[experts])

    return lax.map(one, hb).reshape(B_, S_, D_)


def setup_inputs(seed: int = 0) -> dict:
    key = jax.random.key(seed)
    ks = jax.random.split(key, 24)

    def nrm(k, shape, scale):
        return jax.random.normal(k, shape, jnp.float32) * scale

    D = D_MODEL
    return {
        "x": nrm(ks[0], (BATCH, SEQ, D), 1.0),
        "c": nrm(ks[1], (BATCH, D), 1.0),
        "w_ada": nrm(ks[2], (DEPTH, D, 6 * D), 0.5 * D ** -0.5),
        "b_ada": nrm(ks[3], (DEPTH, 6 * D), 0.02),
        "g_norm1": 1.0 + nrm(ks[4], (DEPTH, D), 0.02),
        "g_norm2": 1.0 + nrm(ks[5], (DEPTH, D), 0.02),
        "w_in": nrm(ks[6], (DEPTH, D, IN_COLS), D ** -0.5),
        "q_norm_g": 1.0 + nrm(ks[7], (DEPTH, HEAD_DIM), 0.02),
        "k_norm_g": 1.0 + nrm(ks[8], (DEPTH, HEAD_DIM), 0.02),
        "conv_w": nrm(ks[9], (DEPTH, CONV_WIDTH, 1, CONV_CH), CONV_WIDTH ** -0.5),
        "conv_b": nrm(ks[10], (DEPTH, CONV_CH), 0.02),
        "conv_ln_g": 1.0 + nrm(ks[11], (DEPTH, CONV_CH), 0.02),
        "conv_ln_b": nrm(ks[12], (DEPTH, CONV_CH), 0.02),
        "rel_bias": nrm(ks[13], (REL_BUCKETS, ATT_HEADS), 0.1),
        "g_out_conv": 1.0 + nrm(ks[14], (DEPTH, CONV_CH), 0.02),
        "g_out_attn": 1.0 + nrm(ks[15], (DEPTH, ATT_W), 0.02),
        "w_out": nrm(ks[16], (DEPTH, D_MIX, D), D_MIX ** -0.5),
        "w_peer_q": nrm(ks[17], (DEPTH, D, PEER_HEADS * PEER_DKEY), D ** -0.5),
        "peer_k1": nrm(ks[18], (DEPTH, PEER_HEADS, N_KEYS, PEER_DKEY // 2), (PEER_DKEY // 2) ** -0.5),
        "peer_k2": nrm(ks[19], (DEPTH, PEER_HEADS, N_KEYS, PEER_DKEY // 2), (PEER_DKEY // 2) ** -0.5),
        "peer_u": nrm(ks[20], (DEPTH, N_EXPERTS, D), D ** -0.5),
        "peer_v": nrm(ks[21], (DEPTH, N_EXPERTS, D), 0.5),
    }


def reference(x, c, w_ada, b_ada, g_norm1, g_norm2, w_in, q_norm_g, k_norm_g, conv_w, conv_b,
              conv_ln_g, conv_ln_b, rel_bias, g_out_conv, g_out_attn, w_out, w_peer_q,
              peer_k1, peer_k2, peer_u, peer_v):
    B_, S_, D_ = x.shape
    for l in range(DEPTH):
        mod = jax.nn.silu(c) @ w_ada[l] + b_ada[l]
        sh1, sc1, gt1, sh2, sc2, gt2 = jnp.split(mod[:, None, :], 6, axis=-1)

        h = rmsnorm(x, g_norm1[l]) * (1.0 + sc1) + sh1
        cv, cg, q, k, v, qi, ki, wi = split_cols(h @ w_in[l])
        q = rmsnorm(q.reshape(B_, S_, ATT_HEADS, HEAD_DIM), q_norm_g[l])
        k = rmsnorm(k.reshape(B_, S_, ATT_HEADS, HEAD_DIM), k_norm_g[l])
        v = v.reshape(B_, S_, ATT_HEADS, HEAD_DIM)
        qi = qi.reshape(B_, S_, IDX_HEADS, IDX_DIM)
        conv_out = conformer_conv(cv, cg, conv_w[l], conv_b[l], conv_ln_g[l], conv_ln_b[l])
        attn_out = dsa_attention(q, k, v, qi, ki, wi, rel_bias)
        mixed = jnp.concatenate([rmsnorm(conv_out, g_out_conv[l]),
                                 rmsnorm(attn_out, g_out_attn[l])], axis=-1)
        x = x + gt1 * (mixed @ w_out[l])

        h2 = rmsnorm(x, g_norm2[l]) * (1.0 + sc2) + sh2
        x = x + gt2 * peer(h2, w_peer_q[l], peer_k1[l], peer_k2[l], peer_u[l], peer_v[l])
    return x
```

```python
import math
from contextlib import ExitStack

import numpy as np
import concourse.bass as bass
import concourse.mybir as mybir
from concourse.bass_utils import run_bass_kernel_spmd

F32 = mybir.dt.float32
BF16 = mybir.dt.bfloat16
U32 = mybir.dt.uint32
ALU = mybir.AluOpType
AF = mybir.ActivationFunctionType
AX = mybir.AxisListType

D = 1024
NCOL = 2884
EPS = 1e-6
NEXP_CH = 128
NEG = -30000.0
ACT_EVAC = False
ENGS = ("pe", "act", "dve", "pool", "sp")


class Prog:
    def __init__(self, nc, stack):
        self.nc = nc
        self.stack = stack
        self.ops = {e: [] for e in ENGS}
        self.cnt = {e: 0 for e in ENGS}
        self.sem = {e: stack.enter_context(nc.semaphore("s_" + e)) for e in ENGS}
        self.seen = {e: {} for e in ENGS}
        self.dsem = {}
        self.lastw = {}
        self.readers = {}
        self.n_instr = 0
        self.engs = {"pe": nc.tensor, "act": nc.scalar, "dve": nc.vector,
                     "pool": nc.gpsimd, "sp": nc.sync}

    def _need(self, e, tok, waits):
        if tok is None:
            return
        sem, val, name = tok
        if e == "pe" and name == "s_pe":
            return
        if self.seen[e].get(name, 0) >= val:
            return
        prev = waits.get(name)
        if prev is None or prev[1] < val:
            waits[name] = (sem, val)

    def _deps(self, e, reads, writes):
        waits = {}
        for k in reads:
            self._need(e, self.lastw.get(k), waits)
        for k in writes:
            self._need(e, self.lastw.get(k), waits)
            for t in self.readers.get(k, {}).values():
                self._need(e, t, waits)
        for name, (sem, val) in waits.items():
            self.seen[e][name] = val
        return list(waits.values())

    def _commit(self, tok, reads, writes):
        for k in writes:
            self.lastw[k] = tok
            self.readers[k] = {}
        for k in reads:
            if k in writes:
                continue
            d = self.readers.setdefault(k, {})
            old = d.get(tok[2])
            if old is None or old[1] < tok[1]:
                d[tok[2]] = tok

    def op(self, e, fn, reads=(), writes=()):
        waits = self._deps(e, reads, writes)
        self.cnt[e] += 1
        val = self.cnt[e]
        sem = self.sem[e]

        def thunk(eng, fn=fn, waits=waits, sem=sem):
            for (s, v) in waits:
                eng.wait_ge(s, v)
            fn(eng).then_inc(sem, 1)

        thunk(self.engs[e])
        self.ops[e].append(1)
        self._commit((sem, val, "s_" + e), reads, writes)
        self.n_instr += 1

    def dma(self, q, out, in_, reads=(), writes=(), semkey=None):
        if semkey is None:
            semkey = writes[0]
        if semkey not in self.dsem:
            self.dsem[semkey] = [self.stack.enter_context(
                self.nc.semaphore("d_%d" % len(self.dsem))), 0]
        waits = self._deps(q, reads, writes)
        ent = self.dsem[semkey]
        ent[1] += 16
        sem, val = ent[0], ent[1]

        def thunk(eng, waits=waits, sem=sem, out=out, in_=in_):
            for (s, v) in waits:
                eng.wait_ge(s, v)
            eng.dma_start(out=out, in_=in_).then_inc(sem, 16)

        thunk(self.engs[q])
        self.ops[q].append(1)
        self._commit((sem, val, "d_" + str(semkey)), reads, writes)
        self.n_instr += 1

    def barrier(self):
        toks = [(self.sem[e], self.cnt[e], "s_" + e) for e in ENGS if self.cnt[e] > 0]
        toks += [(ent[0], ent[1], "d_" + str(k)) for k, ent in self.dsem.items()]
        for e in ENGS:
            waits = {}
            for t in toks:
                self._need(e, t, waits)
            for name, (sem, val) in waits.items():
                self.seen[e][name] = val
                self.engs[e].wait_ge(sem, val)

    def final_wait(self, e, keys):
        waits = self._deps(e, keys, ())

        def thunk(eng, waits=waits):
            for (s, v) in waits:
                eng.wait_ge(s, v)

        thunk(self.engs[e])

    def emit(self):
        return
        nc = self.nc
        with nc.Block() as block:
            @block.tensor
            def _(eng):
                for t in self.ops["pe"]:
                    t(eng)

            @block.scalar
            def _(eng):
                for t in self.ops["act"]:
                    t(eng)

            @block.vector
            def _(eng):
                for t in self.ops["dve"]:
                    t(eng)

            @block.gpsimd
            def _(eng):
                for t in self.ops["pool"]:
                    t(eng)

            @block.sync
            def _(eng):
                for t in self.ops["sp"]:
                    t(eng)


class _Stop(Exception):
    pass


def build(S, NB, NSEL, n_iter=28, phase1_only=False, stop_at=None):
    holder = {}
    try:
        return _build(S, NB, NSEL, n_iter, phase1_only, stop_at, holder)
    except _Stop:
        P = holder["P"]
        P.barrier()
        return holder["nc"]


def _build(S, NB, NSEL, n_iter, phase1_only, stop_at, holder):
    NT = S // 128
    TOK = NB * S
    NTT = TOK // 128
    TG = 256
    NG = TOK // TG
    nc = bass.Bass("TRN2", target_bir_lowering=False)
    holder["nc"] = nc

    def mark(name):
        if stop_at == name:
            raise _Stop()

    def din(name, shape, dt=F32):
        return nc.dram_tensor(name, shape, dt, kind="ExternalInput").ap()

    x_d = din("x", [TOK, D])
    cT_d = din("cT", [128, 8 * NB])
    wada_d = din("w_ada", [D, 6 * D])
    bada_d = din("b_ada", [128, 48])
    g1_d = din("g1", [128, 8])
    g2_d = din("g2", [128, 8])
    win_d = din("w_in", [D, NCOL])
    qg_d = din("qg", [128, 1])
    kg_d = din("kg", [128, 1])
    cw_d = din("cw", [128, 4 * 31])
    cb_d = din("cb", [128, 4])
    clg_d = din("clg", [128, 4])
    clb_d = din("clb", [128, 4])
    goc_d = din("goc", [128, 4])
    goa_d = din("goa", [128, 4])
    biasT_d = din("biasT", [128, 2 * 8 * 128])
    biasfar_d = din("biasfar", [128, 8])
    wout_d = din("w_out", [D, D])
    wpq_d = din("w_pq", [D, 2048])
    k1T_d = din("k1T", [128, 8 * 128])
    k2T_d = din("k2T", [128, 8 * 128])
    uT_d = din("uT", [D, 16384])
    v_d = din("vtab", [16384, D])
    out_d = nc.dram_tensor("out", [TOK, D], F32, kind="ExternalOutput").ap()
    h2T_s = nc.dram_tensor("h2T_s", [TOK, D], BF16, kind="Internal").ap()
    uTb_s = nc.dram_tensor("uTb_s", [D, 16384], BF16, kind="Internal").ap()
    vb_s = nc.dram_tensor("vb_s", [16384, D], BF16, kind="Internal").ap()

    with ExitStack() as st:
        P = Prog(nc, st)
        holder["P"] = P

        def V(fn, r=(), w=()):
            P.op("dve", fn, r, w)

        def A(fn, r=(), w=()):
            P.op("act", fn, r, w)

        def G(fn, r=(), w=()):
            P.op("pool", fn, r, w)

        def T(fn, r=(), w=()):
            P.op("pe", fn, r, w)

        def sbuf(stack, name, shape, dt):
            return stack.enter_context(nc.sbuf_tensor("sb_" + name, shape, dt))

        banks = [st.enter_context(nc.psum_tensor("bk%d" % i, [128, 512], F32)) for i in range(8)]

        class Rot:
            def __init__(self, idxs):
                self.idxs = idxs
                self.n = 0

            def get(self):
                i = self.idxs[self.n % len(self.idxs)]
                self.n += 1
                return banks[i], "bk%d" % i

        ident_f = sbuf(st, "ident_f", [128, 128], F32)
        ident_b = sbuf(st, "ident_b", [128, 128], BF16)
        ones_f = sbuf(st, "ones_f", [128, 128], F32)
        bones_f = sbuf(st, "bones_f", [128, 128], F32)
        iota_f = sbuf(st, "iota_f", [128, 128], F32)
        iota_b = sbuf(st, "iota_b", [128, 128], BF16)
        pid_f = sbuf(st, "pid_f", [128, 1], F32)
        epsc = sbuf(st, "epsc", [128, 1], F32)
        a1 = sbuf(st, "a1", [128, NB, 8], F32)
        sh1 = sbuf(st, "sh1", [128, NB, 8], F32)
        gt1 = sbuf(st, "gt1", [128, NB, 8], F32)
        a2 = sbuf(st, "a2", [128, NB, 8], F32)
        sh2 = sbuf(st, "sh2", [128, NB, 8], F32)
        gt2 = sbuf(st, "gt2", [128, NB, 8], F32)
        g1 = sbuf(st, "g1", [128, 8], F32)
        g2 = sbuf(st, "g2", [128, 8], F32)

        G(lambda e: e.iota(iota_f[:], pattern=[[1, 128]], base=0, channel_multiplier=0,
                           allow_small_or_imprecise_dtypes=True), w=["iota"])
        G(lambda e: e.iota(pid_f[:], pattern=[[0, 1]], base=0, channel_multiplier=1,
                           allow_small_or_imprecise_dtypes=True), w=["pid"])
        V(lambda e: e.tensor_scalar(ident_f[:], iota_f[:], pid_f[:, 0:1], None, ALU.is_equal),
          ["iota", "pid"], ["ident_f"])
        V(lambda e: e.tensor_copy(ident_b[:], ident_f[:]), ["ident_f"], ["ident_b"])
        V(lambda e: e.tensor_copy(iota_b[:], iota_f[:]), ["iota"], ["iota_b"])
        V(lambda e: e.memset(ones_f[:], 1.0), w=["ones_f"])
        V(lambda e: e.memset(bones_f[:], 0.0), w=["bones_f"])
        V(lambda e: e.memset(bones_f[0:64, 0:64], 1.0), ["bones_f"], ["bones_f"])
        V(lambda e: e.memset(bones_f[64:128, 64:128], 1.0), ["bones_f"], ["bones_f"])
        V(lambda e: e.memset(epsc[:], EPS), w=["epsc"])
        P.dma("sp", g1[:], g1_d[:, :], writes=["g1"])
        P.dma("sp", g2[:], g2_d[:, :], writes=["g2"])

        def rsqrt(out, src, scale, rk, wk):
            np_ = out.shape[0]
            A(lambda e: e.activation(out, src, AF.Sqrt, bias=epsc[0:np_, 0:1], scale=scale),
              list(rk) + ["epsc"], wk)
            V(lambda e: e.reciprocal(out, out), wk, wk)

        with ExitStack() as s0:
            cT = sbuf(s0, "cT", [128, 8 * NB], F32)
            scs = sbuf(s0, "scs", [128, 8 * NB], F32)
            bada = sbuf(s0, "bada", [128, 48], F32)
            mod = sbuf(s0, "mod", [128, 48, NB], F32)
            stg = sbuf(s0, "stg", [128, 2, 8, 1024], F32)
            P.dma("sp", cT[:], cT_d[:, :], writes=["cT"])
            P.dma("sp", bada[:], bada_d[:, :], writes=["bada"])
            A(lambda e: e.activation(scs[:], cT[:], AF.Silu), ["cT"], ["scs"])
            rot = Rot(list(range(8)))
            for fp in range(6):
                sl = fp % 2
                P.dma("sp", stg[:, sl], wada_d[:, fp * 1024:(fp + 1) * 1024].rearrange(
                    "(k p) f -> p k f", p=128), writes=["stg%d" % sl])
                bk, bkk = rot.get()

                def mm(e, sl=sl, bk=bk):
                    last = None
                    for fc in range(8):
                        for k in range(8):
                            last = e.matmul(bk[:, fc * NB:(fc + 1) * NB],
                                            stg[:, sl, k, fc * 128:(fc + 1) * 128],
                                            scs[:, k * NB:(k + 1) * NB],
                                            start=(k == 0), stop=(k == 7))
                    return last
                T(mm, ["stg%d" % sl, "scs"], [bkk])
                for b in range(NB):
                    V(lambda e, b=b, bk=bk, fp=fp: e.tensor_tensor(
                        mod[:, fp * 8:(fp + 1) * 8, b],
                        bk[:, 0:8 * NB].rearrange("p (f b) -> p f b", b=NB)[:, :, b],
                        bada[:, fp * 8:(fp + 1) * 8], ALU.add), [bkk, "bada"], ["mod"])
            for b in range(NB):
                V(lambda e, b=b: e.scalar_tensor_tensor(a1[:, b, :], mod[:, 8:16, b], 1.0, g1[:],
                                                        ALU.add, ALU.mult), ["mod", "g1"], ["a1"])
                V(lambda e, b=b: e.scalar_tensor_tensor(a2[:, b, :], mod[:, 32:40, b], 1.0, g2[:],
                                                        ALU.add, ALU.mult), ["mod", "g2"], ["a2"])
                V(lambda e, b=b: e.tensor_copy(sh1[:, b, :], mod[:, 0:8, b]), ["mod"], ["sh1"])
                V(lambda e, b=b: e.tensor_copy(gt1[:, b, :], mod[:, 16:24, b]), ["mod"], ["gt1"])
                V(lambda e, b=b: e.tensor_copy(sh2[:, b, :], mod[:, 24:32, b]), ["mod"], ["sh2"])
                V(lambda e, b=b: e.tensor_copy(gt2[:, b, :], mod[:, 40:48, b]), ["mod"], ["gt2"])

        mark("adaln")
        P.barrier()

        def gate_bcast(dst, gt, b, gkey, dkey, rot):
            for half in range(2):
                bk, bkk = rot.get()
                for kk in range(4):
                    k = half * 4 + kk
                    V(lambda e, k=k: e.tensor_scalar(rep[:], ones_f[:], gt[:, b, k:k + 1], None,
                                                     ALU.mult), ["ones_f", gkey], ["rep"])
                    T(lambda e, kk=kk, bk=bk: e.matmul(bk[:, kk * 128:(kk + 1) * 128], rep[:],
                                                       ident_f[:], start=True, stop=True),
                      ["rep", "ident_f"], [bkk])
                V(lambda e, half=half, bk=bk: e.tensor_copy(dst[:, half * 512:(half + 1) * 512],
                                                            bk[:]), [bkk], [dkey])

        rep = sbuf(st, "rep", [128, 128], F32)

        with ExitStack() as s1:
            w_in = sbuf(s1, "w_in", [128, 8, NCOL], BF16)
            w_out = sbuf(s1, "w_out", [128, 8, D], BF16)
            kT = sbuf(s1, "kT", [128, 4, S], BF16)
            vsb = sbuf(s1, "vsb", [128, NT, 8, 65], BF16)
            kiT = sbuf(s1, "kiT", [64, S], BF16)
            idx = sbuf(s1, "idx", [128, max(S, NCOL)], F32)
            mbs = sbuf(s1, "mbs", [128, 2, S], BF16)
            xts = sbuf(s1, "xts", [128, 2, D], F32)
            Dg = sbuf(s1, "Dg", [128, 2, 128], F32)
            gt1_bc = xts[:, 1]
            hT = sbuf(s1, "hT", [128, 8, 128], BF16)
            h2T = hT
            mixTs = sbuf(s1, "mixTs", [128, 2, 8, 128], BF16)
            ubuf = sbuf(s1, "ubuf", [128, 4, 158], F32)
            yc = sbuf(s1, "yc", [128, 4, 128], F32)
            t2 = yc
            mean = sbuf(s1, "mean", [128, 128], F32)
            var = sbuf(s1, "var", [128, 128], F32)
            r3 = mean
            sq = sbuf(s1, "sq", [128, 512], F32)
            ysq = sq[:].rearrange("p (c t) -> p c t", c=4)
            rl = sq[:].rearrange("p (o t) -> p o t", o=1)
            zc = yc
            rq = sbuf(s1, "rq", [128, 512], F32)
            print("phase1 sbuf bytes free:", nc.sbuf_bytes_remaining)
            qn = sq
            qpads = sbuf(s1, "qpads", [128, 2, 8, 128], BF16)
            qiT = sbuf(s1, "qiT", [64, 4, 128], BF16)
            Eb = sbuf(s1, "Eb", [128, 2, 512], BF16)
            attn = sbuf(s1, "attn", [128, 512], F32)
            biasTb = sbuf(s1, "biasTb", [128, 2, 8, 128], BF16)
            biasfar = sbuf(s1, "biasfar", [128, 8], F32)
            sm = sbuf(s1, "sm", [128, 64], F32)
            wi = sbuf(s1, "wi", [128, 12], F32)
            bis = sbuf(s1, "bis", [128, 8], F32)
            qg = sbuf(s1, "qg", [128, 1], F32)
            kg = sbuf(s1, "kg", [128, 1], F32)
            cw = sbuf(s1, "cw", [128, 4, 31], F32)
            cb = sbuf(s1, "cb", [128, 4], F32)
            clg = sbuf(s1, "clg", [128, 4], F32)
            clb = sbuf(s1, "clb", [128, 4], F32)
            goc = sbuf(s1, "goc", [128, 4], F32)
            goa = sbuf(s1, "goa", [128, 4], F32)

            for nm, t, dsrc in (("qg", qg, qg_d), ("kg", kg, kg_d), ("cb", cb, cb_d),
                                ("clg", clg, clg_d), ("clb", clb, clb_d), ("goc", goc, goc_d),
                                ("goa", goa, goa_d), ("biasfar", biasfar, biasfar_d)):
                P.dma("sp", t[:], dsrc[:, :], writes=[nm])
            P.dma("sp", cw[:], cw_d[:, :].rearrange("p (c j) -> p c j", j=31), writes=["cw"])
            V(lambda e: e.tensor_scalar(qg[:], qg[:], 0.125, None, ALU.mult), ["qg"], ["qg"])
            for blk in range(2):
                P.dma("sp", idx[:, 0:1024], biasT_d[:, blk * 1024:(blk + 1) * 1024], writes=["idx"])
                for h in range(8):
                    V(lambda e, h=h, blk=blk: e.tensor_scalar(
                        biasTb[:, blk, h, :], idx[:, h * 128:(h + 1) * 128],
                        biasfar[:, h:h + 1], None, ALU.subtract), ["idx", "biasfar"], ["biasTb"])
            V(lambda e: e.memset(qpads[:], 0.0), w=["qpad0", "qpad1"])
            V(lambda e: e.memset(vsb[:], 1.0), w=["vsb%d" % j for j in range(NT)])

            for k in range(8):
                P.dma("sp", idx[:, 0:NCOL], win_d[k * 128:(k + 1) * 128, :], writes=["idx"])
                if k % 2 == 0:
                    G(lambda e, k=k: e.tensor_copy(w_in[:, k, :], idx[:, 0:NCOL]), ["idx"], ["w_in"])
                else:
                    A(lambda e, k=k: e.copy(w_in[:, k, :], idx[:, 0:NCOL]), ["idx"], ["w_in"])
            mark("setup1")
            P.barrier()
            rot = Rot(list(range(6)))
            tb_eps = -(2.0 ** -20)

            def norm_to_T(src, ssq_col, r_col, ak, shk, b, dstT, dkey, skey, dsl):
                dks = ["%s%d" % (dkey, k) for k in range(8)]
                A(lambda e: e.activation(dstT[:].rearrange("p k t -> p (k t)"), src, AF.Square,
                                         accum_out=sm[:, ssq_col:ssq_col + 1]),
                  [skey], dks + ["sm%d" % ssq_col])
                rsqrt(sm[:, r_col:r_col + 1], sm[:, ssq_col:ssq_col + 1], 1.0 / D,
                      ["sm%d" % ssq_col], ["sm%d" % r_col])
                V(lambda e: e.tensor_scalar(Dg[:, dsl, :], ident_f[:], sm[:, r_col:r_col + 1], None, ALU.mult),
                  ["ident_f", "sm%d" % r_col], ["Dg%d" % dsl])
                for half in range(2):
                    bk, bkk = rot.get()

                    def tr(e, half=half, bk=bk):
                        last = None
                        for kk in range(4):
                            k = half * 4 + kk
                            last = e.matmul(bk[:, kk * 128:(kk + 1) * 128], src[:, k * 128:(k + 1) * 128],
                                            Dg[:, dsl, :], start=True, stop=True)
                        return last
                    T(tr, [skey, "Dg%d" % dsl], [bkk])
                    for kk in range(4):
                        k = half * 4 + kk
                        V(lambda e, k=k, kk=kk, bk=bk: e.tensor_scalar(
                            dstT[:, k, :], bk[:, kk * 128:(kk + 1) * 128],
                            ak[:, b, k:k + 1], shk[:, b, k:k + 1], ALU.mult, ALU.add),
                          [bkk, "a1", "a2", "sh1", "sh2"], ["%s%d" % (dkey, k)])

            hTk = ["hT%d" % k for k in range(8)]
            h2Tk = ["hT%d" % k for k in range(8)]
            mixk = ["mix%d" % k for k in range(8)]

            for b in range(NB):
                gate_bcast(gt1_bc, gt1, b, "gt1", "xt1", rot)
                for k in range(8):
                    P.dma("sp", idx[:, 0:D], wout_d[k * 128:(k + 1) * 128, :], writes=["idx"])
                    (V if k % 2 == 0 else G)(lambda e, k=k: e.tensor_tensor(
                        w_out[:, k, :], idx[:, 0:D], gt1_bc, ALU.mult), ["idx", "xt1"], ["w_out"])
                mark("wout%d" % b)
                def front(b, i):
                    t0 = i * 128
                    r0 = b * S + t0
                    NK = t0 + 128
                    NKB = i + 1
                    ps_ = i % 2
                    vk = "vsb%d" % i
                    kTk = "kT%d" % i
                    kik = "kiT%d" % i
                    xt = xts[:, ps_]
                    xtk = "xt%d" % ps_
                    mb = mbs[:, ps_]
                    mbk = "mb%d" % ps_
                    qpad = qpads[:, ps_]
                    qpk = "qpad%d" % ps_
                    mixT = mixTs[:, ps_]
                    mixk = ["mix%d_%d" % (ps_, k) for k in range(8)]
                    P.dma("sp", xt, x_d[r0:r0 + 128, :], writes=[xtk])
                    norm_to_T(xt, 0, 1, a1, sh1, b, hT, "hT", xtk, 0)
                    def proj_fm(bk, col0, ncols, ncolblk, M):
                        def f(e):
                            last = None
                            for j in range(ncolblk):
                                for k in range(8):
                                    last = e.matmul(bk[0:M, j * 128:(j + 1) * 128],
                                                    w_in[:, k, col0 + j * M:col0 + (j + 1) * M],
                                                    hT[:, k, :], start=(k == 0), stop=(k == 7))
                            return last
                        return f

                    def qknorm(bsrc, bsrck, gain, gk, is_q):
                        A(lambda e: e.activation(sq[:], bsrc[:], AF.Square), [bsrck], ["sq"])
                        bn, bnk = rot.get()
                        T(lambda e: e.matmul(bn[:], bones_f[:], sq[:], start=True, stop=True),
                          ["sq", "bones_f"], [bnk])
                        rsqrt(rq[:], bn[:], 1.0 / 64, [bnk], ["rq"])
                        V(lambda e: e.scalar_tensor_tensor(qn[:], bsrc[:], gain[:, 0:1], rq[:],
                                                           ALU.mult, ALU.mult), [bsrck, gk, "rq", "sq"], ["sq"])
                        if is_q:
                            for eh in range(2):
                                ps = slice(eh * 64, (eh + 1) * 64)
                                G(lambda e, ps=ps, eh=eh: e.tensor_copy(
                                    qpad[ps].rearrange("p (c two) t -> p c two t", two=2)[:, :, eh, :],
                                    qn[ps, :].rearrange("p (c t) -> p c t", c=4)), ["sq"], [qpk])
                        else:
                            G(lambda e: e.tensor_copy(kT[:, :, t0:t0 + 128],
                                                      qn[:].rearrange("p (c t) -> p c t", c=4)),
                              ["sq"], [kTk])
                    bcv, bcvk = rot.get()
                    T(proj_fm(bcv, 0, 512, 4, 128), hTk + ["w_in"], [bcvk])
                    bcg, bcgk = rot.get()
                    T(proj_fm(bcg, 512, 512, 4, 128), hTk + ["w_in"], [bcgk])
                    A(lambda e, bcg=bcg: e.activation(ubuf[:, :, 30:158], bcg[:].rearrange("p (c t) -> p c t", c=4),
                                                      AF.Sigmoid), [bcgk], ["ubuf_c"])
                    if i == 0:
                        G(lambda e: e.memset(ubuf[:, :, 0:30], 0.0), w=["ubuf_h"])
                    V(lambda e, bcv=bcv: e.tensor_tensor(ubuf[:, :, 30:158],
                                                         bcv[:].rearrange("p (c t) -> p c t", c=4),
                                                         ubuf[:, :, 30:158], ALU.mult), [bcvk, "ubuf_c"], ["ubuf_c"])
                    bq, bqk = rot.get()
                    T(proj_fm(bq, 1024, 512, 4, 128), hTk + ["w_in"], [bqk])
                    qknorm(bq, bqk, qg, "qg", True)
                    bkk_, bkkk = rot.get()
                    T(proj_fm(bkk_, 1536, 512, 4, 128), hTk + ["w_in"], [bkkk])
                    qknorm(bkk_, bkkk, kg, "kg", False)
                    bv, bvk = rot.get()

                    def projv(e, bv=bv):
                        last = None
                        for k in range(8):
                            last = e.matmul(bv[:, 0:512], hT[:, k, :], w_in[:, k, 2048:2560],
                                            start=(k == 0), stop=(k == 7))
                        return last
                    T(projv, hTk + ["w_in"], [bvk])
                    A(lambda e, bv=bv: e.copy(vsb[:, i, :, 0:64], bv[:].rearrange("p (h d) -> p h d", h=8)),
                      [bvk], [vk])
                    bi, bik = rot.get()

                    bi2, bi2k = rot.get()

                    def proji2(e, bi=bi, bi2=bi2):
                        last = None
                        for j in range(4):
                            for k in range(8):
                                last = e.matmul(bi[0:64, j * 128:(j + 1) * 128],
                                                w_in[:, k, 2560 + j * 64:2560 + (j + 1) * 64],
                                                hT[:, k, :], start=(k == 0), stop=(k == 7))
                        for k in range(8):
                            last = e.matmul(bi2[0:64, 0:128], w_in[:, k, 2816:2880], hT[:, k, :],
                                            start=(k == 0), stop=(k == 7))
                        for k in range(8):
                            last = e.matmul(bi2[:, 128:132], hT[:, k, :], w_in[:, k, 2880:2884],
                                            start=(k == 0), stop=(k == 7))
                        return last
                    T(proji2, hTk + ["w_in"], [bik, bi2k])

                    V(lambda e, bi=bi: e.tensor_copy(qiT[:], bi[0:64, :].rearrange("p (h t) -> p h t", h=4)),
                      [bik], ["qiT"])
                    V(lambda e, bi2=bi2: e.tensor_copy(kiT[:, t0:t0 + 128], bi2[0:64, 0:128]), [bi2k], [kik])
                    V(lambda e, bi2=bi2: e.tensor_copy(wi[:, 0:4], bi2[:, 128:132]), [bi2k], ["wi"])
                    kik_all = ["kiT%d" % j for j in range(NKB)]
                    G(lambda e: e.iota(idx[:, 0:NK], pattern=[[1, NK]], base=0, channel_multiplier=0,
                                       allow_small_or_imprecise_dtypes=True), w=["idx"])
                    A(lambda e: e.mul(idx[:, 0:NK], idx[:, 0:NK], tb_eps), ["idx"], ["idx"])
                    nrl = 0
                    for c0 in range(0, NK, 512):
                        cn = min(512, NK - c0)
                        for h in range(4):
                            bx, bxk = rot.get()
                            T(lambda e, bx=bx, h=h, c0=c0, cn=cn: e.matmul(
                                bx[:, 0:cn], qiT[:, h, :], kiT[:, c0:c0 + cn], start=True, stop=True),
                              ["qiT"] + kik_all, [bxk])
                            sl = 0
                            A(lambda e, bx=bx, h=h, cn=cn, sl=sl: e.activation(
                                rl[:, sl, 0:cn], bx[:, 0:cn], AF.Relu),
                              [bxk], ["sq"])
                            V(lambda e, h=h, c0=c0, cn=cn, sl=sl: e.scalar_tensor_tensor(
                                idx[:, c0:c0 + cn], rl[:, sl, 0:cn], wi[:, h:h + 1], idx[:, c0:c0 + cn],
                                ALU.mult, ALU.add), ["sq", "wi", "idx"], ["idx"])
                    mark("index%d" % i)
                    need_search = NK > NSEL
                    if need_search:
                        V(lambda e: e.tensor_reduce(bis[:, 0:1], idx[:, 0:NK], AX.X, ALU.max), ["idx"], ["bis"])
                        V(lambda e: e.tensor_reduce(bis[:, 1:2], idx[:, 0:NK], AX.X, ALU.min), ["idx"], ["bis"])
                        V(lambda e: e.tensor_tensor(bis[:, 2:3], bis[:, 0:1], bis[:, 1:2], ALU.subtract),
                          ["bis"], ["bis"])
                    else:
                        V(lambda e: e.memset(bis[:, 1:2], -1e29), w=["bis"])
                    V(lambda e: e.memset(idx[0:64, t0 + 64:t0 + 128], -1e30), ["idx"], ["idx"])
                    if need_search:
                        for it in range(1, n_iter + 1):
                            sc_ = 2.0 ** (-it)
                            V(lambda e, sc_=sc_: e.scalar_tensor_tensor(bis[:, 3:4], bis[:, 2:3], sc_, bis[:, 1:2],
                                                                        ALU.mult, ALU.add), ["bis"], ["bis"])
                            V(lambda e: e.tensor_scalar(mb[:, 0:NK], idx[:, 0:NK], bis[:, 3:4], None,
                                                        ALU.is_ge, ALU.add, accum_out=bis[:, 4:5]),
                              ["idx", "bis"], [mbk, "bis"])
                            V(lambda e, sc_=sc_: e.tensor_scalar(bis[:, 5:6], bis[:, 4:5], NSEL - 0.5, sc_,
                                                                 ALU.is_ge, ALU.mult), ["bis"], ["bis"])
                            V(lambda e: e.scalar_tensor_tensor(bis[:, 1:2], bis[:, 5:6], bis[:, 2:3], bis[:, 1:2],
                                                               ALU.mult, ALU.add), ["bis"], ["bis"])
                    V(lambda e: e.tensor_scalar(mb[:, 0:NK], idx[:, 0:NK], bis[:, 1:2], NEG,
                                                ALU.is_lt, ALU.mult), ["idx", "bis"], [mbk])

                    mark("bisect%d" % i)
                    for j in range(31):
                        for c in range(4):
                            if j == 0:
                                V(lambda e, c=c: e.tensor_scalar(yc[:, c, :], ubuf[:, c, 0:128],
                                                                 cw[:, c, 0:1], cb[:, c:c + 1],
                                                                 ALU.mult, ALU.add),
                                  ["ubuf_h", "ubuf_c", "cw", "cb"], ["yc%d" % c])
                            else:
                                V(lambda e, c=c, j=j: e.scalar_tensor_tensor(
                                    yc[:, c, :], ubuf[:, c, j:j + 128], cw[:, c, j:j + 1], yc[:, c, :],
                                    ALU.mult, ALU.add),
                                  ["ubuf_h", "ubuf_c", "cw"], ["yc%d" % c])
                    yck = ["yc%d" % c for c in range(4)]
                    ysqk = ["sq"]
                    if i + 1 < NT:
                        G(lambda e: e.tensor_copy(ubuf[:, :, 0:30], ubuf[:, :, 128:158]),
                          ["ubuf_c"], ["ubuf_h"])
                    G(lambda e: e.tensor_tensor(ysq[:], yc[:], yc[:], ALU.mult), yck, ysqk)
                    bs, bsk = rot.get()

                    def lnstat(e, bs=bs):
                        last = None
                        for c in range(4):
                            last = e.matmul(bs[:, 0:128], ones_f[:], yc[:, c, :], start=(c == 0), stop=(c == 3))
                        for c in range(4):
                            last = e.matmul(bs[:, 128:256], ones_f[:], ysq[:, c, :], start=(c == 0), stop=(c == 3))
                        return last
                    T(lnstat, yck + ysqk + ["ones_f"], [bsk])
                    V(lambda e, bs=bs: e.tensor_scalar(mean[:], bs[:, 0:128], 1.0 / 512, None, ALU.mult),
                      [bsk], ["mean"])
                    V(lambda e: e.tensor_tensor(var[:], mean[:], mean[:], ALU.mult), ["mean"], ["var"])
                    V(lambda e, bs=bs: e.scalar_tensor_tensor(var[:], bs[:, 128:256], 1.0 / 512, var[:],
                                                              ALU.mult, ALU.subtract), [bsk, "var"], ["var"])
                    rsqrt(var[:], var[:], 1.0, ["var"], ["var"])
                    V(lambda e: e.tensor_tensor(t2[:], yc[:], mean[:].unsqueeze(1).to_broadcast([128, 4, 128]),
                                                ALU.subtract), yck + ["mean"], yck)
                    V(lambda e: e.tensor_tensor(t2[:], t2[:], var[:].unsqueeze(1).to_broadcast([128, 4, 128]),
                                                ALU.mult), yck + ["var"], yck)
                    for c in range(4):
                        V(lambda e, c=c: e.tensor_scalar(t2[:, c, :], t2[:, c, :], clg[:, c:c + 1], clb[:, c:c + 1],
                                                         ALU.mult, ALU.add), ["yc%d" % c, "clg", "clb"], ["yc%d" % c])
                    A(lambda e: e.activation(zc[:], t2[:], AF.Silu), yck, yck)
                    zck = yck
                    G(lambda e: e.tensor_tensor(ysq[:], zc[:], zc[:], ALU.mult), zck, ysqk)
                    bs2, bs2k = rot.get()

                    def rmstat(e, bs2=bs2):
                        last = None
                        for c in range(4):
                            last = e.matmul(bs2[:, 0:128], ones_f[:], ysq[:, c, :], start=(c == 0), stop=(c == 3))
                        return last
                    T(rmstat, ysqk + ["ones_f"], [bs2k])
                    rsqrt(r3[:], bs2[:, 0:128], 1.0 / 512, [bs2k, "mean"], ["mean"])
                    for c in range(4):
                        V(lambda e, c=c: e.scalar_tensor_tensor(mixT[:, c, :], zc[:, c, :], goc[:, c:c + 1],
                                                                r3[:], ALU.mult, ALU.mult),
                          ["yc%d" % c, "goc", "mean"], [mixk[c]])


                def back(b, i):
                    t0 = i * 128
                    r0 = b * S + t0
                    NK = t0 + 128
                    NKB = i + 1
                    ps_ = i % 2
                    vk = "vsb%d" % i
                    kTk = "kT%d" % i
                    kik = "kiT%d" % i
                    xt = xts[:, ps_]
                    xtk = "xt%d" % ps_
                    mb = mbs[:, ps_]
                    mbk = "mb%d" % ps_
                    qpad = qpads[:, ps_]
                    qpk = "qpad%d" % ps_
                    mixT = mixTs[:, ps_]
                    mixk = ["mix%d_%d" % (ps_, k) for k in range(8)]
                    pvA, pvAk = banks[6], "bk6"
                    pvB, pvBk = banks[7], "bk7"
                    kT_all = ["kT%d" % j for j in range(NKB)]
                    v_all = ["vsb%d" % j for j in range(NKB)]
                    items = [(h, g0) for h in range(8) for g0 in range(0, NKB, 4)]

                    def emit_lg(n):
                        h, g0 = items[n]
                        c = h // 2
                        blks = list(range(g0, min(g0 + 4, NKB)))
                        bl, blk_ = rot.get()

                        def lg(e, bl=bl, blks=blks, h=h, c=c):
                            last = None
                            for n_, kb in enumerate(blks):
                                o = bl[:, n_ * 128:(n_ + 1) * 128]
                                e.matmul(o, kT[:, c, kb * 128:(kb + 1) * 128], qpad[:, h, :],
                                         start=True, stop=False)
                                if kb >= i - 1:
                                    e.matmul(o, ident_b[:], biasTb[:, kb - (i - 1), h, :],
                                             start=False, stop=False)
                                last = e.matmul(o, mb[:, kb * 128:(kb + 1) * 128], ident_b[:],
                                                start=False, stop=True)
                            return last
                        T(lg, kT_all + [qpk, mbk, "ident_b", "biasTb"], [blk_])
                        return bl, blk_, blks

                    nxt = emit_lg(0)
                    for n in range(len(items)):
                        bl, blk_, blks = nxt
                        if n + 1 < len(items):
                            nxt = emit_lg(n + 1)
                        h, g0 = items[n]
                        pv, pvk = (pvA, pvAk) if h < 4 else (pvB, pvBk)
                        hh = h % 4
                        nb_ = len(blks)
                        sl = n % 2
                        A(lambda e, bl=bl, nb_=nb_, sl=sl, h=h: e.activation(
                            Eb[:, sl, 0:nb_ * 128], bl[:, 0:nb_ * 128], AF.Exp,
                            bias=biasfar[:, h:h + 1], scale=1.0), [blk_, "biasfar"], ["Eb%d" % sl])

                        def pvm(e, pv=pv, blks=blks, sl=sl, h=h, hh=hh):
                            last = None
                            for n_, kb in enumerate(blks):
                                last = e.matmul(pv[:, hh * 65:(hh + 1) * 65],
                                                Eb[:, sl, n_ * 128:(n_ + 1) * 128], vsb[:, kb, h, :],
                                                start=(kb == 0), stop=(kb == NKB - 1))
                            return last
                        T(pvm, ["Eb%d" % sl] + v_all, [pvk])
                    for hf, (pv, pvk) in enumerate(((pvA, pvAk), (pvB, pvBk))):
                        pv3 = pv[:, 0:260].rearrange("p (h d) -> p h d", d=65)
                        V(lambda e, pv3=pv3, hf=hf: e.reciprocal(sm[:, 8 + hf * 4:12 + hf * 4], pv3[:, :, 64]),
                          [pvk], ["smr%d" % hf])
                        V(lambda e, pv3=pv3, hf=hf: e.tensor_tensor(
                            attn[:, hf * 256:(hf + 1) * 256].rearrange("p (h d) -> p h d", d=64),
                            pv3[:, :, 0:64],
                            sm[:, 8 + hf * 4:12 + hf * 4].unsqueeze(2).to_broadcast([128, 4, 64]),
                            ALU.mult), [pvk, "smr%d" % hf], ["attn%d" % hf])
                    A(lambda e: e.activation(mixT[:, 4:8, :].rearrange("p c t -> p (c t)"), attn[:], AF.Square,
                                             accum_out=sm[:, 2:3]),
                      ["attn0", "attn1"], mixk[4:8] + ["sm2"])
                    rsqrt(sm[:, 3:4], sm[:, 2:3], 1.0 / 512, ["sm2"], ["sm3"])
                    V(lambda e: e.tensor_scalar(Dg[:, 1, :], ident_f[:], sm[:, 3:4], None, ALU.mult),
                      ["ident_f", "sm3"], ["Dg1"])
                    bt, btk = rot.get()

                    def atr(e, bt=bt):
                        last = None
                        for c in range(4):
                            last = e.matmul(bt[:, c * 128:(c + 1) * 128], attn[:, c * 128:(c + 1) * 128],
                                            Dg[:, 1, :], start=True, stop=True)
                        return last
                    T(atr, ["attn0", "attn1", "Dg1"], [btk])
                    for c in range(4):
                        V(lambda e, c=c, bt=bt: e.tensor_scalar(mixT[:, 4 + c, :], bt[:, c * 128:(c + 1) * 128],
                                                                goa[:, c:c + 1], None, ALU.mult),
                          [btk, "goa"], [mixk[4 + c]])

                    for half in range(2):
                        bo, bok = rot.get()

                        def op_(e, bo=bo, half=half):
                            last = None
                            for k in range(8):
                                last = e.matmul(bo[:], mixT[:, k, :], w_out[:, k, half * 512:(half + 1) * 512],
                                                start=(k == 0), stop=(k == 7))
                            return last
                        T(op_, mixk + ["w_out"], [bok])
                        hs = slice(half * 512, (half + 1) * 512)
                        V(lambda e, bo=bo, hs=hs: e.tensor_tensor(xt[:, hs], bo[:], xt[:, hs], ALU.add),
                          [bok, xtk], [xtk])
                    P.dma("sp", out_d[r0:r0 + 128, :], xt, reads=[xtk],
                          writes=["out_g%d" % (r0 // TG)], semkey="st_x1")
                    norm_to_T(xt, 4, 5, a2, sh2, b, h2T, "hT", xtk, 1)
                    P.dma("sp", h2T_s[r0:r0 + 128, :], h2T[:].rearrange("p k t -> p (k t)"), reads=h2Tk,
                          writes=["h2s%d" % (r0 // 128)], semkey="st_h2")


                front(b, 0)
                for i in range(NT):
                    if i + 1 < NT:
                        front(b, i + 1)
                    back(b, i)

        if phase1_only:
            P.final_wait("sp", ["out_g%d" % g for g in range(NG)])
            return nc
        P.barrier()
        with ExitStack() as s15:
            cin = sbuf(s15, "cin", [128, 2, 8192], F32)
            cout = sbuf(s15, "cout", [128, 2, 8192], BF16)
            srcs = ((uT_d.rearrange("(p a) e -> p (a e)", p=128), uTb_s.rearrange("(p a) e -> p (a e)", p=128), "uTb_s"),
                    (v_d.rearrange("(p a) e -> p (a e)", p=128), vb_s.rearrange("(p a) e -> p (a e)", p=128), "vb_s"))
            n = 0
            for (src, dst, key) in srcs:
                for pc in range(16):
                    sl = n % 2
                    n += 1
                    cs = slice(pc * 8192, (pc + 1) * 8192)
                    P.dma("sp", cin[:, sl, :], src[:, cs], writes=["cin%d" % sl])
                    V(lambda e, sl=sl: e.tensor_copy(cout[:, sl, 0:3072], cin[:, sl, 0:3072]),
                      ["cin%d" % sl], ["couta%d" % sl])
                    A(lambda e, sl=sl: e.copy(cout[:, sl, 3072:6144], cin[:, sl, 3072:6144]),
                      ["cin%d" % sl], ["coutb%d" % sl])
                    G(lambda e, sl=sl: e.tensor_copy(cout[:, sl, 6144:8192], cin[:, sl, 6144:8192]),
                      ["cin%d" % sl], ["coutc%d" % sl])
                    P.dma("sp", dst[:, cs], cout[:, sl, :],
                          reads=["couta%d" % sl, "coutb%d" % sl, "coutc%d" % sl], writes=[key],
                          semkey="st_c%d" % sl)

        P.barrier()
        with ExitStack() as s2:
            w_pq = sbuf(s2, "w_pq", [128, 8, 2048], BF16)
            with ExitStack() as s2a:
                stg = sbuf(s2a, "stgq", [128, 2, 2048], F32)
                for k in range(8):
                    sl = k % 2
                    P.dma("sp", stg[:, sl, :], wpq_d[k * 128:(k + 1) * 128, :], writes=["stgq%d" % sl])
                    if k % 2 == 0:
                        G(lambda e, k=k, sl=sl: e.tensor_copy(w_pq[:, k, :], stg[:, sl, :]),
                          ["stgq%d" % sl], ["w_pq"])
                    else:
                        A(lambda e, k=k, sl=sl: e.copy(w_pq[:, k, :], stg[:, sl, :]),
                          ["stgq%d" % sl], ["w_pq"])
            P.barrier()
            k1T = sbuf(s2, "k1T", [128, 8, 128], F32)
            k2T = sbuf(s2, "k2T", [128, 8, 128], F32)
            gt2_bc = sbuf(s2, "gt2_bc", [128, NB, D], F32)
            h2 = sbuf(s2, "h2", [128, 2, 8, TG], BF16)
            qqT = sbuf(s2, "qqT", [128, 16, 128], F32)
            sc = sbuf(s2, "sc", [128, 16, 128], F32)
            mx = sbuf(s2, "mx", [128, 16, 16], F32)
            ix = sbuf(s2, "ix", [128, 16, 16], U32)
            ixf = sbuf(s2, "ixf", [128, 16, 16], F32)
            cand2 = sbuf(s2, "cand2", [128, 8, 256], F32)
            best = sbuf(s2, "best", [128, 8, 16], F32)
            pos = sbuf(s2, "pos", [128, 8, 16], U32)
            pab_u = sbuf(s2, "pab_u", [128, 2, 8, 16], U32)
            pa = sbuf(s2, "pa", [128, 8, 16], F32)
            pb = sbuf(s2, "pb", [128, 8, 16], F32)
            IJg = sbuf(s2, "IJg", [128, 3, 128], F32)
            ssum = sbuf(s2, "ssum", [128, 16], F32)
            IJgT = sbuf(s2, "IJgT", [128, 2, 3, TG], BF16)
            OHI = sbuf(s2, "OHI", [128, 2, 8, 128], BF16)
            OHJ = sbuf(s2, "OHJ", [128, 2, 8, 128], BF16)
            OHJg = sbuf(s2, "OHJg", [128, 2, 8, 128], BF16)
            W = sbuf(s2, "W", [128, TG, 128], BF16)
            ub = sbuf(s2, "ub", [128, 2, 8, 512], BF16)
            vbuf = sbuf(s2, "vbuf", [128, 2, 4, D], BF16)
            gel = sbuf(s2, "gel", [128, 2, TG], BF16)
            Z = sbuf(s2, "Z", [128, 3, TG], BF16)
            iota16 = iota_f[:, 0:16]
            sc2 = qqT
            cand = sc[:].rearrange("p m n -> p (m n)").rearrange("p (h q) -> p h q", h=8)
            oh = cand2[:].rearrange("p h (a b) -> p h a b", b=16)
            res = qqT[:].rearrange("p m n -> p (m n)")[:, 0:D]
            xr = cand2[:].rearrange("p h q -> p (h q)")[:, 0:D]

            P.dma("sp", k1T[:], k1T_d[:, :].rearrange("p (h n) -> p h n", h=8), writes=["k1T"])
            P.dma("sp", k2T[:], k2T_d[:, :].rearrange("p (h n) -> p h n", h=8), writes=["k2T"])
            rotw = Rot([4, 5, 6, 7])
            for b in range(NB):
                class _D:
                    pass
                gate_bcast(gt2_bc[:, b, :], gt2, b, "gt2", "gt2_bc", rotw)

            def gating_gen(g):
                gb = g % 2
                r0 = g * TG
                for sub in range(2):
                    rr = r0 + sub * 128
                    P.dma("sp", h2[:, gb, :, sub * 128:(sub + 1) * 128],
                          h2T_s[rr:rr + 128, :].rearrange("p (k t) -> p k t", k=8),
                          reads=["h2s%d" % (rr // 128)], writes=["h2_%d_%d" % (gb, sub)])
                    yield
                for sub in range(2):
                    ts_ = slice(sub * 128, (sub + 1) * 128)
                    for m4 in range(4):
                        bk, bkk = rotw.get()

                        def qm(e, bk=bk, m4=m4, ts_=ts_):
                            last = None
                            for mm_ in range(4):
                                m = m4 * 4 + mm_
                                for k in range(8):
                                    last = e.matmul(bk[:, mm_ * 128:(mm_ + 1) * 128],
                                                    w_pq[:, k, m * 128:(m + 1) * 128], h2[:, gb, k, ts_],
                                                    start=(k == 0), stop=(k == 7))
                            return last
                        T(qm, ["w_pq", "h2_%d_%d" % (gb, sub)], [bkk])
                        yield
                        A(lambda e, bk=bk, m4=m4: e.copy(
                            qqT[:, m4 * 4:(m4 + 1) * 4, :].rearrange("p m t -> p (m t)"), bk[:]),
                          [bkk], ["A"])
                        yield
                    for m4 in range(4):
                        bk, bkk = rotw.get()

                        def sm_(e, bk=bk, m4=m4):
                            last = None
                            for mm_ in range(4):
                                m = m4 * 4 + mm_
                                kt = k1T if m % 2 == 0 else k2T
                                last = e.matmul(bk[:, mm_ * 128:(mm_ + 1) * 128], qqT[:, m, :],
                                                kt[:, m // 2, :], start=True, stop=True)
                            return last
                        T(sm_, ["A", "k1T", "k2T"], [bkk])
                        yield
                        A(lambda e, bk=bk, m4=m4: e.copy(
                            sc[:, m4 * 4:(m4 + 1) * 4, :].rearrange("p m t -> p (m t)"), bk[:]),
                          [bkk], ["B"])
                        yield
                    sck = ["B"]
                    for m in range(16):
                        V(lambda e, m=m: e.max(mx[:, m, 0:8], sc[:, m, :]), sck, ["mx%d" % m])
                        yield
                    for m in range(16):
                        V(lambda e, m=m: e.max_index(ix[:, m, 0:8], mx[:, m, 0:8], sc[:, m, :]),
                          sck + ["mx%d" % m], ["ix%d" % m])
                        yield
                    for m in range(16):
                        V(lambda e, m=m: e.match_replace(sc2[:, m, :], mx[:, m, 0:8], sc[:, m, :], -1e30),
                          sck + ["mx%d" % m], ["A"])
                        yield
                    for m in range(16):
                        V(lambda e, m=m: e.max(mx[:, m, 8:16], sc2[:, m, :]), ["A"], ["mx%d" % m])
                        yield
                    for m in range(16):
                        V(lambda e, m=m: e.max_index(ix[:, m, 8:16], mx[:, m, 8:16], sc2[:, m, :]),
                          ["A", "mx%d" % m], ["ix%d" % m])
                        yield
                    mxk = ["mx%d" % m for m in range(16)]
                    ixk = ["ix%d" % m for m in range(16)]
                    V(lambda e: e.tensor_copy(ixf[:], ix[:]), ixk, ["ixf"])
                    yield
                    mxv = mx[:].rearrange("p (h two) a -> p h two a", two=2)
                    ixv = ixf[:].rearrange("p (h two) a -> p h two a", two=2)
                    V(lambda e: e.tensor_tensor(
                        cand.rearrange("p h (a b) -> p h a b", b=16),
                        mxv[:, :, 0, :].unsqueeze(3).to_broadcast([128, 8, 16, 16]),
                        mxv[:, :, 1, :].unsqueeze(2).to_broadcast([128, 8, 16, 16]), ALU.add),
                      mxk + ixk, ["B"])
                    yield
                    for h in range(8):
                        V(lambda e, h=h: e.max(best[:, h, 0:8], cand[:, h, :]), ["B"], ["best%d" % h])
                        yield
                    for h in range(8):
                        V(lambda e, h=h: e.max_index(pos[:, h, 0:8], best[:, h, 0:8], cand[:, h, :]),
                          ["B", "best%d" % h], ["pos%d" % h])
                        yield
                    for h in range(8):
                        V(lambda e, h=h: e.match_replace(cand2[:, h, :], best[:, h, 0:8], cand[:, h, :], -1e30),
                          ["B", "best%d" % h], ["C"])
                        yield
                    for h in range(8):
                        V(lambda e, h=h: e.max(best[:, h, 8:16], cand2[:, h, :]), ["C"], ["best%d" % h])
                        yield
                    for h in range(8):
                        V(lambda e, h=h: e.max_index(pos[:, h, 8:16], best[:, h, 8:16], cand2[:, h, :]),
                          ["C", "best%d" % h], ["pos%d" % h])
                        yield
                    bestk = ["best%d" % h for h in range(8)]
                    posk = ["pos%d" % h for h in range(8)]
                    gsl = IJg[:, 2, :].rearrange("p (h k) -> p h k", k=16)
                    V(lambda e: e.tensor_tensor(gsl, best[:], best[:, :, 0:1].to_broadcast([128, 8, 16]),
                                                ALU.subtract), bestk, ["gate"])
                    yield
                    A(lambda e: e.activation(gsl, gsl, AF.Exp), ["gate"], ["gate"])
                    yield
                    V(lambda e: e.tensor_reduce(ssum[:, 0:8], gsl, AX.X, ALU.add), ["gate"], ["ssum"])
                    yield
                    V(lambda e: e.reciprocal(ssum[:, 8:16], ssum[:, 0:8]), ["ssum"], ["ssum"])
                    yield
                    V(lambda e: e.tensor_tensor(gsl, gsl, ssum[:, 8:16].unsqueeze(2).to_broadcast([128, 8, 16]),
                                                ALU.mult), ["gate", "ssum"], ["gate"])
                    yield
                    V(lambda e: e.tensor_single_scalar(pab_u[:, 0], pos[:], 4, ALU.logical_shift_right),
                      posk, ["pab_u"])
                    yield
                    V(lambda e: e.tensor_single_scalar(pab_u[:, 1], pos[:], 15, ALU.bitwise_and),
                      posk + ["pab_u"], ["pab_u"])
                    yield
                    V(lambda e: e.tensor_copy(pa[:], pab_u[:, 0]), ["pab_u"], ["pa"])
                    yield
                    V(lambda e: e.tensor_copy(pb[:], pab_u[:, 1]), ["pab_u"], ["pb"])
                    yield
                    i16 = iota16.unsqueeze(1).unsqueeze(1).to_broadcast([128, 8, 16, 16])
                    for which, (pp, ppk) in enumerate(((pa, "pa"), (pb, "pb"))):
                        V(lambda e, pp=pp: e.tensor_tensor(oh, pp[:].unsqueeze(3).to_broadcast([128, 8, 16, 16]),
                                                           i16, ALU.is_equal), [ppk, "iota"], ["C"])
                        yield
                        V(lambda e, which=which: e.tensor_tensor(
                            oh, oh, ixv[:, :, which, :].unsqueeze(2).to_broadcast([128, 8, 16, 16]),
                            ALU.mult), ["C", "ixf"], ["C"])
                        yield
                        V(lambda e, which=which: e.tensor_reduce(
                            IJg[:, which, :].rearrange("p (h k) -> p h k", k=16), oh, AX.X, ALU.add),
                          ["C"], ["IJ%d" % which])
                        yield
                    bk, bkk = rotw.get()

                    def ijt(e, bk=bk):
                        last = None
                        for w_ in range(3):
                            last = e.transpose(bk[:, w_ * 128:(w_ + 1) * 128], IJg[:, w_, :], ident_f[:])
                        return last
                    T(ijt, ["IJ0", "IJ1", "gate", "ident_f"], [bkk])
                    yield
                    A(lambda e, bk=bk, ts_=ts_: e.copy(IJgT[:, gb, :, ts_], bk[:, 0:384].rearrange("p (w t) -> p w t", w=3)),
                      [bkk], ["IJgT%d_%d" % (gb, sub)])
                    yield

            def wbuild(g):
                gb = g % 2
                for sub in range(2):
                    for q in range(16):
                        tq = sub * 128 + q * 8
                        sl = q % 2
                        ik = "IJgT%d_%d" % (gb, sub)
                        iob = iota_b[:].unsqueeze(1).to_broadcast([128, 8, 128])
                        V(lambda e, sl=sl, tq=tq: e.tensor_tensor(
                            OHI[:, sl], iob, IJgT[:, gb, 0, tq:tq + 8].unsqueeze(2).to_broadcast([128, 8, 128]),
                            ALU.is_equal), [ik, "iota_b"], ["OHI%d" % sl])
                        V(lambda e, sl=sl, tq=tq: e.tensor_tensor(
                            OHJ[:, sl], iob, IJgT[:, gb, 1, tq:tq + 8].unsqueeze(2).to_broadcast([128, 8, 128]),
                            ALU.is_equal), [ik, "iota_b"], ["OHJ%d" % sl])
                        V(lambda e, sl=sl, tq=tq: e.tensor_tensor(
                            OHJg[:, sl], OHJ[:, sl],
                            IJgT[:, gb, 2, tq:tq + 8].unsqueeze(2).to_broadcast([128, 8, 128]),
                            ALU.mult), [ik, "OHJ%d" % sl], ["OHJg%d" % sl])
                        for q4 in range(2):
                            bk, bkk = rotw.get()

                            def wm(e, bk=bk, sl=sl, q4=q4):
                                last = None
                                for tt in range(4):
                                    t_ = q4 * 4 + tt
                                    last = e.matmul(bk[:, tt * 128:(tt + 1) * 128], OHJg[:, sl, t_, :],
                                                    OHI[:, sl, t_, :], start=True, stop=True)
                                return last
                            T(wm, ["OHJg%d" % sl, "OHI%d" % sl], [bkk])
                            A(lambda e, bk=bk, tq=tq, q4=q4: e.copy(
                                W[:, tq + q4 * 4:tq + q4 * 4 + 4, :].rearrange("p t i -> p (t i)"), bk[:]),
                              [bkk], ["W"])

            gq = gating_gen(0)
            for _ in gq:
                pass
            for g in range(NG):
                r0 = g * TG
                b = r0 // S
                gb = g % 2
                h2k = ["h2_%d_0" % gb, "h2_%d_1" % gb]
                wbuild(g)
                gq = gating_gen(g + 1) if g + 1 < NG else iter(())
                LA = 2
                GI = 5
                pend = []
                for ci in range(NEXP_CH + LA):
                    if ci < NEXP_CH:
                        cg, cc = divmod(ci, 4)
                        sl = cg % 2
                        if cc == 0:
                            e0 = cg * 512
                            P.dma("sp", ub[:, sl], uTb_s[:, e0:e0 + 512].rearrange("(k p) e -> p k e", p=128),
                                  reads=["uTb_s"], writes=["ub%d" % sl])
                            P.dma("sp", vbuf[:, sl], vb_s[e0:e0 + 512, :].rearrange("(c j) d -> j c d", j=128),
                                  reads=["vb_s"], writes=["vbuf%d" % sl])
                        by, byk = rotw.get()

                        def ym(e, by=by, sl=sl, cc=cc, gb=gb):
                            last = None
                            for k in range(8):
                                last = e.matmul(by[:, 0:TG], ub[:, sl, k, cc * 128:(cc + 1) * 128], h2[:, gb, k, :],
                                                start=(k == 0), stop=(k == 7))
                            return last
                        T(ym, ["ub%d" % sl] + h2k, [byk])
                        gs = ci % 2
                        zs = ci % 3
                        A(lambda e, by=by, gs=gs: e.activation(gel[:, gs, :], by[:, 0:TG], AF.Gelu_apprx_tanh),
                          [byk], ["gel%d" % gs])
                        V(lambda e, zs=zs, gs=gs, ci=ci: e.tensor_tensor(Z[:, zs, :], gel[:, gs, :], W[:, :, ci],
                                                                         ALU.mult),
                          ["gel%d" % gs, "W"], ["Z%d" % zs])

                        def vm(e, zs=zs, sl=sl, cc=cc, ci=ci):
                            last = None
                            for sub in range(2):
                                for half in range(2):
                                    last = e.matmul(banks[sub * 2 + half][:],
                                                    Z[:, zs, sub * 128:(sub + 1) * 128],
                                                    vbuf[:, sl, cc, half * 512:(half + 1) * 512],
                                                    start=(ci == 0), stop=(ci == NEXP_CH - 1))
                            return last
                        pend.append((vm, ["Z%d" % zs, "vbuf%d" % sl]))
                    if ci >= LA:
                        vm_, rk_ = pend.pop(0)
                        T(vm_, rk_, ["bk0", "bk1", "bk2", "bk3"])
                    for _ in range(GI):
                        next(gq, None)
                for _ in gq:
                    pass
                for sub in range(2):
                    rr = r0 + sub * 128
                    P.dma("sp", xr, out_d[rr:rr + 128, :], reads=["out_g%d" % g], writes=["C"])
                    for half in range(2):
                        hs = slice(half * 512, (half + 1) * 512)
                        V(lambda e, sub=sub, half=half, hs=hs: e.tensor_tensor(
                            res[:, hs], banks[sub * 2 + half][:], gt2_bc[:, b, hs], ALU.mult),
                          ["bk%d" % (sub * 2 + half), "gt2_bc", "A"], ["A"])
                    G(lambda e: e.tensor_tensor(res, res, xr, ALU.add), ["A", "C"], ["A"])
                    P.dma("sp", out_d[rr:rr + 128, :], res, reads=["A"], writes=["out_f"], semkey="st_res")
            P.final_wait("sp", ["out_f"])
        P.emit()
        print("instructions:", P.n_instr, {e: len(P.ops[e]) for e in ENGS}, "dma sems:", len(P.dsem))
    return nc


def _t5_bucket_table():
    import jax
    import jax.numpy as jnp
    with jax.default_device(jax.devices("cpu")[0]):
        return _t5_bucket_table_impl(jnp)


def _t5_bucket_table_impl(jnp):
    rel = jnp.arange(-255, 128, dtype=jnp.int32)
    half = 16
    max_exact = 8
    ret = jnp.where(rel > 0, half, 0)
    n = jnp.abs(rel)
    nf = jnp.maximum(n, 1).astype(jnp.float32)
    large = max_exact + (jnp.log(nf / max_exact) / math.log(128 / max_exact)
                         * (half - max_exact)).astype(jnp.int32)
    large = jnp.minimum(large, half - 1)
    return np.asarray(ret + jnp.where(n < max_exact, n, large))


def fm(vec, nchunk):
    return np.ascontiguousarray(np.asarray(vec, np.float32).reshape(nchunk, 128).T)


def prep_shared(inp):
    f = lambda a: np.ascontiguousarray(np.asarray(a, np.float32))
    bt = _t5_bucket_table()
    rb = f(inp["rel_bias"])
    ii = np.arange(128)
    biasT = np.empty((128, 2, 8, 128), np.float32)
    for blk in range(2):
        rel = (blk - 1) * 128 + ii[:, None] - ii[None, :]
        biasT[:, blk] = np.transpose(rb[bt[rel + 255]], (0, 2, 1))
    far = rb[15]
    sh = {
        "w_ada": f(inp["w_ada"][0]),
        "b_ada": fm(inp["b_ada"][0], 48),
        "g1": fm(inp["g_norm1"][0], 8),
        "g2": fm(inp["g_norm2"][0], 8),
        "w_in": f(inp["w_in"][0]),
        "qg": np.ascontiguousarray(np.tile(f(inp["q_norm_g"][0]), 2)[:, None]),
        "kg": np.ascontiguousarray(np.tile(f(inp["k_norm_g"][0]), 2)[:, None]),
        "cw": np.ascontiguousarray(np.transpose(f(inp["conv_w"][0])[:, 0, :].reshape(31, 4, 128), (2, 1, 0)).reshape(128, 124)),
        "cb": fm(inp["conv_b"][0], 4),
        "clg": fm(inp["conv_ln_g"][0], 4),
        "clb": fm(inp["conv_ln_b"][0], 4),
        "goc": fm(inp["g_out_conv"][0], 4),
        "goa": fm(inp["g_out_attn"][0], 4),
        "biasT": np.ascontiguousarray(biasT.reshape(128, -1)),
        "biasfar": np.ascontiguousarray(np.broadcast_to(far[None, :], (128, 8))),
        "w_out": f(inp["w_out"][0]),
        "w_pq": f(inp["w_peer_q"][0]),
        "k1T": np.ascontiguousarray(np.transpose(f(inp["peer_k1"][0]), (2, 0, 1)).reshape(128, 1024)),
        "k2T": np.ascontiguousarray(np.transpose(f(inp["peer_k2"][0]), (2, 0, 1)).reshape(128, 1024)),
        "uT": np.ascontiguousarray(f(inp["peer_u"][0]).T),
        "vtab": f(inp["peer_v"][0]),
    }
    return sh


def prep_core(x, c, NB):
    S = x.shape[1]
    cT = np.ascontiguousarray(np.transpose(np.asarray(c, np.float32).reshape(NB, 8, 128), (2, 1, 0)).reshape(128, 8 * NB))
    return {"x": np.ascontiguousarray(np.asarray(x, np.float32).reshape(NB * S, D)), "cT": cT}


N_CORES = 8


def kernel(**inputs):
    x = np.asarray(inputs["x"])
    c = np.asarray(inputs["c"])
    B, S, _ = x.shape
    NB = B // N_CORES
    NSEL = min(256, S // 4)
    nc = build(S, NB, NSEL)
    sh = prep_shared(inputs)
    in_maps = []
    for i in range(N_CORES):
        m = dict(sh)
        m.update(prep_core(x[i * NB:(i + 1) * NB], c[i * NB:(i + 1) * NB], NB))
        in_maps.append(m)
    res = run_bass_kernel_spmd(nc, in_maps, core_ids=list(range(N_CORES)))
    out = np.concatenate([np.asarray(r["out"]).reshape(NB, S, D) for r in res.results], axis=0)
    return out.astype(np.float32)
```

```python
import math
from contextlib import ExitStack

import numpy as np
import concourse.bass as bass
import concourse.mybir as mybir
from concourse.bass_utils import run_bass_kernel_spmd

F32 = mybir.dt.float32
BF16 = mybir.dt.bfloat16
U32 = mybir.dt.uint32
ALU = mybir.AluOpType
AF = mybir.ActivationFunctionType
AX = mybir.AxisListType

D = 1024
NCOL = 2884
EPS = 1e-6
NEXP_CH = 128
NEG = -30000.0
ACT_EVAC = False
ENGS = ("pe", "act", "dve", "pool", "sp")


class Prog:
    def __init__(self, nc, stack):
        self.nc = nc
        self.stack = stack
        self.ops = {e: [] for e in ENGS}
        self.cnt = {e: 0 for e in ENGS}
        self.sem = {e: stack.enter_context(nc.semaphore("s_" + e)) for e in ENGS}
        self.seen = {e: {} for e in ENGS}
        self.dsem = {}
        self.lastw = {}
        self.readers = {}
        self.n_instr = 0
        self.engs = {"pe": nc.tensor, "act": nc.scalar, "dve": nc.vector,
                     "pool": nc.gpsimd, "sp": nc.sync}

    def _need(self, e, tok, waits):
        if tok is None:
            return
        sem, val, name = tok
        if e == "pe" and name == "s_pe":
            return
        if self.seen[e].get(name, 0) >= val:
            return
        prev = waits.get(name)
        if prev is None or prev[1] < val:
            waits[name] = (sem, val)

    def _deps(self, e, reads, writes):
        waits = {}
        for k in reads:
            self._need(e, self.lastw.get(k), waits)
        for k in writes:
            self._need(e, self.lastw.get(k), waits)
            for t in self.readers.get(k, {}).values():
                self._need(e, t, waits)
        for name, (sem, val) in waits.items():
            self.seen[e][name] = val
        return list(waits.values())

    def _commit(self, tok, reads, writes):
        for k in writes:
            self.lastw[k] = tok
            self.readers[k] = {}
        for k in reads:
            if k in writes:
                continue
            d = self.readers.setdefault(k, {})
            old = d.get(tok[2])
            if old is None or old[1] < tok[1]:
                d[tok[2]] = tok

    def op(self, e, fn, reads=(), writes=()):
        waits = self._deps(e, reads, writes)
        self.cnt[e] += 1
        val = self.cnt[e]
        sem = self.sem[e]

        def thunk(eng, fn=fn, waits=waits, sem=sem):
            for (s, v) in waits:
                eng.wait_ge(s, v)
            fn(eng).then_inc(sem, 1)

        thunk(self.engs[e])
        self.ops[e].append(1)
        self._commit((sem, val, "s_" + e), reads, writes)
        self.n_instr += 1

    def dma(self, q, out, in_, reads=(), writes=(), semkey=None):
        if semkey is None:
            semkey = writes[0]
        if semkey not in self.dsem:
            self.dsem[semkey] = [self.stack.enter_context(
                self.nc.semaphore("d_%d" % len(self.dsem))), 0]
        waits = self._deps(q, reads, writes)
        ent = self.dsem[semkey]
        ent[1] += 16
        sem, val = ent[0], ent[1]

        def thunk(eng, waits=waits, sem=sem, out=out, in_=in_):
            for (s, v) in waits:
                eng.wait_ge(s, v)
            eng.dma_start(out=out, in_=in_).then_inc(sem, 16)

        thunk(self.engs[q])
        self.ops[q].append(1)
        self._commit((sem, val, "d_" + str(semkey)), reads, writes)
        self.n_instr += 1

    def barrier(self):
        toks = [(self.sem[e], self.cnt[e], "s_" + e) for e in ENGS if self.cnt[e] > 0]
        toks += [(ent[0], ent[1], "d_" + str(k)) for k, ent in self.dsem.items()]
        for e in ENGS:
            waits = {}
            for t in toks:
                self._need(e, t, waits)
            for name, (sem, val) in waits.items():
                self.seen[e][name] = val
                self.engs[e].wait_ge(sem, val)

    def final_wait(self, e, keys):
        waits = self._deps(e, keys, ())

        def thunk(eng, waits=waits):
            for (s, v) in waits:
                eng.wait_ge(s, v)

        thunk(self.engs[e])

    def emit(self):
        return
        nc = self.nc
        with nc.Block() as block:
            @block.tensor
            def _(eng):
                for t in self.ops["pe"]:
                    t(eng)

            @block.scalar
            def _(eng):
                for t in self.ops["act"]:
                    t(eng)

            @block.vector
            def _(eng):
                for t in self.ops["dve"]:
                    t(eng)

            @block.gpsimd
            def _(eng):
                for t in self.ops["pool"]:
                    t(eng)

            @block.sync
            def _(eng):
                for t in self.ops["sp"]:
                    t(eng)


class _Stop(Exception):
    pass


def build(S, NB, NSEL, n_iter=28, phase1_only=False, stop_at=None):
    holder = {}
    try:
        return _build(S, NB, NSEL, n_iter, phase1_only, stop_at, holder)
    except _Stop:
        P = holder["P"]
        P.barrier()
        return holder["nc"]


def _build(S, NB, NSEL, n_iter, phase1_only, stop_at, holder):
    NT = S // 128
    TOK = NB * S
    NTT = TOK // 128
    TG = 256
    NG = TOK // TG
    nc = bass.Bass("TRN2", target_bir_lowering=False)
    holder["nc"] = nc

    def mark(name):
        if stop_at == name:
            raise _Stop()

    def din(name, shape, dt=F32):
        return nc.dram_tensor(name, shape, dt, kind="ExternalInput").ap()

    x_d = din("x", [TOK, D])
    cT_d = din("cT", [128, 8 * NB])
    wada_d = din("w_ada", [D, 6 * D])
    bada_d = din("b_ada", [128, 48])
    g1_d = din("g1", [128, 8])
    g2_d = din("g2", [128, 8])
    win_d = din("w_in", [D, NCOL])
    qg_d = din("qg", [128, 1])
    kg_d = din("kg", [128, 1])
    cw_d = din("cw", [128, 4 * 31])
    cb_d = din("cb", [128, 4])
    clg_d = din("clg", [128, 4])
    clb_d = din("clb", [128, 4])
    goc_d = din("goc", [128, 4])
    goa_d = din("goa", [128, 4])
    biasT_d = din("biasT", [128, 2 * 8 * 128])
    biasfar_d = din("biasfar", [128, 8])
    wout_d = din("w_out", [D, D])
    wpq_d = din("w_pq", [D, 2048])
    k1T_d = din("k1T", [128, 8 * 128])
    k2T_d = din("k2T", [128, 8 * 128])
    uT_d = din("uT", [D, 16384])
    v_d = din("vtab", [16384, D])
    out_d = nc.dram_tensor("out", [TOK, D], F32, kind="ExternalOutput").ap()
    h2T_s = nc.dram_tensor("h2T_s", [TOK, D], BF16, kind="Internal").ap()
    uTb_s = nc.dram_tensor("uTb_s", [D, 16384], BF16, kind="Internal").ap()
    vb_s = nc.dram_tensor("vb_s", [16384, D], BF16, kind="Internal").ap()

    with ExitStack() as st:
        P = Prog(nc, st)
        holder["P"] = P

        def V(fn, r=(), w=()):
            P.op("dve", fn, r, w)

        def A(fn, r=(), w=()):
            P.op("act", fn, r, w)

        def G(fn, r=(), w=()):
            P.op("pool", fn, r, w)

        def T(fn, r=(), w=()):
            P.op("pe", fn, r, w)

        def sbuf(stack, name, shape, dt):
            return stack.enter_context(nc.sbuf_tensor("sb_" + name, shape, dt))

        banks = [st.enter_context(nc.psum_tensor("bk%d" % i, [128, 512], F32)) for i in range(8)]

        class Rot:
            def __init__(self, idxs):
                self.idxs = idxs
                self.n = 0

            def get(self):
                i = self.idxs[self.n % len(self.idxs)]
                self.n += 1
                return banks[i], "bk%d" % i

        ident_f = sbuf(st, "ident_f", [128, 128], F32)
        ident_b = sbuf(st, "ident_b", [128, 128], BF16)
        ones_f = sbuf(st, "ones_f", [128, 128], F32)
        bones_f = sbuf(st, "bones_f", [128, 128], F32)
        iota_f = sbuf(st, "iota_f", [128, 128], F32)
        iota_b = sbuf(st, "iota_b", [128, 128], BF16)
        pid_f = sbuf(st, "pid_f", [128, 1], F32)
        epsc = sbuf(st, "epsc", [128, 1], F32)
        a1 = sbuf(st, "a1", [128, NB, 8], F32)
        sh1 = sbuf(st, "sh1", [128, NB, 8], F32)
        gt1 = sbuf(st, "gt1", [128, NB, 8], F32)
        a2 = sbuf(st, "a2", [128, NB, 8], F32)
        sh2 = sbuf(st, "sh2", [128, NB, 8], F32)
        gt2 = sbuf(st, "gt2", [128, NB, 8], F32)
        g1 = sbuf(st, "g1", [128, 8], F32)
        g2 = sbuf(st, "g2", [128, 8], F32)

        G(lambda e: e.iota(iota_f[:], pattern=[[1, 128]], base=0, channel_multiplier=0,
                           allow_small_or_imprecise_dtypes=True), w=["iota"])
        G(lambda e: e.iota(pid_f[:], pattern=[[0, 1]], base=0, channel_multiplier=1,
                           allow_small_or_imprecise_dtypes=True), w=["pid"])
        V(lambda e: e.tensor_scalar(ident_f[:], iota_f[:], pid_f[:, 0:1], None, ALU.is_equal),
          ["iota", "pid"], ["ident_f"])
        V(lambda e: e.tensor_copy(ident_b[:], ident_f[:]), ["ident_f"], ["ident_b"])
        V(lambda e: e.tensor_copy(iota_b[:], iota_f[:]), ["iota"], ["iota_b"])
        V(lambda e: e.memset(ones_f[:], 1.0), w=["ones_f"])
        V(lambda e: e.memset(bones_f[:], 0.0), w=["bones_f"])
        V(lambda e: e.memset(bones_f[0:64, 0:64], 1.0), ["bones_f"], ["bones_f"])
        V(lambda e: e.memset(bones_f[64:128, 64:128], 1.0), ["bones_f"], ["bones_f"])
        V(lambda e: e.memset(epsc[:], EPS), w=["epsc"])
        P.dma("sp", g1[:], g1_d[:, :], writes=["g1"])
        P.dma("sp", g2[:], g2_d[:, :], writes=["g2"])

        def rsqrt(out, src, scale, rk, wk):
            np_ = out.shape[0]
            A(lambda e: e.activation(out, src, AF.Sqrt, bias=epsc[0:np_, 0:1], scale=scale),
              list(rk) + ["epsc"], wk)
            V(lambda e: e.reciprocal(out, out), wk, wk)

        with ExitStack() as s0:
            cT = sbuf(s0, "cT", [128, 8 * NB], F32)
            scs = sbuf(s0, "scs", [128, 8 * NB], F32)
            bada = sbuf(s0, "bada", [128, 48], F32)
            mod = sbuf(s0, "mod", [128, 48, NB], F32)
            stg = sbuf(s0, "stg", [128, 2, 8, 1024], F32)
            P.dma("sp", cT[:], cT_d[:, :], writes=["cT"])
            P.dma("sp", bada[:], bada_d[:, :], writes=["bada"])
            A(lambda e: e.activation(scs[:], cT[:], AF.Silu), ["cT"], ["scs"])
            rot = Rot(list(range(8)))
            for fp in range(6):
                sl = fp % 2
                P.dma("sp", stg[:, sl], wada_d[:, fp * 1024:(fp + 1) * 1024].rearrange(
                    "(k p) f -> p k f", p=128), writes=["stg%d" % sl])
                bk, bkk = rot.get()

                def mm(e, sl=sl, bk=bk):
                    last = None
                    for fc in range(8):
                        for k in range(8):
                            last = e.matmul(bk[:, fc * NB:(fc + 1) * NB],
                                            stg[:, sl, k, fc * 128:(fc + 1) * 128],
                                            scs[:, k * NB:(k + 1) * NB],
                                            start=(k == 0), stop=(k == 7))
                    return last
                T(mm, ["stg%d" % sl, "scs"], [bkk])
                for b in range(NB):
                    V(lambda e, b=b, bk=bk, fp=fp: e.tensor_tensor(
                        mod[:, fp * 8:(fp + 1) * 8, b],
                        bk[:, 0:8 * NB].rearrange("p (f b) -> p f b", b=NB)[:, :, b],
                        bada[:, fp * 8:(fp + 1) * 8], ALU.add), [bkk, "bada"], ["mod"])
            for b in range(NB):
                V(lambda e, b=b: e.scalar_tensor_tensor(a1[:, b, :], mod[:, 8:16, b], 1.0, g1[:],
                                                        ALU.add, ALU.mult), ["mod", "g1"], ["a1"])
                V(lambda e, b=b: e.scalar_tensor_tensor(a2[:, b, :], mod[:, 32:40, b], 1.0, g2[:],
                                                        ALU.add, ALU.mult), ["mod", "g2"], ["a2"])
                V(lambda e, b=b: e.tensor_copy(sh1[:, b, :], mod[:, 0:8, b]), ["mod"], ["sh1"])
                V(lambda e, b=b: e.tensor_copy(gt1[:, b, :], mod[:, 16:24, b]), ["mod"], ["gt1"])
                V(lambda e, b=b: e.tensor_copy(sh2[:, b, :], mod[:, 24:32, b]), ["mod"], ["sh2"])
                V(lambda e, b=b: e.tensor_copy(gt2[:, b, :], mod[:, 40:48, b]), ["mod"], ["gt2"])

        mark("adaln")
        P.barrier()

        def gate_bcast(dst, gt, b, gkey, dkey, rot):
            for half in range(2):
                bk, bkk = rot.get()
                for kk in range(4):
                    k = half * 4 + kk
                    V(lambda e, k=k: e.tensor_scalar(rep[:], ones_f[:], gt[:, b, k:k + 1], None,
                                                     ALU.mult), ["ones_f", gkey], ["rep"])
                    T(lambda e, kk=kk, bk=bk: e.matmul(bk[:, kk * 128:(kk + 1) * 128], rep[:],
                                                       ident_f[:], start=True, stop=True),
                      ["rep", "ident_f"], [bkk])
                V(lambda e, half=half, bk=bk: e.tensor_copy(dst[:, half * 512:(half + 1) * 512],
                                                            bk[:]), [bkk], [dkey])

        rep = sbuf(st, "rep", [128, 128], F32)

        with ExitStack() as s1:
            w_in = sbuf(s1, "w_in", [128, 8, NCOL], BF16)
            w_out = sbuf(s1, "w_out", [128, 8, D], BF16)
            kT = sbuf(s1, "kT", [128, 4, S], BF16)
            vsb = sbuf(s1, "vsb", [128, NT, 8, 65], BF16)
            kiT = sbuf(s1, "kiT", [64, S], BF16)
            idx = sbuf(s1, "idx", [128, max(S, NCOL)], F32)
            mbs = sbuf(s1, "mbs", [128, 2, S], BF16)
            xts = sbuf(s1, "xts", [128, 2, D], F32)
            Dg = sbuf(s1, "Dg", [128, 2, 128], F32)
            gt1_bc = xts[:, 1]
            hT = sbuf(s1, "hT", [128, 8, 128], BF16)
            h2T = hT
            mixTs = sbuf(s1, "mixTs", [128, 2, 8, 128], BF16)
            ubuf = sbuf(s1, "ubuf", [128, 4, 158], F32)
            yc = sbuf(s1, "yc", [128, 4, 128], F32)
            t2 = yc
            mean = sbuf(s1, "mean", [128, 128], F32)
            var = sbuf(s1, "var", [128, 128], F32)
            r3 = mean
            sq = sbuf(s1, "sq", [128, 512], F32)
            ysq = sq[:].rearrange("p (c t) -> p c t", c=4)
            rl = sq[:].rearrange("p (o t) -> p o t", o=1)
            zc = yc
            rq = sbuf(s1, "rq", [128, 512], F32)
            print("phase1 sbuf bytes free:", nc.sbuf_bytes_remaining)
            qn = sq
            qpads = sbuf(s1, "qpads", [128, 2, 8, 128], BF16)
            qiT = sbuf(s1, "qiT", [64, 4, 128], BF16)
            Eb = sbuf(s1, "Eb", [128, 2, 512], BF16)
            attn = sbuf(s1, "attn", [128, 512], F32)
            biasTb = sbuf(s1, "biasTb", [128, 2, 8, 128], BF16)
            biasfar = sbuf(s1, "biasfar", [128, 8], F32)
            sm = sbuf(s1, "sm", [128, 64], F32)
            wi = sbuf(s1, "wi", [128, 12], F32)
            bis = sbuf(s1, "bis", [128, 8], F32)
            qg = sbuf(s1, "qg", [128, 1], F32)
            kg = sbuf(s1, "kg", [128, 1], F32)
            cw = sbuf(s1, "cw", [128, 4, 31], F32)
            cb = sbuf(s1, "cb", [128, 4], F32)
            clg = sbuf(s1, "clg", [128, 4], F32)
            clb = sbuf(s1, "clb", [128, 4], F32)
            goc = sbuf(s1, "goc", [128, 4], F32)
            goa = sbuf(s1, "goa", [128, 4], F32)

            for nm, t, dsrc in (("qg", qg, qg_d), ("kg", kg, kg_d), ("cb", cb, cb_d),
                                ("clg", clg, clg_d), ("clb", clb, clb_d), ("goc", goc, goc_d),
                                ("goa", goa, goa_d), ("biasfar", biasfar, biasfar_d)):
                P.dma("sp", t[:], dsrc[:, :], writes=[nm])
            P.dma("sp", cw[:], cw_d[:, :].rearrange("p (c j) -> p c j", j=31), writes=["cw"])
            V(lambda e: e.tensor_scalar(qg[:], qg[:], 0.125, None, ALU.mult), ["qg"], ["qg"])
            for blk in range(2):
                P.dma("sp", idx[:, 0:1024], biasT_d[:, blk * 1024:(blk + 1) * 1024], writes=["idx"])
                for h in range(8):
                    V(lambda e, h=h, blk=blk: e.tensor_scalar(
                        biasTb[:, blk, h, :], idx[:, h * 128:(h + 1) * 128],
                        biasfar[:, h:h + 1], None, ALU.subtract), ["idx", "biasfar"], ["biasTb"])
            V(lambda e: e.memset(qpads[:], 0.0), w=["qpad0", "qpad1"])
            V(lambda e: e.memset(vsb[:], 1.0), w=["vsb%d" % j for j in range(NT)])

            for k in range(8):
                P.dma("sp", idx[:, 0:NCOL], win_d[k * 128:(k + 1) * 128, :], writes=["idx"])
                if k % 2 == 0:
                    G(lambda e, k=k: e.tensor_copy(w_in[:, k, :], idx[:, 0:NCOL]), ["idx"], ["w_in"])
                else:
                    A(lambda e, k=k: e.copy(w_in[:, k, :], idx[:, 0:NCOL]), ["idx"], ["w_in"])
            mark("setup1")
            P.barrier()
            rot = Rot(list(range(6)))
            tb_eps = -(2.0 ** -20)

            def norm_to_T(src, ssq_col, r_col, ak, shk, b, dstT, dkey, skey, dsl):
                dks = ["%s%d" % (dkey, k) for k in range(8)]
                A(lambda e: e.activation(dstT[:].rearrange("p k t -> p (k t)"), src, AF.Square,
                                         accum_out=sm[:, ssq_col:ssq_col + 1]),
                  [skey], dks + ["sm%d" % ssq_col])
                rsqrt(sm[:, r_col:r_col + 1], sm[:, ssq_col:ssq_col + 1], 1.0 / D,
                      ["sm%d" % ssq_col], ["sm%d" % r_col])
                V(lambda e: e.tensor_scalar(Dg[:, dsl, :], ident_f[:], sm[:, r_col:r_col + 1], None, ALU.mult),
                  ["ident_f", "sm%d" % r_col], ["Dg%d" % dsl])
                for half in range(2):
                    bk, bkk = rot.get()

                    def tr(e, half=half, bk=bk):
                        last = None
                        for kk in range(4):
                            k = half * 4 + kk
                            last = e.matmul(bk[:, kk * 128:(kk + 1) * 128], src[:, k * 128:(k + 1) * 128],
                                            Dg[:, dsl, :], start=True, stop=True)
                        return last
                    T(tr, [skey, "Dg%d" % dsl], [bkk])
                    for kk in range(4):
                        k = half * 4 + kk
                        V(lambda e, k=k, kk=kk, bk=bk: e.tensor_scalar(
                            dstT[:, k, :], bk[:, kk * 128:(kk + 1) * 128],
                            ak[:, b, k:k + 1], shk[:, b, k:k + 1], ALU.mult, ALU.add),
                          [bkk, "a1", "a2", "sh1", "sh2"], ["%s%d" % (dkey, k)])

            hTk = ["hT%d" % k for k in range(8)]
            h2Tk = ["hT%d" % k for k in range(8)]
            mixk = ["mix%d" % k for k in range(8)]

            for b in range(NB):
                gate_bcast(gt1_bc, gt1, b, "gt1", "xt1", rot)
                for k in range(8):
                    P.dma("sp", idx[:, 0:D], wout_d[k * 128:(k + 1) * 128, :], writes=["idx"])
                    (V if k % 2 == 0 else G)(lambda e, k=k: e.tensor_tensor(
                        w_out[:, k, :], idx[:, 0:D], gt1_bc, ALU.mult), ["idx", "xt1"], ["w_out"])
                mark("wout%d" % b)
                def front(b, i):
                    t0 = i * 128
                    r0 = b * S + t0
                    NK = t0 + 128
                    NKB = i + 1
                    ps_ = i % 2
                    vk = "vsb%d" % i
                    kTk = "kT%d" % i
                    kik = "kiT%d" % i
                    xt = xts[:, ps_]
                    xtk = "xt%d" % ps_
                    mb = mbs[:, ps_]
                    mbk = "mb%d" % ps_
                    qpad = qpads[:, ps_]
                    qpk = "qpad%d" % ps_
                    mixT = mixTs[:, ps_]
                    mixk = ["mix%d_%d" % (ps_, k) for k in range(8)]
                    P.dma("sp", xt, x_d[r0:r0 + 128, :], writes=[xtk])
                    norm_to_T(xt, 0, 1, a1, sh1, b, hT, "hT", xtk, 0)
                    def proj_fm(bk, col0, ncols, ncolblk, M):
                        def f(e):
                            last = None
                            for j in range(ncolblk):
                                for k in range(8):
                                    last = e.matmul(bk[0:M, j * 128:(j + 1) * 128],
                                                    w_in[:, k, col0 + j * M:col0 + (j + 1) * M],
                                                    hT[:, k, :], start=(k == 0), stop=(k == 7))
                            return last
                        return f

                    def qknorm(bsrc, bsrck, gain, gk, is_q):
                        A(lambda e: e.activation(sq[:], bsrc[:], AF.Square), [bsrck], ["sq"])
                        bn, bnk = rot.get()
                        T(lambda e: e.matmul(bn[:], bones_f[:], sq[:], start=True, stop=True),
                          ["sq", "bones_f"], [bnk])
                        rsqrt(rq[:], bn[:], 1.0 / 64, [bnk], ["rq"])
                        V(lambda e: e.scalar_tensor_tensor(qn[:], bsrc[:], gain[:, 0:1], rq[:],
                                                           ALU.mult, ALU.mult), [bsrck, gk, "rq", "sq"], ["sq"])
                        if is_q:
                            for eh in range(2):
                                ps = slice(eh * 64, (eh + 1) * 64)
                                G(lambda e, ps=ps, eh=eh: e.tensor_copy(
                                    qpad[ps].rearrange("p (c two) t -> p c two t", two=2)[:, :, eh, :],
                                    qn[ps, :].rearrange("p (c t) -> p c t", c=4)), ["sq"], [qpk])
                        else:
                            G(lambda e: e.tensor_copy(kT[:, :, t0:t0 + 128],
                                                      qn[:].rearrange("p (c t) -> p c t", c=4)),
                              ["sq"], [kTk])
                    bcv, bcvk = rot.get()
                    T(proj_fm(bcv, 0, 512, 4, 128), hTk + ["w_in"], [bcvk])
                    bcg, bcgk = rot.get()
                    T(proj_fm(bcg, 512, 512, 4, 128), hTk + ["w_in"], [bcgk])
                    A(lambda e, bcg=bcg: e.activation(ubuf[:, :, 30:158], bcg[:].rearrange("p (c t) -> p c t", c=4),
                                                      AF.Sigmoid), [bcgk], ["ubuf_c"])
                    if i == 0:
                        G(lambda e: e.memset(ubuf[:, :, 0:30], 0.0), w=["ubuf_h"])
                    V(lambda e, bcv=bcv: e.tensor_tensor(ubuf[:, :, 30:158],
                                                         bcv[:].rearrange("p (c t) -> p c t", c=4),
                                                         ubuf[:, :, 30:158], ALU.mult), [bcvk, "ubuf_c"], ["ubuf_c"])
                    bq, bqk = rot.get()
                    T(proj_fm(bq, 1024, 512, 4, 128), hTk + ["w_in"], [bqk])
                    qknorm(bq, bqk, qg, "qg", True)
                    bkk_, bkkk = rot.get()
                    T(proj_fm(bkk_, 1536, 512, 4, 128), hTk + ["w_in"], [bkkk])
                    qknorm(bkk_, bkkk, kg, "kg", False)
                    bv, bvk = rot.get()

                    def projv(e, bv=bv):
                        last = None
                        for k in range(8):
                            last = e.matmul(bv[:, 0:512], hT[:, k, :], w_in[:, k, 2048:2560],
                                            start=(k == 0), stop=(k == 7))
                        return last
                    T(projv, hTk + ["w_in"], [bvk])
                    A(lambda e, bv=bv: e.copy(vsb[:, i, :, 0:64], bv[:].rearrange("p (h d) -> p h d", h=8)),
                      [bvk], [vk])
                    bi, bik = rot.get()

                    bi2, bi2k = rot.get()

                    def proji2(e, bi=bi, bi2=bi2):
                        last = None
                        for j in range(4):
                            for k in range(8):
                                last = e.matmul(bi[0:64, j * 128:(j + 1) * 128],
                                                w_in[:, k, 2560 + j * 64:2560 + (j + 1) * 64],
                                                hT[:, k, :], start=(k == 0), stop=(k == 7))
                        for k in range(8):
                            last = e.matmul(bi2[0:64, 0:128], w_in[:, k, 2816:2880], hT[:, k, :],
                                            start=(k == 0), stop=(k == 7))
                        for k in range(8):
                            last = e.matmul(bi2[:, 128:132], hT[:, k, :], w_in[:, k, 2880:2884],
                                            start=(k == 0), stop=(k == 7))
                        return last
                    T(proji2, hTk + ["w_in"], [bik, bi2k])

                    V(lambda e, bi=bi: e.tensor_copy(qiT[:], bi[0:64, :].rearrange("p (h t) -> p h t", h=4)),
                      [bik], ["qiT"])
                    V(lambda e, bi2=bi2: e.tensor_copy(kiT[:, t0:t0 + 128], bi2[0:64, 0:128]), [bi2k], [kik])
                    V(lambda e, bi2=bi2: e.tensor_copy(wi[:, 0:4], bi2[:, 128:132]), [bi2k], ["wi"])
                    kik_all = ["kiT%d" % j for j in range(NKB)]
                    G(lambda e: e.iota(idx[:, 0:NK], pattern=[[1, NK]], base=0, channel_multiplier=0,
                                       allow_small_or_imprecise_dtypes=True), w=["idx"])
                    A(lambda e: e.mul(idx[:, 0:NK], idx[:, 0:NK], tb_eps), ["idx"], ["idx"])
                    nrl = 0
                    for c0 in range(0, NK, 512):
                        cn = min(512, NK - c0)
                        for h in range(4):
                            bx, bxk = rot.get()
                            T(lambda e, bx=bx, h=h, c0=c0, cn=cn: e.matmul(
                                bx[:, 0:cn], qiT[:, h, :], kiT[:, c0:c0 + cn], start=True, stop=True),
                              ["qiT"] + kik_all, [bxk])
                            sl = 0
                            A(lambda e, bx=bx, h=h, cn=cn, sl=sl: e.activation(
                                rl[:, sl, 0:cn], bx[:, 0:cn], AF.Relu),
                              [bxk], ["sq"])
                            V(lambda e, h=h, c0=c0, cn=cn, sl=sl: e.scalar_tensor_tensor(
                                idx[:, c0:c0 + cn], rl[:, sl, 0:cn], wi[:, h:h + 1], idx[:, c0:c0 + cn],
                                ALU.mult, ALU.add), ["sq", "wi", "idx"], ["idx"])
                    mark("index%d" % i)
                    need_search = NK > NSEL
                    if need_search:
                        V(lambda e: e.tensor_reduce(bis[:, 0:1], idx[:, 0:NK], AX.X, ALU.max), ["idx"], ["bis"])
                        V(lambda e: e.tensor_reduce(bis[:, 1:2], idx[:, 0:NK], AX.X, ALU.min), ["idx"], ["bis"])
                        V(lambda e: e.tensor_tensor(bis[:, 2:3], bis[:, 0:1], bis[:, 1:2], ALU.subtract),
                          ["bis"], ["bis"])
                    else:
                        V(lambda e: e.memset(bis[:, 1:2], -1e29), w=["bis"])
                    V(lambda e: e.memset(idx[0:64, t0 + 64:t0 + 128], -1e30), ["idx"], ["idx"])
                    if need_search:
                        for it in range(1, n_iter + 1):
                            sc_ = 2.0 ** (-it)
                            V(lambda e, sc_=sc_: e.scalar_tensor_tensor(bis[:, 3:4], bis[:, 2:3], sc_, bis[:, 1:2],
                                                                        ALU.mult, ALU.add), ["bis"], ["bis"])
                            V(lambda e: e.tensor_scalar(mb[:, 0:NK], idx[:, 0:NK], bis[:, 3:4], None,
                                                        ALU.is_ge, ALU.add, accum_out=bis[:, 4:5]),
                              ["idx", "bis"], [mbk, "bis"])
                            V(lambda e, sc_=sc_: e.tensor_scalar(bis[:, 5:6], bis[:, 4:5], NSEL - 0.5, sc_,
                                                                 ALU.is_ge, ALU.mult), ["bis"], ["bis"])
                            V(lambda e: e.scalar_tensor_tensor(bis[:, 1:2], bis[:, 5:6], bis[:, 2:3], bis[:, 1:2],
                                                               ALU.mult, ALU.add), ["bis"], ["bis"])
                    V(lambda e: e.tensor_scalar(mb[:, 0:NK], idx[:, 0:NK], bis[:, 1:2], NEG,
                                                ALU.is_lt, ALU.mult), ["idx", "bis"], [mbk])

                    mark("bisect%d" % i)
                def convblk(b, i):
                    t0 = i * 128
                    r0 = b * S + t0
                    NK = t0 + 128
                    NKB = i + 1
                    ps_ = i % 2
                    vk = "vsb%d" % i
                    kTk = "kT%d" % i
                    kik = "kiT%d" % i
                    xt = xts[:, ps_]
                    xtk = "xt%d" % ps_
                    mb = mbs[:, ps_]
                    mbk = "mb%d" % ps_
                    qpad = qpads[:, ps_]
                    qpk = "qpad%d" % ps_
                    mixT = mixTs[:, ps_]
                    mixk = ["mix%d_%d" % (ps_, k) for k in range(8)]
                    for j in range(31):
                        for c in range(4):
                            if j == 0:
                                V(lambda e, c=c: e.tensor_scalar(yc[:, c, :], ubuf[:, c, 0:128],
                                                                 cw[:, c, 0:1], cb[:, c:c + 1],
                                                                 ALU.mult, ALU.add),
                                  ["ubuf_h", "ubuf_c", "cw", "cb"], ["yc%d" % c])
                            else:
                                V(lambda e, c=c, j=j: e.scalar_tensor_tensor(
                                    yc[:, c, :], ubuf[:, c, j:j + 128], cw[:, c, j:j + 1], yc[:, c, :],
                                    ALU.mult, ALU.add),
                                  ["ubuf_h", "ubuf_c", "cw"], ["yc%d" % c])
                    yck = ["yc%d" % c for c in range(4)]
                    ysqk = ["sq"]
                    if i + 1 < NT:
                        G(lambda e: e.tensor_copy(ubuf[:, :, 0:30], ubuf[:, :, 128:158]),
                          ["ubuf_c"], ["ubuf_h"])
                    G(lambda e: e.tensor_tensor(ysq[:], yc[:], yc[:], ALU.mult), yck, ysqk)
                    bs, bsk = rot.get()

                    def lnstat(e, bs=bs):
                        last = None
                        for c in range(4):
                            last = e.matmul(bs[:, 0:128], ones_f[:], yc[:, c, :], start=(c == 0), stop=(c == 3))
                        for c in range(4):
                            last = e.matmul(bs[:, 128:256], ones_f[:], ysq[:, c, :], start=(c == 0), stop=(c == 3))
                        return last
                    T(lnstat, yck + ysqk + ["ones_f"], [bsk])
                    V(lambda e, bs=bs: e.tensor_scalar(mean[:], bs[:, 0:128], 1.0 / 512, None, ALU.mult),
                      [bsk], ["mean"])
                    V(lambda e: e.tensor_tensor(var[:], mean[:], mean[:], ALU.mult), ["mean"], ["var"])
                    V(lambda e, bs=bs: e.scalar_tensor_tensor(var[:], bs[:, 128:256], 1.0 / 512, var[:],
                                                              ALU.mult, ALU.subtract), [bsk, "var"], ["var"])
                    rsqrt(var[:], var[:], 1.0, ["var"], ["var"])
                    V(lambda e: e.tensor_tensor(t2[:], yc[:], mean[:].unsqueeze(1).to_broadcast([128, 4, 128]),
                                                ALU.subtract), yck + ["mean"], yck)
                    V(lambda e: e.tensor_tensor(t2[:], t2[:], var[:].unsqueeze(1).to_broadcast([128, 4, 128]),
                                                ALU.mult), yck + ["var"], yck)
                    for c in range(4):
                        V(lambda e, c=c: e.tensor_scalar(t2[:, c, :], t2[:, c, :], clg[:, c:c + 1], clb[:, c:c + 1],
                                                         ALU.mult, ALU.add), ["yc%d" % c, "clg", "clb"], ["yc%d" % c])
                    A(lambda e: e.activation(zc[:], t2[:], AF.Silu), yck, yck)
                    zck = yck
                    G(lambda e: e.tensor_tensor(ysq[:], zc[:], zc[:], ALU.mult), zck, ysqk)
                    bs2, bs2k = rot.get()

                    def rmstat(e, bs2=bs2):
                        last = None
                        for c in range(4):
                            last = e.matmul(bs2[:, 0:128], ones_f[:], ysq[:, c, :], start=(c == 0), stop=(c == 3))
                        return last
                    T(rmstat, ysqk + ["ones_f"], [bs2k])
                    rsqrt(r3[:], bs2[:, 0:128], 1.0 / 512, [bs2k, "mean"], ["mean"])
                    for c in range(4):
                        V(lambda e, c=c: e.scalar_tensor_tensor(mixT[:, c, :], zc[:, c, :], goc[:, c:c + 1],
                                                                r3[:], ALU.mult, ALU.mult),
                          ["yc%d" % c, "goc", "mean"], [mixk[c]])


                def attloop(b, i):
                    t0 = i * 128
                    r0 = b * S + t0
                    NK = t0 + 128
                    NKB = i + 1
                    ps_ = i % 2
                    vk = "vsb%d" % i
                    kTk = "kT%d" % i
                    kik = "kiT%d" % i
                    xt = xts[:, ps_]
                    xtk = "xt%d" % ps_
                    mb = mbs[:, ps_]
                    mbk = "mb%d" % ps_
                    qpad = qpads[:, ps_]
                    qpk = "qpad%d" % ps_
                    mixT = mixTs[:, ps_]
                    mixk = ["mix%d_%d" % (ps_, k) for k in range(8)]
                    pvA, pvAk = banks[6], "bk6"
                    pvB, pvBk = banks[7], "bk7"
                    kT_all = ["kT%d" % j for j in range(NKB)]
                    v_all = ["vsb%d" % j for j in range(NKB)]
                    items = [(h, g0) for h in range(8) for g0 in range(0, NKB, 4)]

                    def emit_lg(n):
                        h, g0 = items[n]
                        c = h // 2
                        blks = list(range(g0, min(g0 + 4, NKB)))
                        bl, blk_ = rot.get()

                        def lg(e, bl=bl, blks=blks, h=h, c=c):
                            last = None
                            for n_, kb in enumerate(blks):
                                o = bl[:, n_ * 128:(n_ + 1) * 128]
                                e.matmul(o, kT[:, c, kb * 128:(kb + 1) * 128], qpad[:, h, :],
                                         start=True, stop=False)
                                if kb >= i - 1:
                                    e.matmul(o, ident_b[:], biasTb[:, kb - (i - 1), h, :],
                                             start=False, stop=False)
                                last = e.matmul(o, mb[:, kb * 128:(kb + 1) * 128], ident_b[:],
                                                start=False, stop=True)
                            return last
                        T(lg, kT_all + [qpk, mbk, "ident_b", "biasTb"], [blk_])
                        return bl, blk_, blks

                    nxt = emit_lg(0)
                    for n in range(len(items)):
                        bl, blk_, blks = nxt
                        if n + 1 < len(items):
                            nxt = emit_lg(n + 1)
                        h, g0 = items[n]
                        pv, pvk = (pvA, pvAk) if h < 4 else (pvB, pvBk)
                        hh = h % 4
                        nb_ = len(blks)
                        sl = n % 2
                        A(lambda e, bl=bl, nb_=nb_, sl=sl, h=h: e.activation(
                            Eb[:, sl, 0:nb_ * 128], bl[:, 0:nb_ * 128], AF.Exp,
                            bias=biasfar[:, h:h + 1], scale=1.0), [blk_, "biasfar"], ["Eb%d" % sl])

                        def pvm(e, pv=pv, blks=blks, sl=sl, h=h, hh=hh):
                            last = None
                            for n_, kb in enumerate(blks):
                                last = e.matmul(pv[:, hh * 65:(hh + 1) * 65],
                                                Eb[:, sl, n_ * 128:(n_ + 1) * 128], vsb[:, kb, h, :],
                                                start=(kb == 0), stop=(kb == NKB - 1))
                            return last
                        T(pvm, ["Eb%d" % sl] + v_all, [pvk])
                    return pvA, pvAk, pvB, pvBk

                def backrest(b, i, pvA, pvAk, pvB, pvBk):
                    t0 = i * 128
                    r0 = b * S + t0
                    NK = t0 + 128
                    NKB = i + 1
                    ps_ = i % 2
                    vk = "vsb%d" % i
                    kTk = "kT%d" % i
                    kik = "kiT%d" % i
                    xt = xts[:, ps_]
                    xtk = "xt%d" % ps_
                    mb = mbs[:, ps_]
                    mbk = "mb%d" % ps_
                    qpad = qpads[:, ps_]
                    qpk = "qpad%d" % ps_
                    mixT = mixTs[:, ps_]
                    mixk = ["mix%d_%d" % (ps_, k) for k in range(8)]
                    for hf, (pv, pvk) in enumerate(((pvA, pvAk), (pvB, pvBk))):
                        pv3 = pv[:, 0:260].rearrange("p (h d) -> p h d", d=65)
                        V(lambda e, pv3=pv3, hf=hf: e.reciprocal(sm[:, 8 + hf * 4:12 + hf * 4], pv3[:, :, 64]),
                          [pvk], ["smr%d" % hf])
                        V(lambda e, pv3=pv3, hf=hf: e.tensor_tensor(
                            attn[:, hf * 256:(hf + 1) * 256].rearrange("p (h d) -> p h d", d=64),
                            pv3[:, :, 0:64],
                            sm[:, 8 + hf * 4:12 + hf * 4].unsqueeze(2).to_broadcast([128, 4, 64]),
                            ALU.mult), [pvk, "smr%d" % hf], ["attn%d" % hf])
                    A(lambda e: e.activation(mixT[:, 4:8, :].rearrange("p c t -> p (c t)"), attn[:], AF.Square,
                                             accum_out=sm[:, 2:3]),
                      ["attn0", "attn1"], mixk[4:8] + ["sm2"])
                    rsqrt(sm[:, 3:4], sm[:, 2:3], 1.0 / 512, ["sm2"], ["sm3"])
                    V(lambda e: e.tensor_scalar(Dg[:, 1, :], ident_f[:], sm[:, 3:4], None, ALU.mult),
                      ["ident_f", "sm3"], ["Dg1"])
                    bt, btk = rot.get()

                    def atr(e, bt=bt):
                        last = None
                        for c in range(4):
                            last = e.matmul(bt[:, c * 128:(c + 1) * 128], attn[:, c * 128:(c + 1) * 128],
                                            Dg[:, 1, :], start=True, stop=True)
                        return last
                    T(atr, ["attn0", "attn1", "Dg1"], [btk])
                    for c in range(4):
                        V(lambda e, c=c, bt=bt: e.tensor_scalar(mixT[:, 4 + c, :], bt[:, c * 128:(c + 1) * 128],
                                                                goa[:, c:c + 1], None, ALU.mult),
                          [btk, "goa"], [mixk[4 + c]])

                    for half in range(2):
                        bo, bok = rot.get()

                        def op_(e, bo=bo, half=half):
                            last = None
                            for k in range(8):
                                last = e.matmul(bo[:], mixT[:, k, :], w_out[:, k, half * 512:(half + 1) * 512],
                                                start=(k == 0), stop=(k == 7))
                            return last
                        T(op_, mixk + ["w_out"], [bok])
                        hs = slice(half * 512, (half + 1) * 512)
                        V(lambda e, bo=bo, hs=hs: e.tensor_tensor(xt[:, hs], bo[:], xt[:, hs], ALU.add),
                          [bok, xtk], [xtk])
                    P.dma("sp", out_d[r0:r0 + 128, :], xt, reads=[xtk],
                          writes=["out_g%d" % (r0 // TG)], semkey="st_x1")
                    norm_to_T(xt, 4, 5, a2, sh2, b, h2T, "hT", xtk, 1)
                    P.dma("sp", h2T_s[r0:r0 + 128, :], h2T[:].rearrange("p k t -> p (k t)"), reads=h2Tk,
                          writes=["h2s%d" % (r0 // 128)], semkey="st_h2")


                front(b, 0)
                convblk(b, 0)
                for i in range(NT):
                    if i + 1 < NT:
                        front(b, i + 1)
                    pvs = attloop(b, i)
                    if i + 1 < NT:
                        convblk(b, i + 1)
                    backrest(b, i, *pvs)

        if phase1_only:
            P.final_wait("sp", ["out_g%d" % g for g in range(NG)])
            return nc
        P.barrier()
        with ExitStack() as s15:
            cin = sbuf(s15, "cin", [128, 2, 8192], F32)
            cout = sbuf(s15, "cout", [128, 2, 8192], BF16)
            srcs = ((uT_d.rearrange("(p a) e -> p (a e)", p=128), uTb_s.rearrange("(p a) e -> p (a e)", p=128), "uTb_s"),
                    (v_d.rearrange("(p a) e -> p (a e)", p=128), vb_s.rearrange("(p a) e -> p (a e)", p=128), "vb_s"))
            n = 0
            for (src, dst, key) in srcs:
                for pc in range(16):
                    sl = n % 2
                    n += 1
                    cs = slice(pc * 8192, (pc + 1) * 8192)
                    P.dma("sp", cin[:, sl, :], src[:, cs], writes=["cin%d" % sl])
                    V(lambda e, sl=sl: e.tensor_copy(cout[:, sl, 0:3072], cin[:, sl, 0:3072]),
                      ["cin%d" % sl], ["couta%d" % sl])
                    A(lambda e, sl=sl: e.copy(cout[:, sl, 3072:6144], cin[:, sl, 3072:6144]),
                      ["cin%d" % sl], ["coutb%d" % sl])
                    G(lambda e, sl=sl: e.tensor_copy(cout[:, sl, 6144:8192], cin[:, sl, 6144:8192]),
                      ["cin%d" % sl], ["coutc%d" % sl])
                    P.dma("sp", dst[:, cs], cout[:, sl, :],
                          reads=["couta%d" % sl, "coutb%d" % sl, "coutc%d" % sl], writes=[key],
                          semkey="st_c%d" % sl)

        P.barrier()
        with ExitStack() as s2:
            w_pq = sbuf(s2, "w_pq", [128, 8, 2048], BF16)
            with ExitStack() as s2a:
                stg = sbuf(s2a, "stgq", [128, 2, 2048], F32)
                for k in range(8):
                    sl = k % 2
                    P.dma("sp", stg[:, sl, :], wpq_d[k * 128:(k + 1) * 128, :], writes=["stgq%d" % sl])
                    if k % 2 == 0:
                        G(lambda e, k=k, sl=sl: e.tensor_copy(w_pq[:, k, :], stg[:, sl, :]),
                          ["stgq%d" % sl], ["w_pq"])
                    else:
                        A(lambda e, k=k, sl=sl: e.copy(w_pq[:, k, :], stg[:, sl, :]),
                          ["stgq%d" % sl], ["w_pq"])
            P.barrier()
            k1T = sbuf(s2, "k1T", [128, 8, 128], F32)
            k2T = sbuf(s2, "k2T", [128, 8, 128], F32)
            gt2_bc = sbuf(s2, "gt2_bc", [128, NB, D], F32)
            h2 = sbuf(s2, "h2", [128, 2, 8, TG], BF16)
            qqT = sbuf(s2, "qqT", [128, 16, 128], F32)
            sc = sbuf(s2, "sc", [128, 16, 128], F32)
            mx = sbuf(s2, "mx", [128, 16, 16], F32)
            ix = sbuf(s2, "ix", [128, 16, 16], U32)
            ixf = sbuf(s2, "ixf", [128, 16, 16], F32)
            cand2 = sbuf(s2, "cand2", [128, 8, 256], F32)
            best = sbuf(s2, "best", [128, 8, 16], F32)
            pos = sbuf(s2, "pos", [128, 8, 16], U32)
            pab_u = sbuf(s2, "pab_u", [128, 2, 8, 16], U32)
            pa = sbuf(s2, "pa", [128, 8, 16], F32)
            pb = sbuf(s2, "pb", [128, 8, 16], F32)
            IJg = sbuf(s2, "IJg", [128, 3, 128], F32)
            ssum = sbuf(s2, "ssum", [128, 16], F32)
            IJgT = sbuf(s2, "IJgT", [128, 2, 3, TG], BF16)
            OHI = sbuf(s2, "OHI", [128, 2, 8, 128], BF16)
            OHJ = sbuf(s2, "OHJ", [128, 2, 8, 128], BF16)
            OHJg = sbuf(s2, "OHJg", [128, 2, 8, 128], BF16)
            W = sbuf(s2, "W", [128, TG, 128], BF16)
            ub = sbuf(s2, "ub", [128, 2, 8, 512], BF16)
            vbuf = sbuf(s2, "vbuf", [128, 2, 4, D], BF16)
            gel = sbuf(s2, "gel", [128, 2, TG], BF16)
            Z = sbuf(s2, "Z", [128, 3, TG], BF16)
            iota16 = iota_f[:, 0:16]
            sc2 = qqT
            cand = sc[:].rearrange("p m n -> p (m n)").rearrange("p (h q) -> p h q", h=8)
            oh = cand2[:].rearrange("p h (a b) -> p h a b", b=16)
            res = qqT[:].rearrange("p m n -> p (m n)")[:, 0:D]
            xr = cand2[:].rearrange("p h q -> p (h q)")[:, 0:D]

            P.dma("sp", k1T[:], k1T_d[:, :].rearrange("p (h n) -> p h n", h=8), writes=["k1T"])
            P.dma("sp", k2T[:], k2T_d[:, :].rearrange("p (h n) -> p h n", h=8), writes=["k2T"])
            rotw = Rot([4, 5, 6, 7])
            for b in range(NB):
                class _D:
                    pass
                gate_bcast(gt2_bc[:, b, :], gt2, b, "gt2", "gt2_bc", rotw)

            def gating_gen(g):
                gb = g % 2
                r0 = g * TG
                for sub in range(2):
                    rr = r0 + sub * 128
                    P.dma("sp", h2[:, gb, :, sub * 128:(sub + 1) * 128],
                          h2T_s[rr:rr + 128, :].rearrange("p (k t) -> p k t", k=8),
                          reads=["h2s%d" % (rr // 128)], writes=["h2_%d_%d" % (gb, sub)])
                    yield
                for sub in range(2):
                    ts_ = slice(sub * 128, (sub + 1) * 128)
                    for m4 in range(4):
                        bk, bkk = rotw.get()

                        def qm(e, bk=bk, m4=m4, ts_=ts_):
                            last = None
                            for mm_ in range(4):
                                m = m4 * 4 + mm_
                                for k in range(8):
                                    last = e.matmul(bk[:, mm_ * 128:(mm_ + 1) * 128],
                                                    w_pq[:, k, m * 128:(m + 1) * 128], h2[:, gb, k, ts_],
                                                    start=(k == 0), stop=(k == 7))
                            return last
                        T(qm, ["w_pq", "h2_%d_%d" % (gb, sub)], [bkk])
                        yield
                        A(lambda e, bk=bk, m4=m4: e.copy(
                            qqT[:, m4 * 4:(m4 + 1) * 4, :].rearrange("p m t -> p (m t)"), bk[:]),
                          [bkk], ["A"])
                        yield
                    for m4 in range(4):
                        bk, bkk = rotw.get()

                        def sm_(e, bk=bk, m4=m4):
                            last = None
                            for mm_ in range(4):
                                m = m4 * 4 + mm_
                                kt = k1T if m % 2 == 0 else k2T
                                last = e.matmul(bk[:, mm_ * 128:(mm_ + 1) * 128], qqT[:, m, :],
                                                kt[:, m // 2, :], start=True, stop=True)
                            return last
                        T(sm_, ["A", "k1T", "k2T"], [bkk])
                        yield
                        A(lambda e, bk=bk, m4=m4: e.copy(
                            sc[:, m4 * 4:(m4 + 1) * 4, :].rearrange("p m t -> p (m t)"), bk[:]),
                          [bkk], ["B"])
                        yield
                    sck = ["B"]
                    for m in range(16):
                        V(lambda e, m=m: e.max(mx[:, m, 0:8], sc[:, m, :]), sck, ["mx%d" % m])
                        yield
                    for m in range(16):
                        V(lambda e, m=m: e.max_index(ix[:, m, 0:8], mx[:, m, 0:8], sc[:, m, :]),
                          sck + ["mx%d" % m], ["ix%d" % m])
                        yield
                    for m in range(16):
                        V(lambda e, m=m: e.match_replace(sc2[:, m, :], mx[:, m, 0:8], sc[:, m, :], -1e30),
                          sck + ["mx%d" % m], ["A"])
                        yield
                    for m in range(16):
                        V(lambda e, m=m: e.max(mx[:, m, 8:16], sc2[:, m, :]), ["A"], ["mx%d" % m])
                        yield
                    for m in range(16):
                        V(lambda e, m=m: e.max_index(ix[:, m, 8:16], mx[:, m, 8:16], sc2[:, m, :]),
                          ["A", "mx%d" % m], ["ix%d" % m])
                        yield
                    mxk = ["mx%d" % m for m in range(16)]
                    ixk = ["ix%d" % m for m in range(16)]
                    V(lambda e: e.tensor_copy(ixf[:], ix[:]), ixk, ["ixf"])
                    yield
                    mxv = mx[:].rearrange("p (h two) a -> p h two a", two=2)
                    ixv = ixf[:].rearrange("p (h two) a -> p h two a", two=2)
                    V(lambda e: e.tensor_tensor(
                        cand.rearrange("p h (a b) -> p h a b", b=16),
                        mxv[:, :, 0, :].unsqueeze(3).to_broadcast([128, 8, 16, 16]),
                        mxv[:, :, 1, :].unsqueeze(2).to_broadcast([128, 8, 16, 16]), ALU.add),
                      mxk + ixk, ["B"])
                    yield
                    for h in range(8):
                        V(lambda e, h=h: e.max(best[:, h, 0:8], cand[:, h, :]), ["B"], ["best%d" % h])
                        yield
                    for h in range(8):
                        V(lambda e, h=h: e.max_index(pos[:, h, 0:8], best[:, h, 0:8], cand[:, h, :]),
                          ["B", "best%d" % h], ["pos%d" % h])
                        yield
                    for h in range(8):
                        V(lambda e, h=h: e.match_replace(cand2[:, h, :], best[:, h, 0:8], cand[:, h, :], -1e30),
                          ["B", "best%d" % h], ["C"])
                        yield
                    for h in range(8):
                        V(lambda e, h=h: e.max(best[:, h, 8:16], cand2[:, h, :]), ["C"], ["best%d" % h])
                        yield
                    for h in range(8):
                        V(lambda e, h=h: e.max_index(pos[:, h, 8:16], best[:, h, 8:16], cand2[:, h, :]),
                          ["C", "best%d" % h], ["pos%d" % h])
                        yield
                    bestk = ["best%d" % h for h in range(8)]
                    posk = ["pos%d" % h for h in range(8)]
                    gsl = IJg[:, 2, :].rearrange("p (h k) -> p h k", k=16)
                    V(lambda e: e.tensor_tensor(gsl, best[:], best[:, :, 0:1].to_broadcast([128, 8, 16]),
                                                ALU.subtract), bestk, ["gate"])
                    yield
                    A(lambda e: e.activation(gsl, gsl, AF.Exp), ["gate"], ["gate"])
                    yield
                    V(lambda e: e.tensor_reduce(ssum[:, 0:8], gsl, AX.X, ALU.add), ["gate"], ["ssum"])
                    yield
                    V(lambda e: e.reciprocal(ssum[:, 8:16], ssum[:, 0:8]), ["ssum"], ["ssum"])
                    yield
                    V(lambda e: e.tensor_tensor(gsl, gsl, ssum[:, 8:16].unsqueeze(2).to_broadcast([128, 8, 16]),
                                                ALU.mult), ["gate", "ssum"], ["gate"])
                    yield
                    V(lambda e: e.tensor_single_scalar(pab_u[:, 0], pos[:], 4, ALU.logical_shift_right),
                      posk, ["pab_u"])
                    yield
                    V(lambda e: e.tensor_single_scalar(pab_u[:, 1], pos[:], 15, ALU.bitwise_and),
                      posk + ["pab_u"], ["pab_u"])
                    yield
                    V(lambda e: e.tensor_copy(pa[:], pab_u[:, 0]), ["pab_u"], ["pa"])
                    yield
                    V(lambda e: e.tensor_copy(pb[:], pab_u[:, 1]), ["pab_u"], ["pb"])
                    yield
                    i16 = iota16.unsqueeze(1).unsqueeze(1).to_broadcast([128, 8, 16, 16])
                    for which, (pp, ppk) in enumerate(((pa, "pa"), (pb, "pb"))):
                        V(lambda e, pp=pp: e.tensor_tensor(oh, pp[:].unsqueeze(3).to_broadcast([128, 8, 16, 16]),
                                                           i16, ALU.is_equal), [ppk, "iota"], ["C"])
                        yield
                        V(lambda e, which=which: e.tensor_tensor(
                            oh, oh, ixv[:, :, which, :].unsqueeze(2).to_broadcast([128, 8, 16, 16]),
                            ALU.mult), ["C", "ixf"], ["C"])
                        yield
                        V(lambda e, which=which: e.tensor_reduce(
                            IJg[:, which, :].rearrange("p (h k) -> p h k", k=16), oh, AX.X, ALU.add),
                          ["C"], ["IJ%d" % which])
                        yield
                    bk, bkk = rotw.get()

                    def ijt(e, bk=bk):
                        last = None
                        for w_ in range(3):
                            last = e.transpose(bk[:, w_ * 128:(w_ + 1) * 128], IJg[:, w_, :], ident_f[:])
                        return last
                    T(ijt, ["IJ0", "IJ1", "gate", "ident_f"], [bkk])
                    yield
                    A(lambda e, bk=bk, ts_=ts_: e.copy(IJgT[:, gb, :, ts_], bk[:, 0:384].rearrange("p (w t) -> p w t", w=3)),
                      [bkk], ["IJgT%d_%d" % (gb, sub)])
                    yield

            def wbuild(g):
                gb = g % 2
                for sub in range(2):
                    for q in range(16):
                        tq = sub * 128 + q * 8
                        sl = q % 2
                        ik = "IJgT%d_%d" % (gb, sub)
                        iob = iota_b[:].unsqueeze(1).to_broadcast([128, 8, 128])
                        V(lambda e, sl=sl, tq=tq: e.tensor_tensor(
                            OHI[:, sl], iob, IJgT[:, gb, 0, tq:tq + 8].unsqueeze(2).to_broadcast([128, 8, 128]),
                            ALU.is_equal), [ik, "iota_b"], ["OHI%d" % sl])
                        V(lambda e, sl=sl, tq=tq: e.tensor_tensor(
                            OHJ[:, sl], iob, IJgT[:, gb, 1, tq:tq + 8].unsqueeze(2).to_broadcast([128, 8, 128]),
                            ALU.is_equal), [ik, "iota_b"], ["OHJ%d" % sl])
                        V(lambda e, sl=sl, tq=tq: e.tensor_tensor(
                            OHJg[:, sl], OHJ[:, sl],
                            IJgT[:, gb, 2, tq:tq + 8].unsqueeze(2).to_broadcast([128, 8, 128]),
                            ALU.mult), [ik, "OHJ%d" % sl], ["OHJg%d" % sl])
                        for q4 in range(2):
                            bk, bkk = rotw.get()

                            def wm(e, bk=bk, sl=sl, q4=q4):
                                last = None
                                for tt in range(4):
                                    t_ = q4 * 4 + tt
                                    last = e.matmul(bk[:, tt * 128:(tt + 1) * 128], OHJg[:, sl, t_, :],
                                                    OHI[:, sl, t_, :], start=True, stop=True)
                                return last
                            T(wm, ["OHJg%d" % sl, "OHI%d" % sl], [bkk])
                            A(lambda e, bk=bk, tq=tq, q4=q4: e.copy(
                                W[:, tq + q4 * 4:tq + q4 * 4 + 4, :].rearrange("p t i -> p (t i)"), bk[:]),
                              [bkk], ["W"])

            gq = gating_gen(0)
            for _ in gq:
                pass
            for g in range(NG):
                r0 = g * TG
                b = r0 // S
                gb = g % 2
                h2k = ["h2_%d_0" % gb, "h2_%d_1" % gb]
                wbuild(g)
                gq = gating_gen(g + 1) if g + 1 < NG else iter(())
                LA = 2
                GI = 5
                pend = []
                for ci in range(NEXP_CH + LA):
                    if ci < NEXP_CH:
                        cg, cc = divmod(ci, 4)
                        sl = cg % 2
                        if cc == 0:
                            e0 = cg * 512
                            P.dma("sp", ub[:, sl], uTb_s[:, e0:e0 + 512].rearrange("(k p) e -> p k e", p=128),
                                  reads=["uTb_s"], writes=["ub%d" % sl])
                            P.dma("sp", vbuf[:, sl], vb_s[e0:e0 + 512, :].rearrange("(c j) d -> j c d", j=128),
                                  reads=["vb_s"], writes=["vbuf%d" % sl])
                        by, byk = rotw.get()

                        def ym(e, by=by, sl=sl, cc=cc, gb=gb):
                            last = None
                            for k in range(8):
                                last = e.matmul(by[:, 0:TG], ub[:, sl, k, cc * 128:(cc + 1) * 128], h2[:, gb, k, :],
                                                start=(k == 0), stop=(k == 7))
                            return last
                        T(ym, ["ub%d" % sl] + h2k, [byk])
                        gs = ci % 2
                        zs = ci % 3
                        A(lambda e, by=by, gs=gs: e.activation(gel[:, gs, :], by[:, 0:TG], AF.Gelu_apprx_tanh),
                          [byk], ["gel%d" % gs])
                        V(lambda e, zs=zs, gs=gs, ci=ci: e.tensor_tensor(Z[:, zs, :], gel[:, gs, :], W[:, :, ci],
                                                                         ALU.mult),
                          ["gel%d" % gs, "W"], ["Z%d" % zs])

                        def vm(e, zs=zs, sl=sl, cc=cc, ci=ci):
                            last = None
                            for sub in range(2):
                                for half in range(2):
                                    last = e.matmul(banks[sub * 2 + half][:],
                                                    Z[:, zs, sub * 128:(sub + 1) * 128],
                                                    vbuf[:, sl, cc, half * 512:(half + 1) * 512],
                                                    start=(ci == 0), stop=(ci == NEXP_CH - 1))
                            return last
                        pend.append((vm, ["Z%d" % zs, "vbuf%d" % sl]))
                    if ci >= LA:
                        vm_, rk_ = pend.pop(0)
                        T(vm_, rk_, ["bk0", "bk1", "bk2", "bk3"])
                    for _ in range(GI):
                        next(gq, None)
                for _ in gq:
                    pass
                for sub in range(2):
                    rr = r0 + sub * 128
                    P.dma("sp", xr, out_d[rr:rr + 128, :], reads=["out_g%d" % g], writes=["C"])
                    for half in range(2):
                        hs = slice(half * 512, (half + 1) * 512)
                        V(lambda e, sub=sub, half=half, hs=hs: e.tensor_tensor(
                            res[:, hs], banks[sub * 2 + half][:], gt2_bc[:, b, hs], ALU.mult),
                          ["bk%d" % (sub * 2 + half), "gt2_bc", "A"], ["A"])
                    G(lambda e: e.tensor_tensor(res, res, xr, ALU.add), ["A", "C"], ["A"])
                    P.dma("sp", out_d[rr:rr + 128, :], res, reads=["A"], writes=["out_f"], semkey="st_res")
            P.final_wait("sp", ["out_f"])
        P.emit()
        print("instructions:", P.n_instr, {e: len(P.ops[e]) for e in ENGS}, "dma sems:", len(P.dsem))
    return nc


def _t5_bucket_table():
    import jax
    import jax.numpy as jnp
    with jax.default_device(jax.devices("cpu")[0]):
        return _t5_bucket_table_impl(jnp)


def _t5_bucket_table_impl(jnp):
    rel = jnp.arange(-255, 128, dtype=jnp.int32)
    half = 16
    max_exact = 8
    ret = jnp.where(rel > 0, half, 0)
    n = jnp.abs(rel)
    nf = jnp.maximum(n, 1).astype(jnp.float32)
    large = max_exact + (jnp.log(nf / max_exact) / math.log(128 / max_exact)
                         * (half - max_exact)).astype(jnp.int32)
    large = jnp.minimum(large, half - 1)
    return np.asarray(ret + jnp.where(n < max_exact, n, large))


def fm(vec, nchunk):
    return np.ascontiguousarray(np.asarray(vec, np.float32).reshape(nchunk, 128).T)


def prep_shared(inp):
    f = lambda a: np.ascontiguousarray(np.asarray(a, np.float32))
    bt = _t5_bucket_table()
    rb = f(inp["rel_bias"])
    ii = np.arange(128)
    biasT = np.empty((128, 2, 8, 128), np.float32)
    for blk in range(2):
        rel = (blk - 1) * 128 + ii[:, None] - ii[None, :]
        biasT[:, blk] = np.transpose(rb[bt[rel + 255]], (0, 2, 1))
    far = rb[15]
    sh = {
        "w_ada": f(inp["w_ada"][0]),
        "b_ada": fm(inp["b_ada"][0], 48),
        "g1": fm(inp["g_norm1"][0], 8),
        "g2": fm(inp["g_norm2"][0], 8),
        "w_in": f(inp["w_in"][0]),
        "qg": np.ascontiguousarray(np.tile(f(inp["q_norm_g"][0]), 2)[:, None]),
        "kg": np.ascontiguousarray(np.tile(f(inp["k_norm_g"][0]), 2)[:, None]),
        "cw": np.ascontiguousarray(np.transpose(f(inp["conv_w"][0])[:, 0, :].reshape(31, 4, 128), (2, 1, 0)).reshape(128, 124)),
        "cb": fm(inp["conv_b"][0], 4),
        "clg": fm(inp["conv_ln_g"][0], 4),
        "clb": fm(inp["conv_ln_b"][0], 4),
        "goc": fm(inp["g_out_conv"][0], 4),
        "goa": fm(inp["g_out_attn"][0], 4),
        "biasT": np.ascontiguousarray(biasT.reshape(128, -1)),
        "biasfar": np.ascontiguousarray(np.broadcast_to(far[None, :], (128, 8))),
        "w_out": f(inp["w_out"][0]),
        "w_pq": f(inp["w_peer_q"][0]),
        "k1T": np.ascontiguousarray(np.transpose(f(inp["peer_k1"][0]), (2, 0, 1)).reshape(128, 1024)),
        "k2T": np.ascontiguousarray(np.transpose(f(inp["peer_k2"][0]), (2, 0, 1)).reshape(128, 1024)),
        "uT": np.ascontiguousarray(f(inp["peer_u"][0]).T),
        "vtab": f(inp["peer_v"][0]),
    }
    return sh


def prep_core(x, c, NB):
    S = x.shape[1]
    cT = np.ascontiguousarray(np.transpose(np.asarray(c, np.float32).reshape(NB, 8, 128), (2, 1, 0)).reshape(128, 8 * NB))
    return {"x": np.ascontiguousarray(np.asarray(x, np.float32).reshape(NB * S, D)), "cT": cT}


N_CORES = 8


def kernel(**inputs):
    x = np.asarray(inputs["x"])
    c = np.asarray(inputs["c"])
    B, S, _ = x.shape
    NB = B // N_CORES
    NSEL = min(256, S // 4)
    nc = build(S, NB, NSEL)
    sh = prep_shared(inputs)
    in_maps = []
    for i in range(N_CORES):
        m = dict(sh)
        m.update(prep_core(x[i * NB:(i + 1) * NB], c[i * NB:(i + 1) * NB], NB))
        in_maps.append(m)
    res = run_bass_kernel_spmd(nc, in_maps, core_ids=list(range(N_CORES)))
    out = np.concatenate([np.asarray(r["out"]).reshape(NB, S, D) for r in res.results], axis=0)
    return out.astype(np.float32)
```

```python
import math
from contextlib import ExitStack

import numpy as np
import concourse.bass as bass
import concourse.mybir as mybir
from concourse.bass_utils import run_bass_kernel_spmd

F32 = mybir.dt.float32
BF16 = mybir.dt.bfloat16
U32 = mybir.dt.uint32
ALU = mybir.AluOpType
AF = mybir.ActivationFunctionType
AX = mybir.AxisListType

D = 1024
NCOL = 2884
EPS = 1e-6
NEXP_CH = 128
NEG = -30000.0
ACT_EVAC = False
ENGS = ("pe", "act", "dve", "pool", "sp")


class Prog:
    def __init__(self, nc, stack):
        self.nc = nc
        self.stack = stack
        self.ops = {e: [] for e in ENGS}
        self.cnt = {e: 0 for e in ENGS}
        self.sem = {e: stack.enter_context(nc.semaphore("s_" + e)) for e in ENGS}
        self.seen = {e: {} for e in ENGS}
        self.dsem = {}
        self.lastw = {}
        self.readers = {}
        self.n_instr = 0
        self.engs = {"pe": nc.tensor, "act": nc.scalar, "dve": nc.vector,
                     "pool": nc.gpsimd, "sp": nc.sync}

    def _need(self, e, tok, waits):
        if tok is None:
            return
        sem, val, name = tok
        if e == "pe" and name == "s_pe":
            return
        if self.seen[e].get(name, 0) >= val:
            return
        prev = waits.get(name)
        if prev is None or prev[1] < val:
            waits[name] = (sem, val)

    def _deps(self, e, reads, writes):
        waits = {}
        for k in reads:
            self._need(e, self.lastw.get(k), waits)
        for k in writes:
            self._need(e, self.lastw.get(k), waits)
            for t in self.readers.get(k, {}).values():
                self._need(e, t, waits)
        for name, (sem, val) in waits.items():
            self.seen[e][name] = val
        return list(waits.values())

    def _commit(self, tok, reads, writes):
        for k in writes:
            self.lastw[k] = tok
            self.readers[k] = {}
        for k in reads:
            if k in writes:
                continue
            d = self.readers.setdefault(k, {})
            old = d.get(tok[2])
            if old is None or old[1] < tok[1]:
                d[tok[2]] = tok

    def op(self, e, fn, reads=(), writes=()):
        waits = self._deps(e, reads, writes)
        self.cnt[e] += 1
        val = self.cnt[e]
        sem = self.sem[e]

        def thunk(eng, fn=fn, waits=waits, sem=sem):
            for (s, v) in waits:
                eng.wait_ge(s, v)
            fn(eng).then_inc(sem, 1)

        thunk(self.engs[e])
        self.ops[e].append(1)
        self._commit((sem, val, "s_" + e), reads, writes)
        self.n_instr += 1

    def dma(self, q, out, in_, reads=(), writes=(), semkey=None):
        if semkey is None:
            semkey = writes[0]
        if semkey not in self.dsem:
            self.dsem[semkey] = [self.stack.enter_context(
                self.nc.semaphore("d_%d" % len(self.dsem))), 0]
        waits = self._deps(q, reads, writes)
        ent = self.dsem[semkey]
        ent[1] += 16
        sem, val = ent[0], ent[1]

        def thunk(eng, waits=waits, sem=sem, out=out, in_=in_):
            for (s, v) in waits:
                eng.wait_ge(s, v)
            eng.dma_start(out=out, in_=in_).then_inc(sem, 16)

        thunk(self.engs[q])
        self.ops[q].append(1)
        self._commit((sem, val, "d_" + str(semkey)), reads, writes)
        self.n_instr += 1

    def barrier(self):
        toks = [(self.sem[e], self.cnt[e], "s_" + e) for e in ENGS if self.cnt[e] > 0]
        toks += [(ent[0], ent[1], "d_" + str(k)) for k, ent in self.dsem.items()
                 if k not in ("uTb_s", "vb_s")]
        for e in ENGS:
            waits = {}
            for t in toks:
                self._need(e, t, waits)
            for name, (sem, val) in waits.items():
                self.seen[e][name] = val
                self.engs[e].wait_ge(sem, val)

    def final_wait(self, e, keys):
        waits = self._deps(e, keys, ())

        def thunk(eng, waits=waits):
            for (s, v) in waits:
                eng.wait_ge(s, v)

        thunk(self.engs[e])

    def emit(self):
        return
        nc = self.nc
        with nc.Block() as block:
            @block.tensor
            def _(eng):
                for t in self.ops["pe"]:
                    t(eng)

            @block.scalar
            def _(eng):
                for t in self.ops["act"]:
                    t(eng)

            @block.vector
            def _(eng):
                for t in self.ops["dve"]:
                    t(eng)

            @block.gpsimd
            def _(eng):
                for t in self.ops["pool"]:
                    t(eng)

            @block.sync
            def _(eng):
                for t in self.ops["sp"]:
                    t(eng)


class _Stop(Exception):
    pass


def build(S, NB, NSEL, n_iter=28, phase1_only=False, stop_at=None):
    holder = {}
    try:
        return _build(S, NB, NSEL, n_iter, phase1_only, stop_at, holder)
    except _Stop:
        P = holder["P"]
        P.barrier()
        return holder["nc"]


def _build(S, NB, NSEL, n_iter, phase1_only, stop_at, holder):
    NT = S // 128
    TOK = NB * S
    NTT = TOK // 128
    TG = 256
    NG = TOK // TG
    nc = bass.Bass("TRN2", target_bir_lowering=False)
    holder["nc"] = nc

    def mark(name):
        if stop_at == name:
            raise _Stop()

    def din(name, shape, dt=F32):
        return nc.dram_tensor(name, shape, dt, kind="ExternalInput").ap()

    x_d = din("x", [TOK, D])
    cT_d = din("cT", [128, 8 * NB])
    wada_d = din("w_ada", [D, 6 * D])
    bada_d = din("b_ada", [128, 48])
    g1_d = din("g1", [128, 8])
    g2_d = din("g2", [128, 8])
    win_d = din("w_in", [D, NCOL])
    qg_d = din("qg", [128, 1])
    kg_d = din("kg", [128, 1])
    cw_d = din("cw", [128, 4 * 31])
    cb_d = din("cb", [128, 4])
    clg_d = din("clg", [128, 4])
    clb_d = din("clb", [128, 4])
    goc_d = din("goc", [128, 4])
    goa_d = din("goa", [128, 4])
    biasT_d = din("biasT", [128, 2 * 8 * 128])
    biasfar_d = din("biasfar", [128, 8])
    wout_d = din("w_out", [D, D])
    wpq_d = din("w_pq", [D, 2048])
    k1T_d = din("k1T", [128, 8 * 128])
    k2T_d = din("k2T", [128, 8 * 128])
    uT_d = din("uT", [D, 16384])
    v_d = din("vtab", [16384, D])
    out_d = nc.dram_tensor("out", [TOK, D], F32, kind="ExternalOutput").ap()
    h2T_s = nc.dram_tensor("h2T_s", [TOK, D], BF16, kind="Internal").ap()
    uTb_s = nc.dram_tensor("uTb_s", [D, 16384], BF16, kind="Internal").ap()
    vb_s = nc.dram_tensor("vb_s", [16384, D], BF16, kind="Internal").ap()

    with ExitStack() as st:
        P = Prog(nc, st)
        holder["P"] = P

        def V(fn, r=(), w=()):
            P.op("dve", fn, r, w)

        def A(fn, r=(), w=()):
            P.op("act", fn, r, w)

        def G(fn, r=(), w=()):
            P.op("pool", fn, r, w)

        def T(fn, r=(), w=()):
            P.op("pe", fn, r, w)

        def sbuf(stack, name, shape, dt):
            return stack.enter_context(nc.sbuf_tensor("sb_" + name, shape, dt))

        banks = [st.enter_context(nc.psum_tensor("bk%d" % i, [128, 512], F32)) for i in range(8)]

        class Rot:
            def __init__(self, idxs):
                self.idxs = idxs
                self.n = 0

            def get(self):
                i = self.idxs[self.n % len(self.idxs)]
                self.n += 1
                return banks[i], "bk%d" % i

        ident_f = sbuf(st, "ident_f", [128, 128], F32)
        ident_b = sbuf(st, "ident_b", [128, 128], BF16)
        ones_f = sbuf(st, "ones_f", [128, 128], F32)
        bones_f = sbuf(st, "bones_f", [128, 128], F32)
        iota_f = sbuf(st, "iota_f", [128, 128], F32)
        iota_b = sbuf(st, "iota_b", [128, 128], BF16)
        pid_f = sbuf(st, "pid_f", [128, 1], F32)
        epsc = sbuf(st, "epsc", [128, 1], F32)
        a1 = sbuf(st, "a1", [128, NB, 8], F32)
        sh1 = sbuf(st, "sh1", [128, NB, 8], F32)
        gt1 = sbuf(st, "gt1", [128, NB, 8], F32)
        a2 = sbuf(st, "a2", [128, NB, 8], F32)
        sh2 = sbuf(st, "sh2", [128, NB, 8], F32)
        gt2 = sbuf(st, "gt2", [128, NB, 8], F32)
        g1 = sbuf(st, "g1", [128, 8], F32)
        g2 = sbuf(st, "g2", [128, 8], F32)

        G(lambda e: e.iota(iota_f[:], pattern=[[1, 128]], base=0, channel_multiplier=0,
                           allow_small_or_imprecise_dtypes=True), w=["iota"])
        G(lambda e: e.iota(pid_f[:], pattern=[[0, 1]], base=0, channel_multiplier=1,
                           allow_small_or_imprecise_dtypes=True), w=["pid"])
        V(lambda e: e.tensor_scalar(ident_f[:], iota_f[:], pid_f[:, 0:1], None, ALU.is_equal),
          ["iota", "pid"], ["ident_f"])
        V(lambda e: e.tensor_copy(ident_b[:], ident_f[:]), ["ident_f"], ["ident_b"])
        V(lambda e: e.tensor_copy(iota_b[:], iota_f[:]), ["iota"], ["iota_b"])
        V(lambda e: e.memset(ones_f[:], 1.0), w=["ones_f"])
        V(lambda e: e.memset(bones_f[:], 0.0), w=["bones_f"])
        V(lambda e: e.memset(bones_f[0:64, 0:64], 1.0), ["bones_f"], ["bones_f"])
        V(lambda e: e.memset(bones_f[64:128, 64:128], 1.0), ["bones_f"], ["bones_f"])
        V(lambda e: e.memset(epsc[:], EPS), w=["epsc"])
        P.dma("sp", g1[:], g1_d[:, :], writes=["g1"])
        P.dma("sp", g2[:], g2_d[:, :], writes=["g2"])
        for (src, dst, key) in ((uT_d, uTb_s, "uTb_s"), (v_d, vb_s, "vb_s")):
            sv = src.rearrange("(p a) e -> p (a e)", p=128)
            dv = dst.rearrange("(p a) e -> p (a e)", p=128)
            for pc in range(16):
                cs = slice(pc * 8192, (pc + 1) * 8192)
                P.dma("pool", dv[:, cs], sv[:, cs], writes=[key])

        def rsqrt(out, src, scale, rk, wk):
            np_ = out.shape[0]
            A(lambda e: e.activation(out, src, AF.Sqrt, bias=epsc[0:np_, 0:1], scale=scale),
              list(rk) + ["epsc"], wk)
            V(lambda e: e.reciprocal(out, out), wk, wk)

        with ExitStack() as s0:
            cT = sbuf(s0, "cT", [128, 8 * NB], F32)
            scs = sbuf(s0, "scs", [128, 8 * NB], F32)
            bada = sbuf(s0, "bada", [128, 48], F32)
            mod = sbuf(s0, "mod", [128, 48, NB], F32)
            stg = sbuf(s0, "stg", [128, 2, 8, 1024], F32)
            P.dma("sp", cT[:], cT_d[:, :], writes=["cT"])
            P.dma("sp", bada[:], bada_d[:, :], writes=["bada"])
            A(lambda e: e.activation(scs[:], cT[:], AF.Silu), ["cT"], ["scs"])
            rot = Rot(list(range(8)))
            for fp in range(6):
                sl = fp % 2
                P.dma("sp", stg[:, sl], wada_d[:, fp * 1024:(fp + 1) * 1024].rearrange(
                    "(k p) f -> p k f", p=128), writes=["stg%d" % sl])
                bk, bkk = rot.get()

                def mm(e, sl=sl, bk=bk):
                    last = None
                    for fc in range(8):
                        for k in range(8):
                            last = e.matmul(bk[:, fc * NB:(fc + 1) * NB],
                                            stg[:, sl, k, fc * 128:(fc + 1) * 128],
                                            scs[:, k * NB:(k + 1) * NB],
                                            start=(k == 0), stop=(k == 7))
                    return last
                T(mm, ["stg%d" % sl, "scs"], [bkk])
                for b in range(NB):
                    V(lambda e, b=b, bk=bk, fp=fp: e.tensor_tensor(
                        mod[:, fp * 8:(fp + 1) * 8, b],
                        bk[:, 0:8 * NB].rearrange("p (f b) -> p f b", b=NB)[:, :, b],
                        bada[:, fp * 8:(fp + 1) * 8], ALU.add), [bkk, "bada"], ["mod"])
            for b in range(NB):
                V(lambda e, b=b: e.scalar_tensor_tensor(a1[:, b, :], mod[:, 8:16, b], 1.0, g1[:],
                                                        ALU.add, ALU.mult), ["mod", "g1"], ["a1"])
                V(lambda e, b=b: e.scalar_tensor_tensor(a2[:, b, :], mod[:, 32:40, b], 1.0, g2[:],
                                                        ALU.add, ALU.mult), ["mod", "g2"], ["a2"])
                V(lambda e, b=b: e.tensor_copy(sh1[:, b, :], mod[:, 0:8, b]), ["mod"], ["sh1"])
                V(lambda e, b=b: e.tensor_copy(gt1[:, b, :], mod[:, 16:24, b]), ["mod"], ["gt1"])
                V(lambda e, b=b: e.tensor_copy(sh2[:, b, :], mod[:, 24:32, b]), ["mod"], ["sh2"])
                V(lambda e, b=b: e.tensor_copy(gt2[:, b, :], mod[:, 40:48, b]), ["mod"], ["gt2"])

        mark("adaln")
        P.barrier()

        def gate_bcast(dst, gt, b, gkey, dkey, rot):
            for half in range(2):
                bk, bkk = rot.get()
                for kk in range(4):
                    k = half * 4 + kk
                    V(lambda e, k=k: e.tensor_scalar(rep[:], ones_f[:], gt[:, b, k:k + 1], None,
                                                     ALU.mult), ["ones_f", gkey], ["rep"])
                    T(lambda e, kk=kk, bk=bk: e.matmul(bk[:, kk * 128:(kk + 1) * 128], rep[:],
                                                       ident_f[:], start=True, stop=True),
                      ["rep", "ident_f"], [bkk])
                V(lambda e, half=half, bk=bk: e.tensor_copy(dst[:, half * 512:(half + 1) * 512],
                                                            bk[:]), [bkk], [dkey])

        rep = sbuf(st, "rep", [128, 128], F32)

        with ExitStack() as s1:
            w_in = sbuf(s1, "w_in", [128, 8, NCOL], BF16)
            w_out = sbuf(s1, "w_out", [128, 8, D], BF16)
            kT = sbuf(s1, "kT", [128, 4, S], BF16)
            vsb = sbuf(s1, "vsb", [128, NT, 8, 65], BF16)
            kiT = sbuf(s1, "kiT", [64, S], BF16)
            idx = sbuf(s1, "idx", [128, max(S, NCOL)], F32)
            mbs = sbuf(s1, "mbs", [128, 2, S], BF16)
            xts = sbuf(s1, "xts", [128, 2, D], F32)
            Dg = sbuf(s1, "Dg", [128, 2, 128], F32)
            gt1_bc = xts[:, 1]
            hT = sbuf(s1, "hT", [128, 8, 128], BF16)
            h2T = hT
            mixTs = sbuf(s1, "mixTs", [128, 2, 8, 128], BF16)
            ubuf = sbuf(s1, "ubuf", [128, 4, 158], F32)
            yc = sbuf(s1, "yc", [128, 4, 128], F32)
            t2 = yc
            mean = sbuf(s1, "mean", [128, 128], F32)
            var = sbuf(s1, "var", [128, 128], F32)
            r3 = mean
            sq = sbuf(s1, "sq", [128, 512], F32)
            ysq = sq[:].rearrange("p (c t) -> p c t", c=4)
            rl = sq[:].rearrange("p (o t) -> p o t", o=1)
            zc = yc
            rq = sbuf(s1, "rq", [128, 512], F32)
            print("phase1 sbuf bytes free:", nc.sbuf_bytes_remaining)
            qn = sq
            qpads = sbuf(s1, "qpads", [128, 2, 8, 128], BF16)
            qiT = sbuf(s1, "qiT", [64, 4, 128], BF16)
            Eb = sbuf(s1, "Eb", [128, 2, 512], BF16)
            attn = sbuf(s1, "attn", [128, 512], F32)
            biasTb = sbuf(s1, "biasTb", [128, 2, 8, 128], BF16)
            biasfar = sbuf(s1, "biasfar", [128, 8], F32)
            sm = sbuf(s1, "sm", [128, 64], F32)
            wi = sbuf(s1, "wi", [128, 12], F32)
            bis = sbuf(s1, "bis", [128, 8], F32)
            qg = sbuf(s1, "qg", [128, 1], F32)
            kg = sbuf(s1, "kg", [128, 1], F32)
            cw = sbuf(s1, "cw", [128, 4, 31], F32)
            cb = sbuf(s1, "cb", [128, 4], F32)
            clg = sbuf(s1, "clg", [128, 4], F32)
            clb = sbuf(s1, "clb", [128, 4], F32)
            goc = sbuf(s1, "goc", [128, 4], F32)
            goa = sbuf(s1, "goa", [128, 4], F32)

            for nm, t, dsrc in (("qg", qg, qg_d), ("kg", kg, kg_d), ("cb", cb, cb_d),
                                ("clg", clg, clg_d), ("clb", clb, clb_d), ("goc", goc, goc_d),
                                ("goa", goa, goa_d), ("biasfar", biasfar, biasfar_d)):
                P.dma("sp", t[:], dsrc[:, :], writes=[nm])
            P.dma("sp", cw[:], cw_d[:, :].rearrange("p (c j) -> p c j", j=31), writes=["cw"])
            V(lambda e: e.tensor_scalar(qg[:], qg[:], 0.125, None, ALU.mult), ["qg"], ["qg"])
            for blk in range(2):
                P.dma("sp", idx[:, 0:1024], biasT_d[:, blk * 1024:(blk + 1) * 1024], writes=["idx"])
                for h in range(8):
                    V(lambda e, h=h, blk=blk: e.tensor_scalar(
                        biasTb[:, blk, h, :], idx[:, h * 128:(h + 1) * 128],
                        biasfar[:, h:h + 1], None, ALU.subtract), ["idx", "biasfar"], ["biasTb"])
            V(lambda e: e.memset(qpads[:], 0.0), w=["qpad0", "qpad1"])
            V(lambda e: e.memset(vsb[:], 1.0), w=["vsb%d" % j for j in range(NT)])

            for k in range(8):
                P.dma("sp", idx[:, 0:NCOL], win_d[k * 128:(k + 1) * 128, :], writes=["idx"])
                if k % 2 == 0:
                    G(lambda e, k=k: e.tensor_copy(w_in[:, k, :], idx[:, 0:NCOL]), ["idx"], ["w_in"])
                else:
                    A(lambda e, k=k: e.copy(w_in[:, k, :], idx[:, 0:NCOL]), ["idx"], ["w_in"])
            mark("setup1")
            P.barrier()
            rot = Rot(list(range(6)))
            tb_eps = -(2.0 ** -20)

            def norm_to_T(src, ssq_col, r_col, ak, shk, b, dstT, dkey, skey, dsl):
                dks = ["%s%d" % (dkey, k) for k in range(8)]
                A(lambda e: e.activation(dstT[:].rearrange("p k t -> p (k t)"), src, AF.Square,
                                         accum_out=sm[:, ssq_col:ssq_col + 1]),
                  [skey], dks + ["sm%d" % ssq_col])
                rsqrt(sm[:, r_col:r_col + 1], sm[:, ssq_col:ssq_col + 1], 1.0 / D,
                      ["sm%d" % ssq_col], ["sm%d" % r_col])
                V(lambda e: e.tensor_scalar(Dg[:, dsl, :], ident_f[:], sm[:, r_col:r_col + 1], None, ALU.mult),
                  ["ident_f", "sm%d" % r_col], ["Dg%d" % dsl])
                for half in range(2):
                    bk, bkk = rot.get()

                    def tr(e, half=half, bk=bk):
                        last = None
                        for kk in range(4):
                            k = half * 4 + kk
                            last = e.matmul(bk[:, kk * 128:(kk + 1) * 128], src[:, k * 128:(k + 1) * 128],
                                            Dg[:, dsl, :], start=True, stop=True)
                        return last
                    T(tr, [skey, "Dg%d" % dsl], [bkk])
                    for kk in range(4):
                        k = half * 4 + kk
                        V(lambda e, k=k, kk=kk, bk=bk: e.tensor_scalar(
                            dstT[:, k, :], bk[:, kk * 128:(kk + 1) * 128],
                            ak[:, b, k:k + 1], shk[:, b, k:k + 1], ALU.mult, ALU.add),
                          [bkk, "a1", "a2", "sh1", "sh2"], ["%s%d" % (dkey, k)])

            hTk = ["hT%d" % k for k in range(8)]
            h2Tk = ["hT%d" % k for k in range(8)]
            mixk = ["mix%d" % k for k in range(8)]

            for b in range(NB):
                gate_bcast(gt1_bc, gt1, b, "gt1", "xt1", rot)
                for k in range(8):
                    P.dma("sp", idx[:, 0:D], wout_d[k * 128:(k + 1) * 128, :], writes=["idx"])
                    (V if k % 2 == 0 else G)(lambda e, k=k: e.tensor_tensor(
                        w_out[:, k, :], idx[:, 0:D], gt1_bc, ALU.mult), ["idx", "xt1"], ["w_out"])
                mark("wout%d" % b)
                def front(b, i):
                    t0 = i * 128
                    r0 = b * S + t0
                    NK = t0 + 128
                    NKB = i + 1
                    ps_ = i % 2
                    vk = "vsb%d" % i
                    kTk = "kT%d" % i
                    kik = "kiT%d" % i
                    xt = xts[:, ps_]
                    xtk = "xt%d" % ps_
                    mb = mbs[:, ps_]
                    mbk = "mb%d" % ps_
                    qpad = qpads[:, ps_]
                    qpk = "qpad%d" % ps_
                    mixT = mixTs[:, ps_]
                    mixk = ["mix%d_%d" % (ps_, k) for k in range(8)]
                    P.dma("sp", xt, x_d[r0:r0 + 128, :], writes=[xtk])
                    norm_to_T(xt, 0, 1, a1, sh1, b, hT, "hT", xtk, 0)
                    def proj_fm(bk, col0, ncols, ncolblk, M):
                        def f(e):
                            last = None
                            for j in range(ncolblk):
                                for k in range(8):
                                    last = e.matmul(bk[0:M, j * 128:(j + 1) * 128],
                                                    w_in[:, k, col0 + j * M:col0 + (j + 1) * M],
                                                    hT[:, k, :], start=(k == 0), stop=(k == 7))
                            return last
                        return f

                    def qknorm(bsrc, bsrck, gain, gk, is_q):
                        A(lambda e: e.activation(sq[:], bsrc[:], AF.Square), [bsrck], ["sq"])
                        bn, bnk = rot.get()
                        T(lambda e: e.matmul(bn[:], bones_f[:], sq[:], start=True, stop=True),
                          ["sq", "bones_f"], [bnk])
                        rsqrt(rq[:], bn[:], 1.0 / 64, [bnk], ["rq"])
                        V(lambda e: e.scalar_tensor_tensor(qn[:], bsrc[:], gain[:, 0:1], rq[:],
                                                           ALU.mult, ALU.mult), [bsrck, gk, "rq", "sq"], ["sq"])
                        if is_q:
                            for eh in range(2):
                                ps = slice(eh * 64, (eh + 1) * 64)
                                G(lambda e, ps=ps, eh=eh: e.tensor_copy(
                                    qpad[ps].rearrange("p (c two) t -> p c two t", two=2)[:, :, eh, :],
                                    qn[ps, :].rearrange("p (c t) -> p c t", c=4)), ["sq"], [qpk])
                        else:
                            G(lambda e: e.tensor_copy(kT[:, :, t0:t0 + 128],
                                                      qn[:].rearrange("p (c t) -> p c t", c=4)),
                              ["sq"], [kTk])
                    bcv, bcvk = rot.get()
                    T(proj_fm(bcv, 0, 512, 4, 128), hTk + ["w_in"], [bcvk])
                    bcg, bcgk = rot.get()
                    T(proj_fm(bcg, 512, 512, 4, 128), hTk + ["w_in"], [bcgk])
                    A(lambda e, bcg=bcg: e.activation(ubuf[:, :, 30:158], bcg[:].rearrange("p (c t) -> p c t", c=4),
                                                      AF.Sigmoid), [bcgk], ["ubuf_c"])
                    if i == 0:
                        G(lambda e: e.memset(ubuf[:, :, 0:30], 0.0), w=["ubuf_h"])
                    V(lambda e, bcv=bcv: e.tensor_tensor(ubuf[:, :, 30:158],
                                                         bcv[:].rearrange("p (c t) -> p c t", c=4),
                                                         ubuf[:, :, 30:158], ALU.mult), [bcvk, "ubuf_c"], ["ubuf_c"])
                    bq, bqk = rot.get()
                    T(proj_fm(bq, 1024, 512, 4, 128), hTk + ["w_in"], [bqk])
                    qknorm(bq, bqk, qg, "qg", True)
                    bkk_, bkkk = rot.get()
                    T(proj_fm(bkk_, 1536, 512, 4, 128), hTk + ["w_in"], [bkkk])
                    qknorm(bkk_, bkkk, kg, "kg", False)
                    bv, bvk = rot.get()

                    def projv(e, bv=bv):
                        last = None
                        for k in range(8):
                            last = e.matmul(bv[:, 0:512], hT[:, k, :], w_in[:, k, 2048:2560],
                                            start=(k == 0), stop=(k == 7))
                        return last
                    T(projv, hTk + ["w_in"], [bvk])
                    A(lambda e, bv=bv: e.copy(vsb[:, i, :, 0:64], bv[:].rearrange("p (h d) -> p h d", h=8)),
                      [bvk], [vk])
                    bi, bik = rot.get()

                    bi2, bi2k = rot.get()

                    def proji2(e, bi=bi, bi2=bi2):
                        last = None
                        for j in range(4):
                            for k in range(8):
                                last = e.matmul(bi[0:64, j * 128:(j + 1) * 128],
                                                w_in[:, k, 2560 + j * 64:2560 + (j + 1) * 64],
                                                hT[:, k, :], start=(k == 0), stop=(k == 7))
                        for k in range(8):
                            last = e.matmul(bi2[0:64, 0:128], w_in[:, k, 2816:2880], hT[:, k, :],
                                            start=(k == 0), stop=(k == 7))
                        for k in range(8):
                            last = e.matmul(bi2[:, 128:132], hT[:, k, :], w_in[:, k, 2880:2884],
                                            start=(k == 0), stop=(k == 7))
                        return last
                    T(proji2, hTk + ["w_in"], [bik, bi2k])

                    V(lambda e, bi=bi: e.tensor_copy(qiT[:], bi[0:64, :].rearrange("p (h t) -> p h t", h=4)),
                      [bik], ["qiT"])
                    V(lambda e, bi2=bi2: e.tensor_copy(kiT[:, t0:t0 + 128], bi2[0:64, 0:128]), [bi2k], [kik])
                    V(lambda e, bi2=bi2: e.tensor_copy(wi[:, 0:4], bi2[:, 128:132]), [bi2k], ["wi"])
                    kik_all = ["kiT%d" % j for j in range(NKB)]
                    G(lambda e: e.iota(idx[:, 0:NK], pattern=[[1, NK]], base=0, channel_multiplier=0,
                                       allow_small_or_imprecise_dtypes=True), w=["idx"])
                    A(lambda e: e.mul(idx[:, 0:NK], idx[:, 0:NK], tb_eps), ["idx"], ["idx"])
                    nrl = 0
                    for c0 in range(0, NK, 512):
                        cn = min(512, NK - c0)
                        for h in range(4):
                            bx, bxk = rot.get()
                            T(lambda e, bx=bx, h=h, c0=c0, cn=cn: e.matmul(
                                bx[:, 0:cn], qiT[:, h, :], kiT[:, c0:c0 + cn], start=True, stop=True),
                              ["qiT"] + kik_all, [bxk])
                            sl = 0
                            A(lambda e, bx=bx, h=h, cn=cn, sl=sl: e.activation(
                                rl[:, sl, 0:cn], bx[:, 0:cn], AF.Relu),
                              [bxk], ["sq"])
                            V(lambda e, h=h, c0=c0, cn=cn, sl=sl: e.scalar_tensor_tensor(
                                idx[:, c0:c0 + cn], rl[:, sl, 0:cn], wi[:, h:h + 1], idx[:, c0:c0 + cn],
                                ALU.mult, ALU.add), ["sq", "wi", "idx"], ["idx"])
                    mark("index%d" % i)
                    need_search = NK > NSEL
                    if need_search:
                        V(lambda e: e.tensor_reduce(bis[:, 0:1], idx[:, 0:NK], AX.X, ALU.max), ["idx"], ["bis"])
                        V(lambda e: e.tensor_reduce(bis[:, 1:2], idx[:, 0:NK], AX.X, ALU.min), ["idx"], ["bis"])
                        V(lambda e: e.tensor_tensor(bis[:, 2:3], bis[:, 0:1], bis[:, 1:2], ALU.subtract),
                          ["bis"], ["bis"])
                    else:
                        V(lambda e: e.memset(bis[:, 1:2], -1e29), w=["bis"])
                    V(lambda e: e.memset(idx[0:64, t0 + 64:t0 + 128], -1e30), ["idx"], ["idx"])
                    if need_search:
                        for it in range(1, n_iter + 1):
                            sc_ = 2.0 ** (-it)
                            V(lambda e, sc_=sc_: e.scalar_tensor_tensor(bis[:, 3:4], bis[:, 2:3], sc_, bis[:, 1:2],
                                                                        ALU.mult, ALU.add), ["bis"], ["bis"])
                            V(lambda e: e.tensor_scalar(mb[:, 0:NK], idx[:, 0:NK], bis[:, 3:4], None,
                                                        ALU.is_ge, ALU.add, accum_out=bis[:, 4:5]),
                              ["idx", "bis"], [mbk, "bis"])
                            V(lambda e, sc_=sc_: e.tensor_scalar(bis[:, 5:6], bis[:, 4:5], NSEL - 0.5, sc_,
                                                                 ALU.is_ge, ALU.mult), ["bis"], ["bis"])
                            V(lambda e: e.scalar_tensor_tensor(bis[:, 1:2], bis[:, 5:6], bis[:, 2:3], bis[:, 1:2],
                                                               ALU.mult, ALU.add), ["bis"], ["bis"])
                    V(lambda e: e.tensor_scalar(mb[:, 0:NK], idx[:, 0:NK], bis[:, 1:2], NEG,
                                                ALU.is_lt, ALU.mult), ["idx", "bis"], [mbk])

                    mark("bisect%d" % i)
                def convblk(b, i):
                    t0 = i * 128
                    r0 = b * S + t0
                    NK = t0 + 128
                    NKB = i + 1
                    ps_ = i % 2
                    vk = "vsb%d" % i
                    kTk = "kT%d" % i
                    kik = "kiT%d" % i
                    xt = xts[:, ps_]
                    xtk = "xt%d" % ps_
                    mb = mbs[:, ps_]
                    mbk = "mb%d" % ps_
                    qpad = qpads[:, ps_]
                    qpk = "qpad%d" % ps_
                    mixT = mixTs[:, ps_]
                    mixk = ["mix%d_%d" % (ps_, k) for k in range(8)]
                    for j in range(31):
                        for c in range(4):
                            if j == 0:
                                V(lambda e, c=c: e.tensor_scalar(yc[:, c, :], ubuf[:, c, 0:128],
                                                                 cw[:, c, 0:1], cb[:, c:c + 1],
                                                                 ALU.mult, ALU.add),
                                  ["ubuf_h", "ubuf_c", "cw", "cb"], ["yc%d" % c])
                            else:
                                V(lambda e, c=c, j=j: e.scalar_tensor_tensor(
                                    yc[:, c, :], ubuf[:, c, j:j + 128], cw[:, c, j:j + 1], yc[:, c, :],
                                    ALU.mult, ALU.add),
                                  ["ubuf_h", "ubuf_c", "cw"], ["yc%d" % c])
                    yck = ["yc%d" % c for c in range(4)]
                    ysqk = ["sq"]
                    if i + 1 < NT:
                        G(lambda e: e.tensor_copy(ubuf[:, :, 0:30], ubuf[:, :, 128:158]),
                          ["ubuf_c"], ["ubuf_h"])
                    G(lambda e: e.tensor_tensor(ysq[:], yc[:], yc[:], ALU.mult), yck, ysqk)
                    bs, bsk = rot.get()

                    def lnstat(e, bs=bs):
                        last = None
                        for c in range(4):
                            last = e.matmul(bs[:, 0:128], ones_f[:], yc[:, c, :], start=(c == 0), stop=(c == 3))
                        for c in range(4):
                            last = e.matmul(bs[:, 128:256], ones_f[:], ysq[:, c, :], start=(c == 0), stop=(c == 3))
                        return last
                    T(lnstat, yck + ysqk + ["ones_f"], [bsk])
                    V(lambda e, bs=bs: e.tensor_scalar(mean[:], bs[:, 0:128], 1.0 / 512, None, ALU.mult),
                      [bsk], ["mean"])
                    V(lambda e: e.tensor_tensor(var[:], mean[:], mean[:], ALU.mult), ["mean"], ["var"])
                    V(lambda e, bs=bs: e.scalar_tensor_tensor(var[:], bs[:, 128:256], 1.0 / 512, var[:],
                                                              ALU.mult, ALU.subtract), [bsk, "var"], ["var"])
                    rsqrt(var[:], var[:], 1.0, ["var"], ["var"])
                    V(lambda e: e.tensor_tensor(t2[:], yc[:], mean[:].unsqueeze(1).to_broadcast([128, 4, 128]),
                                                ALU.subtract), yck + ["mean"], yck)
                    V(lambda e: e.tensor_tensor(t2[:], t2[:], var[:].unsqueeze(1).to_broadcast([128, 4, 128]),
                                                ALU.mult), yck + ["var"], yck)
                    for c in range(4):
                        V(lambda e, c=c: e.tensor_scalar(t2[:, c, :], t2[:, c, :], clg[:, c:c + 1], clb[:, c:c + 1],
                                                         ALU.mult, ALU.add), ["yc%d" % c, "clg", "clb"], ["yc%d" % c])
                    A(lambda e: e.activation(zc[:], t2[:], AF.Silu), yck, yck)
                    zck = yck
                    G(lambda e: e.tensor_tensor(ysq[:], zc[:], zc[:], ALU.mult), zck, ysqk)
                    bs2, bs2k = rot.get()

                    def rmstat(e, bs2=bs2):
                        last = None
                        for c in range(4):
                            last = e.matmul(bs2[:, 0:128], ones_f[:], ysq[:, c, :], start=(c == 0), stop=(c == 3))
                        return last
                    T(rmstat, ysqk + ["ones_f"], [bs2k])
                    rsqrt(r3[:], bs2[:, 0:128], 1.0 / 512, [bs2k, "mean"], ["mean"])
                    for c in range(4):
                        V(lambda e, c=c: e.scalar_tensor_tensor(mixT[:, c, :], zc[:, c, :], goc[:, c:c + 1],
                                                                r3[:], ALU.mult, ALU.mult),
                          ["yc%d" % c, "goc", "mean"], [mixk[c]])


                def attloop(b, i):
                    t0 = i * 128
                    r0 = b * S + t0
                    NK = t0 + 128
                    NKB = i + 1
                    ps_ = i % 2
                    vk = "vsb%d" % i
                    kTk = "kT%d" % i
                    kik = "kiT%d" % i
                    xt = xts[:, ps_]
                    xtk = "xt%d" % ps_
                    mb = mbs[:, ps_]
                    mbk = "mb%d" % ps_
                    qpad = qpads[:, ps_]
                    qpk = "qpad%d" % ps_
                    mixT = mixTs[:, ps_]
                    mixk = ["mix%d_%d" % (ps_, k) for k in range(8)]
                    pvA, pvAk = banks[6], "bk6"
                    pvB, pvBk = banks[7], "bk7"
                    kT_all = ["kT%d" % j for j in range(NKB)]
                    v_all = ["vsb%d" % j for j in range(NKB)]
                    items = [(h, g0) for h in range(8) for g0 in range(0, NKB, 4)]

                    def emit_lg(n):
                        h, g0 = items[n]
                        c = h // 2
                        blks = list(range(g0, min(g0 + 4, NKB)))
                        bl, blk_ = rot.get()

                        def lg(e, bl=bl, blks=blks, h=h, c=c):
                            last = None
                            for n_, kb in enumerate(blks):
                                o = bl[:, n_ * 128:(n_ + 1) * 128]
                                e.matmul(o, kT[:, c, kb * 128:(kb + 1) * 128], qpad[:, h, :],
                                         start=True, stop=False)
                                if kb >= i - 1:
                                    e.matmul(o, ident_b[:], biasTb[:, kb - (i - 1), h, :],
                                             start=False, stop=False)
                                last = e.matmul(o, mb[:, kb * 128:(kb + 1) * 128], ident_b[:],
                                                start=False, stop=True)
                            return last
                        T(lg, kT_all + [qpk, mbk, "ident_b", "biasTb"], [blk_])
                        return bl, blk_, blks

                    nxt = emit_lg(0)
                    for n in range(len(items)):
                        bl, blk_, blks = nxt
                        if n + 1 < len(items):
                            nxt = emit_lg(n + 1)
                        h, g0 = items[n]
                        pv, pvk = (pvA, pvAk) if h < 4 else (pvB, pvBk)
                        hh = h % 4
                        nb_ = len(blks)
                        sl = n % 2
                        A(lambda e, bl=bl, nb_=nb_, sl=sl, h=h: e.activation(
                            Eb[:, sl, 0:nb_ * 128], bl[:, 0:nb_ * 128], AF.Exp,
                            bias=biasfar[:, h:h + 1], scale=1.0), [blk_, "biasfar"], ["Eb%d" % sl])

                        def pvm(e, pv=pv, blks=blks, sl=sl, h=h, hh=hh):
                            last = None
                            for n_, kb in enumerate(blks):
                                last = e.matmul(pv[:, hh * 65:(hh + 1) * 65],
                                                Eb[:, sl, n_ * 128:(n_ + 1) * 128], vsb[:, kb, h, :],
                                                start=(kb == 0), stop=(kb == NKB - 1))
                            return last
                        T(pvm, ["Eb%d" % sl] + v_all, [pvk])
                    return pvA, pvAk, pvB, pvBk

                def backrest(b, i, pvA, pvAk, pvB, pvBk):
                    t0 = i * 128
                    r0 = b * S + t0
                    NK = t0 + 128
                    NKB = i + 1
                    ps_ = i % 2
                    vk = "vsb%d" % i
                    kTk = "kT%d" % i
                    kik = "kiT%d" % i
                    xt = xts[:, ps_]
                    xtk = "xt%d" % ps_
                    mb = mbs[:, ps_]
                    mbk = "mb%d" % ps_
                    qpad = qpads[:, ps_]
                    qpk = "qpad%d" % ps_
                    mixT = mixTs[:, ps_]
                    mixk = ["mix%d_%d" % (ps_, k) for k in range(8)]
                    for hf, (pv, pvk) in enumerate(((pvA, pvAk), (pvB, pvBk))):
                        pv3 = pv[:, 0:260].rearrange("p (h d) -> p h d", d=65)
                        V(lambda e, pv3=pv3, hf=hf: e.reciprocal(sm[:, 8 + hf * 4:12 + hf * 4], pv3[:, :, 64]),
                          [pvk], ["smr%d" % hf])
                        V(lambda e, pv3=pv3, hf=hf: e.tensor_tensor(
                            attn[:, hf * 256:(hf + 1) * 256].rearrange("p (h d) -> p h d", d=64),
                            pv3[:, :, 0:64],
                            sm[:, 8 + hf * 4:12 + hf * 4].unsqueeze(2).to_broadcast([128, 4, 64]),
                            ALU.mult), [pvk, "smr%d" % hf], ["attn%d" % hf])
                    A(lambda e: e.activation(mixT[:, 4:8, :].rearrange("p c t -> p (c t)"), attn[:], AF.Square,
                                             accum_out=sm[:, 2:3]),
                      ["attn0", "attn1"], mixk[4:8] + ["sm2"])
                    rsqrt(sm[:, 3:4], sm[:, 2:3], 1.0 / 512, ["sm2"], ["sm3"])
                    V(lambda e: e.tensor_scalar(Dg[:, 1, :], ident_f[:], sm[:, 3:4], None, ALU.mult),
                      ["ident_f", "sm3"], ["Dg1"])
                    bt, btk = rot.get()

                    def atr(e, bt=bt):
                        last = None
                        for c in range(4):
                            last = e.matmul(bt[:, c * 128:(c + 1) * 128], attn[:, c * 128:(c + 1) * 128],
                                            Dg[:, 1, :], start=True, stop=True)
                        return last
                    T(atr, ["attn0", "attn1", "Dg1"], [btk])
                    for c in range(4):
                        V(lambda e, c=c, bt=bt: e.tensor_scalar(mixT[:, 4 + c, :], bt[:, c * 128:(c + 1) * 128],
                                                                goa[:, c:c + 1], None, ALU.mult),
                          [btk, "goa"], [mixk[4 + c]])

                    for half in range(2):
                        bo, bok = rot.get()

                        def op_(e, bo=bo, half=half):
                            last = None
                            for k in range(8):
                                last = e.matmul(bo[:], mixT[:, k, :], w_out[:, k, half * 512:(half + 1) * 512],
                                                start=(k == 0), stop=(k == 7))
                            return last
                        T(op_, mixk + ["w_out"], [bok])
                        hs = slice(half * 512, (half + 1) * 512)
                        V(lambda e, bo=bo, hs=hs: e.tensor_tensor(xt[:, hs], bo[:], xt[:, hs], ALU.add),
                          [bok, xtk], [xtk])
                    P.dma("sp", out_d[r0:r0 + 128, :], xt, reads=[xtk],
                          writes=["out_g%d" % (r0 // TG)], semkey="st_x1")
                    norm_to_T(xt, 4, 5, a2, sh2, b, h2T, "hT", xtk, 1)
                    P.dma("sp", h2T_s[r0:r0 + 128, :], h2T[:].rearrange("p k t -> p (k t)"), reads=h2Tk,
                          writes=["h2s%d" % (r0 // 128)], semkey="st_h2")


                front(b, 0)
                convblk(b, 0)
                for i in range(NT):
                    if i + 1 < NT:
                        front(b, i + 1)
                    pvs = attloop(b, i)
                    if i + 1 < NT:
                        convblk(b, i + 1)
                    backrest(b, i, *pvs)

        if phase1_only:
            P.final_wait("sp", ["out_g%d" % g for g in range(NG)])
            return nc
        P.barrier()
        with ExitStack() as s2:
            w_pq = sbuf(s2, "w_pq", [128, 8, 2048], BF16)
            with ExitStack() as s2a:
                stg = sbuf(s2a, "stgq", [128, 2, 2048], F32)
                for k in range(8):
                    sl = k % 2
                    P.dma("sp", stg[:, sl, :], wpq_d[k * 128:(k + 1) * 128, :], writes=["stgq%d" % sl])
                    if k % 2 == 0:
                        G(lambda e, k=k, sl=sl: e.tensor_copy(w_pq[:, k, :], stg[:, sl, :]),
                          ["stgq%d" % sl], ["w_pq"])
                    else:
                        A(lambda e, k=k, sl=sl: e.copy(w_pq[:, k, :], stg[:, sl, :]),
                          ["stgq%d" % sl], ["w_pq"])
            P.barrier()
            k1T = sbuf(s2, "k1T", [128, 8, 128], F32)
            k2T = sbuf(s2, "k2T", [128, 8, 128], F32)
            gt2_bc = sbuf(s2, "gt2_bc", [128, NB, D], F32)
            h2 = sbuf(s2, "h2", [128, 2, 8, TG], BF16)
            qqT = sbuf(s2, "qqT", [128, 16, 128], F32)
            sc = sbuf(s2, "sc", [128, 16, 128], F32)
            mx = sbuf(s2, "mx", [128, 16, 16], F32)
            ix = sbuf(s2, "ix", [128, 16, 16], U32)
            ixf = sbuf(s2, "ixf", [128, 16, 16], F32)
            cand2 = sbuf(s2, "cand2", [128, 8, 256], F32)
            best = sbuf(s2, "best", [128, 8, 16], F32)
            pos = sbuf(s2, "pos", [128, 8, 16], U32)
            pab_u = sbuf(s2, "pab_u", [128, 2, 8, 16], U32)
            pa = sbuf(s2, "pa", [128, 8, 16], F32)
            pb = sbuf(s2, "pb", [128, 8, 16], F32)
            IJg = sbuf(s2, "IJg", [128, 3, 128], F32)
            ssum = sbuf(s2, "ssum", [128, 16], F32)
            IJgT = sbuf(s2, "IJgT", [128, 2, 3, TG], BF16)
            OHI = sbuf(s2, "OHI", [128, 2, 8, 128], BF16)
            OHJ = sbuf(s2, "OHJ", [128, 2, 8, 128], BF16)
            OHJg = sbuf(s2, "OHJg", [128, 2, 8, 128], BF16)
            W = sbuf(s2, "W", [128, TG, 128], BF16)
            ub = sbuf(s2, "ub", [128, 2, 8, 512], BF16)
            vbuf = sbuf(s2, "vbuf", [128, 2, 4, D], BF16)
            gel = sbuf(s2, "gel", [128, 2, TG], BF16)
            Z = sbuf(s2, "Z", [128, 3, TG], BF16)
            iota16 = iota_f[:, 0:16]
            sc2 = qqT
            cand = sc[:].rearrange("p m n -> p (m n)").rearrange("p (h q) -> p h q", h=8)
            oh = cand2[:].rearrange("p h (a b) -> p h a b", b=16)
            res = qqT[:].rearrange("p m n -> p (m n)")[:, 0:D]
            xr = cand2[:].rearrange("p h q -> p (h q)")[:, 0:D]

            P.dma("sp", k1T[:], k1T_d[:, :].rearrange("p (h n) -> p h n", h=8), writes=["k1T"])
            P.dma("sp", k2T[:], k2T_d[:, :].rearrange("p (h n) -> p h n", h=8), writes=["k2T"])
            rotw = Rot([4, 5, 6, 7])
            for b in range(NB):
                class _D:
                    pass
                gate_bcast(gt2_bc[:, b, :], gt2, b, "gt2", "gt2_bc", rotw)

            def gating_gen(g):
                gb = g % 2
                r0 = g * TG
                for sub in range(2):
                    rr = r0 + sub * 128
                    P.dma("sp", h2[:, gb, :, sub * 128:(sub + 1) * 128],
                          h2T_s[rr:rr + 128, :].rearrange("p (k t) -> p k t", k=8),
                          reads=["h2s%d" % (rr // 128)], writes=["h2_%d_%d" % (gb, sub)])
                    yield
                for sub in range(2):
                    ts_ = slice(sub * 128, (sub + 1) * 128)
                    for m4 in range(4):
                        bk, bkk = rotw.get()

                        def qm(e, bk=bk, m4=m4, ts_=ts_):
                            last = None
                            for mm_ in range(4):
                                m = m4 * 4 + mm_
                                for k in range(8):
                                    last = e.matmul(bk[:, mm_ * 128:(mm_ + 1) * 128],
                                                    w_pq[:, k, m * 128:(m + 1) * 128], h2[:, gb, k, ts_],
                                                    start=(k == 0), stop=(k == 7))
                            return last
                        T(qm, ["w_pq", "h2_%d_%d" % (gb, sub)], [bkk])
                        yield
                        A(lambda e, bk=bk, m4=m4: e.copy(
                            qqT[:, m4 * 4:(m4 + 1) * 4, :].rearrange("p m t -> p (m t)"), bk[:]),
                          [bkk], ["A"])
                        yield
                    for m4 in range(4):
                        bk, bkk = rotw.get()

                        def sm_(e, bk=bk, m4=m4):
                            last = None
                            for mm_ in range(4):
                                m = m4 * 4 + mm_
                                kt = k1T if m % 2 == 0 else k2T
                                last = e.matmul(bk[:, mm_ * 128:(mm_ + 1) * 128], qqT[:, m, :],
                                                kt[:, m // 2, :], start=True, stop=True)
                            return last
                        T(sm_, ["A", "k1T", "k2T"], [bkk])
                        yield
                        A(lambda e, bk=bk, m4=m4: e.copy(
                            sc[:, m4 * 4:(m4 + 1) * 4, :].rearrange("p m t -> p (m t)"), bk[:]),
                          [bkk], ["B"])
                        yield
                    sck = ["B"]
                    for m in range(16):
                        V(lambda e, m=m: e.max(mx[:, m, 0:8], sc[:, m, :]), sck, ["mx%d" % m])
                        yield
                    for m in range(16):
                        V(lambda e, m=m: e.max_index(ix[:, m, 0:8], mx[:, m, 0:8], sc[:, m, :]),
                          sck + ["mx%d" % m], ["ix%d" % m])
                        yield
                    for m in range(16):
                        V(lambda e, m=m: e.match_replace(sc2[:, m, :], mx[:, m, 0:8], sc[:, m, :], -1e30),
                          sck + ["mx%d" % m], ["A"])
                        yield
                    for m in range(16):
                        V(lambda e, m=m: e.max(mx[:, m, 8:16], sc2[:, m, :]), ["A"], ["mx%d" % m])
                        yield
                    for m in range(16):
                        V(lambda e, m=m: e.max_index(ix[:, m, 8:16], mx[:, m, 8:16], sc2[:, m, :]),
                          ["A", "mx%d" % m], ["ix%d" % m])
                        yield
                    mxk = ["mx%d" % m for m in range(16)]
                    ixk = ["ix%d" % m for m in range(16)]
                    V(lambda e: e.tensor_copy(ixf[:], ix[:]), ixk, ["ixf"])
                    yield
                    mxv = mx[:].rearrange("p (h two) a -> p h two a", two=2)
                    ixv = ixf[:].rearrange("p (h two) a -> p h two a", two=2)
                    V(lambda e: e.tensor_tensor(
                        cand.rearrange("p h (a b) -> p h a b", b=16),
                        mxv[:, :, 0, :].unsqueeze(3).to_broadcast([128, 8, 16, 16]),
                        mxv[:, :, 1, :].unsqueeze(2).to_broadcast([128, 8, 16, 16]), ALU.add),
                      mxk + ixk, ["B"])
                    yield
                    for h in range(8):
                        V(lambda e, h=h: e.max(best[:, h, 0:8], cand[:, h, :]), ["B"], ["best%d" % h])
                        yield
                    for h in range(8):
                        V(lambda e, h=h: e.max_index(pos[:, h, 0:8], best[:, h, 0:8], cand[:, h, :]),
                          ["B", "best%d" % h], ["pos%d" % h])
                        yield
                    for h in range(8):
                        V(lambda e, h=h: e.match_replace(cand2[:, h, :], best[:, h, 0:8], cand[:, h, :], -1e30),
                          ["B", "best%d" % h], ["C"])
                        yield
                    for h in range(8):
                        V(lambda e, h=h: e.max(best[:, h, 8:16], cand2[:, h, :]), ["C"], ["best%d" % h])
                        yield
                    for h in range(8):
                        V(lambda e, h=h: e.max_index(pos[:, h, 8:16], best[:, h, 8:16], cand2[:, h, :]),
                          ["C", "best%d" % h], ["pos%d" % h])
                        yield
                    bestk = ["best%d" % h for h in range(8)]
                    posk = ["pos%d" % h for h in range(8)]
                    gsl = IJg[:, 2, :].rearrange("p (h k) -> p h k", k=16)
                    V(lambda e: e.tensor_tensor(gsl, best[:], best[:, :, 0:1].to_broadcast([128, 8, 16]),
                                                ALU.subtract), bestk, ["gate"])
                    yield
                    A(lambda e: e.activation(gsl, gsl, AF.Exp), ["gate"], ["gate"])
                    yield
                    V(lambda e: e.tensor_reduce(ssum[:, 0:8], gsl, AX.X, ALU.add), ["gate"], ["ssum"])
                    yield
                    V(lambda e: e.reciprocal(ssum[:, 8:16], ssum[:, 0:8]), ["ssum"], ["ssum"])
                    yield
                    V(lambda e: e.tensor_tensor(gsl, gsl, ssum[:, 8:16].unsqueeze(2).to_broadcast([128, 8, 16]),
                                                ALU.mult), ["gate", "ssum"], ["gate"])
                    yield
                    V(lambda e: e.tensor_single_scalar(pab_u[:, 0], pos[:], 4, ALU.logical_shift_right),
                      posk, ["pab_u"])
                    yield
                    V(lambda e: e.tensor_single_scalar(pab_u[:, 1], pos[:], 15, ALU.bitwise_and),
                      posk + ["pab_u"], ["pab_u"])
                    yield
                    V(lambda e: e.tensor_copy(pa[:], pab_u[:, 0]), ["pab_u"], ["pa"])
                    yield
                    V(lambda e: e.tensor_copy(pb[:], pab_u[:, 1]), ["pab_u"], ["pb"])
                    yield
                    i16 = iota16.unsqueeze(1).unsqueeze(1).to_broadcast([128, 8, 16, 16])
                    for which, (pp, ppk) in enumerate(((pa, "pa"), (pb, "pb"))):
                        V(lambda e, pp=pp: e.tensor_tensor(oh, pp[:].unsqueeze(3).to_broadcast([128, 8, 16, 16]),
                                                           i16, ALU.is_equal), [ppk, "iota"], ["C"])
                        yield
                        V(lambda e, which=which: e.tensor_tensor(
                            oh, oh, ixv[:, :, which, :].unsqueeze(2).to_broadcast([128, 8, 16, 16]),
                            ALU.mult), ["C", "ixf"], ["C"])
                        yield
                        V(lambda e, which=which: e.tensor_reduce(
                            IJg[:, which, :].rearrange("p (h k) -> p h k", k=16), oh, AX.X, ALU.add),
                          ["C"], ["IJ%d" % which])
                        yield
                    bk, bkk = rotw.get()

                    def ijt(e, bk=bk):
                        last = None
                        for w_ in range(3):
                            last = e.transpose(bk[:, w_ * 128:(w_ + 1) * 128], IJg[:, w_, :], ident_f[:])
                        return last
                    T(ijt, ["IJ0", "IJ1", "gate", "ident_f"], [bkk])
                    yield
                    A(lambda e, bk=bk, ts_=ts_: e.copy(IJgT[:, gb, :, ts_], bk[:, 0:384].rearrange("p (w t) -> p w t", w=3)),
                      [bkk], ["IJgT%d_%d" % (gb, sub)])
                    yield

            def wbuild(g):
                gb = g % 2
                for sub in range(2):
                    for q in range(16):
                        tq = sub * 128 + q * 8
                        sl = q % 2
                        ik = "IJgT%d_%d" % (gb, sub)
                        iob = iota_b[:].unsqueeze(1).to_broadcast([128, 8, 128])
                        V(lambda e, sl=sl, tq=tq: e.tensor_tensor(
                            OHI[:, sl], iob, IJgT[:, gb, 0, tq:tq + 8].unsqueeze(2).to_broadcast([128, 8, 128]),
                            ALU.is_equal), [ik, "iota_b"], ["OHI%d" % sl])
                        V(lambda e, sl=sl, tq=tq: e.tensor_tensor(
                            OHJ[:, sl], iob, IJgT[:, gb, 1, tq:tq + 8].unsqueeze(2).to_broadcast([128, 8, 128]),
                            ALU.is_equal), [ik, "iota_b"], ["OHJ%d" % sl])
                        V(lambda e, sl=sl, tq=tq: e.tensor_tensor(
                            OHJg[:, sl], OHJ[:, sl],
                            IJgT[:, gb, 2, tq:tq + 8].unsqueeze(2).to_broadcast([128, 8, 128]),
                            ALU.mult), [ik, "OHJ%d" % sl], ["OHJg%d" % sl])
                        for q4 in range(2):
                            bk, bkk = rotw.get()

                            def wm(e, bk=bk, sl=sl, q4=q4):
                                last = None
                                for tt in range(4):
                                    t_ = q4 * 4 + tt
                                    last = e.matmul(bk[:, tt * 128:(tt + 1) * 128], OHJg[:, sl, t_, :],
                                                    OHI[:, sl, t_, :], start=True, stop=True)
                                return last
                            T(wm, ["OHJg%d" % sl, "OHI%d" % sl], [bkk])
                            A(lambda e, bk=bk, tq=tq, q4=q4: e.copy(
                                W[:, tq + q4 * 4:tq + q4 * 4 + 4, :].rearrange("p t i -> p (t i)"), bk[:]),
                              [bkk], ["W"])

            gq = gating_gen(0)
            for _ in gq:
                pass
            for g in range(NG):
                r0 = g * TG
                b = r0 // S
                gb = g % 2
                h2k = ["h2_%d_0" % gb, "h2_%d_1" % gb]
                wbuild(g)
                gq = gating_gen(g + 1) if g + 1 < NG else iter(())
                LA = 2
                GI = 5
                pend = []
                for ci in range(NEXP_CH + LA):
                    if ci < NEXP_CH:
                        cg, cc = divmod(ci, 4)
                        sl = cg % 2
                        if cc == 0:
                            e0 = cg * 512
                            P.dma("sp", ub[:, sl], uTb_s[:, e0:e0 + 512].rearrange("(k p) e -> p k e", p=128),
                                  reads=["uTb_s"], writes=["ub%d" % sl])
                            P.dma("sp", vbuf[:, sl], vb_s[e0:e0 + 512, :].rearrange("(c j) d -> j c d", j=128),
                                  reads=["vb_s"], writes=["vbuf%d" % sl])
                        by, byk = rotw.get()

                        def ym(e, by=by, sl=sl, cc=cc, gb=gb):
                            last = None
                            for k in range(8):
                                last = e.matmul(by[:, 0:TG], ub[:, sl, k, cc * 128:(cc + 1) * 128], h2[:, gb, k, :],
                                                start=(k == 0), stop=(k == 7))
                            return last
                        T(ym, ["ub%d" % sl] + h2k, [byk])
                        gs = ci % 2
                        zs = ci % 3
                        A(lambda e, by=by, gs=gs: e.activation(gel[:, gs, :], by[:, 0:TG], AF.Gelu_apprx_tanh),
                          [byk], ["gel%d" % gs])
                        V(lambda e, zs=zs, gs=gs, ci=ci: e.tensor_tensor(Z[:, zs, :], gel[:, gs, :], W[:, :, ci],
                                                                         ALU.mult),
                          ["gel%d" % gs, "W"], ["Z%d" % zs])

                        def vm(e, zs=zs, sl=sl, cc=cc, ci=ci):
                            last = None
                            for sub in range(2):
                                for half in range(2):
                                    last = e.matmul(banks[sub * 2 + half][:],
                                                    Z[:, zs, sub * 128:(sub + 1) * 128],
                                                    vbuf[:, sl, cc, half * 512:(half + 1) * 512],
                                                    start=(ci == 0), stop=(ci == NEXP_CH - 1))
                            return last
                        pend.append((vm, ["Z%d" % zs, "vbuf%d" % sl]))
                    if ci >= LA:
                        vm_, rk_ = pend.pop(0)
                        T(vm_, rk_, ["bk0", "bk1", "bk2", "bk3"])
                    for _ in range(GI):
                        next(gq, None)
                for _ in gq:
                    pass
                for sub in range(2):
                    rr = r0 + sub * 128
                    P.dma("sp", xr, out_d[rr:rr + 128, :], reads=["out_g%d" % g], writes=["C"])
                    for half in range(2):
                        hs = slice(half * 512, (half + 1) * 512)
                        V(lambda e, sub=sub, half=half, hs=hs: e.tensor_tensor(
                            res[:, hs], banks[sub * 2 + half][:], gt2_bc[:, b, hs], ALU.mult),
                          ["bk%d" % (sub * 2 + half), "gt2_bc", "A"], ["A"])
                    G(lambda e: e.tensor_tensor(res, res, xr, ALU.add), ["A", "C"], ["A"])
                    P.dma("sp", out_d[rr:rr + 128, :], res, reads=["A"], writes=["out_f"], semkey="st_res")
            P.final_wait("sp", ["out_f"])
        P.emit()
        print("instructions:", P.n_instr, {e: len(P.ops[e]) for e in ENGS}, "dma sems:", len(P.dsem))
    return nc


def _t5_bucket_table():
    import jax
    import jax.numpy as jnp
    with jax.default_device(jax.devices("cpu")[0]):
        return _t5_bucket_table_impl(jnp)


def _t5_bucket_table_impl(jnp):
    rel = jnp.arange(-255, 128, dtype=jnp.int32)
    half = 16
    max_exact = 8
    ret = jnp.where(rel > 0, half, 0)
    n = jnp.abs(rel)
    nf = jnp.maximum(n, 1).astype(jnp.float32)
    large = max_exact + (jnp.log(nf / max_exact) / math.log(128 / max_exact)
                         * (half - max_exact)).astype(jnp.int32)
    large = jnp.minimum(large, half - 1)
    return np.asarray(ret + jnp.where(n < max_exact, n, large))


def fm(vec, nchunk):
    return np.ascontiguousarray(np.asarray(vec, np.float32).reshape(nchunk, 128).T)


def prep_shared(inp):
    f = lambda a: np.ascontiguousarray(np.asarray(a, np.float32))
    bt = _t5_bucket_table()
    rb = f(inp["rel_bias"])
    ii = np.arange(128)
    biasT = np.empty((128, 2, 8, 128), np.float32)
    for blk in range(2):
        rel = (blk - 1) * 128 + ii[:, None] - ii[None, :]
        biasT[:, blk] = np.transpose(rb[bt[rel + 255]], (0, 2, 1))
    far = rb[15]
    sh = {
        "w_ada": f(inp["w_ada"][0]),
        "b_ada": fm(inp["b_ada"][0], 48),
        "g1": fm(inp["g_norm1"][0], 8),
        "g2": fm(inp["g_norm2"][0], 8),
        "w_in": f(inp["w_in"][0]),
        "qg": np.ascontiguousarray(np.tile(f(inp["q_norm_g"][0]), 2)[:, None]),
        "kg": np.ascontiguousarray(np.tile(f(inp["k_norm_g"][0]), 2)[:, None]),
        "cw": np.ascontiguousarray(np.transpose(f(inp["conv_w"][0])[:, 0, :].reshape(31, 4, 128), (2, 1, 0)).reshape(128, 124)),
        "cb": fm(inp["conv_b"][0], 4),
        "clg": fm(inp["conv_ln_g"][0], 4),
        "clb": fm(inp["conv_ln_b"][0], 4),
        "goc": fm(inp["g_out_conv"][0], 4),
        "goa": fm(inp["g_out_attn"][0], 4),
        "biasT": np.ascontiguousarray(biasT.reshape(128, -1)),
        "biasfar": np.ascontiguousarray(np.broadcast_to(far[None, :], (128, 8))),
        "w_out": f(inp["w_out"][0]),
        "w_pq": f(inp["w_peer_q"][0]),
        "k1T": np.ascontiguousarray(np.transpose(f(inp["peer_k1"][0]), (2, 0, 1)).reshape(128, 1024)),
        "k2T": np.ascontiguousarray(np.transpose(f(inp["peer_k2"][0]), (2, 0, 1)).reshape(128, 1024)),
        "uT": np.ascontiguousarray(f(inp["peer_u"][0]).T),
        "vtab": f(inp["peer_v"][0]),
    }
    return sh


def prep_core(x, c, NB):
    S = x.shape[1]
    cT = np.ascontiguousarray(np.transpose(np.asarray(c, np.float32).reshape(NB, 8, 128), (2, 1, 0)).reshape(128, 8 * NB))
    return {"x": np.ascontiguousarray(np.asarray(x, np.float32).reshape(NB * S, D)), "cT": cT}


N_CORES = 8


def kernel(**inputs):
    x = np.asarray(inputs["x"])
    c = np.asarray(inputs["c"])
    B, S, _ = x.shape
    NB = B // N_CORES
    NSEL = min(256, S // 4)
    nc = build(S, NB, NSEL)
    sh = prep_shared(inputs)
    in_maps = []
    for i in range(N_CORES):
        m = dict(sh)
        m.update(prep_core(x[i * NB:(i + 1) * NB], c[i * NB:(i + 1) * NB], NB))
        in_maps.append(m)
    res = run_bass_kernel_spmd(nc, in_maps, core_ids=list(range(N_CORES)))
    out = np.concatenate([np.asarray(r["out"]).reshape(NB, S, D) for r in res.results], axis=0)
    return out.astype(np.float32)
```

```python
import math
from contextlib import ExitStack

import numpy as np
import concourse.bass as bass
import concourse.mybir as mybir
from concourse.bass_utils import run_bass_kernel_spmd

F32 = mybir.dt.float32
BF16 = mybir.dt.bfloat16
U32 = mybir.dt.uint32
ALU = mybir.AluOpType
AF = mybir.ActivationFunctionType
AX = mybir.AxisListType

D = 1024
NCOL = 2884
EPS = 1e-6
NEXP_CH = 128
NEG = -30000.0
ACT_EVAC = False
ENGS = ("pe", "act", "dve", "pool", "sp")


class Prog:
    def __init__(self, nc, stack):
        self.nc = nc
        self.stack = stack
        self.ops = {e: [] for e in ENGS}
        self.cnt = {e: 0 for e in ENGS}
        self.sem = {e: stack.enter_context(nc.semaphore("s_" + e)) for e in ENGS}
        self.seen = {e: {} for e in ENGS}
        self.dsem = {}
        self.lastw = {}
        self.readers = {}
        self.n_instr = 0
        self.engs = {"pe": nc.tensor, "act": nc.scalar, "dve": nc.vector,
                     "pool": nc.gpsimd, "sp": nc.sync}

    def _need(self, e, tok, waits):
        if tok is None:
            return
        sem, val, name = tok
        if e == "pe" and name == "s_pe":
            return
        if self.seen[e].get(name, 0) >= val:
            return
        prev = waits.get(name)
        if prev is None or prev[1] < val:
            waits[name] = (sem, val)

    def _deps(self, e, reads, writes):
        waits = {}
        for k in reads:
            self._need(e, self.lastw.get(k), waits)
        for k in writes:
            self._need(e, self.lastw.get(k), waits)
            for t in self.readers.get(k, {}).values():
                self._need(e, t, waits)
        for name, (sem, val) in waits.items():
            self.seen[e][name] = val
        return list(waits.values())

    def _commit(self, tok, reads, writes):
        for k in writes:
            self.lastw[k] = tok
            self.readers[k] = {}
        for k in reads:
            if k in writes:
                continue
            d = self.readers.setdefault(k, {})
            old = d.get(tok[2])
            if old is None or old[1] < tok[1]:
                d[tok[2]] = tok

    def op(self, e, fn, reads=(), writes=()):
        waits = self._deps(e, reads, writes)
        self.cnt[e] += 1
        val = self.cnt[e]
        sem = self.sem[e]

        def thunk(eng, fn=fn, waits=waits, sem=sem):
            for (s, v) in waits:
                eng.wait_ge(s, v)
            fn(eng).then_inc(sem, 1)

        thunk(self.engs[e])
        self.ops[e].append(1)
        self._commit((sem, val, "s_" + e), reads, writes)
        self.n_instr += 1

    def dma(self, q, out, in_, reads=(), writes=(), semkey=None):
        if semkey is None:
            semkey = writes[0]
        if semkey not in self.dsem:
            self.dsem[semkey] = [self.stack.enter_context(
                self.nc.semaphore("d_%d" % len(self.dsem))), 0]
        waits = self._deps(q, reads, writes)
        ent = self.dsem[semkey]
        ent[1] += 16
        sem, val = ent[0], ent[1]

        def thunk(eng, waits=waits, sem=sem, out=out, in_=in_):
            for (s, v) in waits:
                eng.wait_ge(s, v)
            eng.dma_start(out=out, in_=in_).then_inc(sem, 16)

        thunk(self.engs[q])
        self.ops[q].append(1)
        self._commit((sem, val, "d_" + str(semkey)), reads, writes)
        self.n_instr += 1

    def barrier(self):
        toks = [(self.sem[e], self.cnt[e], "s_" + e) for e in ENGS if self.cnt[e] > 0]
        toks += [(ent[0], ent[1], "d_" + str(k)) for k, ent in self.dsem.items()
                 if k not in ("uTb_s", "vb_s")]
        for e in ENGS:
            waits = {}
            for t in toks:
                self._need(e, t, waits)
            for name, (sem, val) in waits.items():
                self.seen[e][name] = val
                self.engs[e].wait_ge(sem, val)

    def final_wait(self, e, keys):
        waits = self._deps(e, keys, ())

        def thunk(eng, waits=waits):
            for (s, v) in waits:
                eng.wait_ge(s, v)

        thunk(self.engs[e])

    def emit(self):
        return
        nc = self.nc
        with nc.Block() as block:
            @block.tensor
            def _(eng):
                for t in self.ops["pe"]:
                    t(eng)

            @block.scalar
            def _(eng):
                for t in self.ops["act"]:
                    t(eng)

            @block.vector
            def _(eng):
                for t in self.ops["dve"]:
                    t(eng)

            @block.gpsimd
            def _(eng):
                for t in self.ops["pool"]:
                    t(eng)

            @block.sync
            def _(eng):
                for t in self.ops["sp"]:
                    t(eng)


class _Stop(Exception):
    pass


def build(S, NB, NSEL, n_iter=28, phase1_only=False, stop_at=None):
    holder = {}
    try:
        return _build(S, NB, NSEL, n_iter, phase1_only, stop_at, holder)
    except _Stop:
        P = holder["P"]
        P.barrier()
        return holder["nc"]


def _build(S, NB, NSEL, n_iter, phase1_only, stop_at, holder):
    NT = S // 128
    TOK = NB * S
    NTT = TOK // 128
    TG = 256
    NG = TOK // TG
    nc = bass.Bass("TRN2", target_bir_lowering=False)
    holder["nc"] = nc

    def mark(name):
        if stop_at == name:
            raise _Stop()

    def din(name, shape, dt=F32):
        return nc.dram_tensor(name, shape, dt, kind="ExternalInput").ap()

    x_d = din("x", [TOK, D])
    cT_d = din("cT", [128, 8 * NB])
    wada_d = din("w_ada", [D, 6 * D])
    bada_d = din("b_ada", [128, 48])
    g1_d = din("g1", [128, 8])
    g2_d = din("g2", [128, 8])
    win_d = din("w_in", [D, NCOL])
    qg_d = din("qg", [128, 1])
    kg_d = din("kg", [128, 1])
    cw_d = din("cw", [128, 4 * 31])
    cb_d = din("cb", [128, 4])
    clg_d = din("clg", [128, 4])
    clb_d = din("clb", [128, 4])
    goc_d = din("goc", [128, 4])
    goa_d = din("goa", [128, 4])
    biasT_d = din("biasT", [128, 2 * 8 * 128])
    biasfar_d = din("biasfar", [128, 8])
    wout_d = din("w_out", [D, D])
    wpq_d = din("w_pq", [D, 2048])
    k1T_d = din("k1T", [128, 8 * 128])
    k2T_d = din("k2T", [128, 8 * 128])
    uT_d = din("uT", [D, 16384])
    v_d = din("vtab", [16384, D])
    out_d = nc.dram_tensor("out", [TOK, D], F32, kind="ExternalOutput").ap()
    h2T_s = nc.dram_tensor("h2T_s", [TOK, D], BF16, kind="Internal").ap()
    uTb_s = nc.dram_tensor("uTb_s", [D, 16384], BF16, kind="Internal").ap()
    vb_s = nc.dram_tensor("vb_s", [16384, D], BF16, kind="Internal").ap()

    with ExitStack() as st:
        P = Prog(nc, st)
        holder["P"] = P

        def V(fn, r=(), w=()):
            P.op("dve", fn, r, w)

        def A(fn, r=(), w=()):
            P.op("act", fn, r, w)

        def G(fn, r=(), w=()):
            P.op("pool", fn, r, w)

        def T(fn, r=(), w=()):
            P.op("pe", fn, r, w)

        def sbuf(stack, name, shape, dt):
            return stack.enter_context(nc.sbuf_tensor("sb_" + name, shape, dt))

        banks = [st.enter_context(nc.psum_tensor("bk%d" % i, [128, 512], F32)) for i in range(8)]

        class Rot:
            def __init__(self, idxs):
                self.idxs = idxs
                self.n = 0

            def get(self):
                i = self.idxs[self.n % len(self.idxs)]
                self.n += 1
                return banks[i], "bk%d" % i

        ident_f = sbuf(st, "ident_f", [128, 128], F32)
        ident_b = sbuf(st, "ident_b", [128, 128], BF16)
        ones_f = sbuf(st, "ones_f", [128, 128], F32)
        bones_f = sbuf(st, "bones_f", [128, 128], F32)
        iota_f = sbuf(st, "iota_f", [128, 128], F32)
        iota_b = sbuf(st, "iota_b", [128, 128], BF16)
        pid_f = sbuf(st, "pid_f", [128, 1], F32)
        epsc = sbuf(st, "epsc", [128, 1], F32)
        a1 = sbuf(st, "a1", [128, NB, 8], F32)
        sh1 = sbuf(st, "sh1", [128, NB, 8], F32)
        gt1 = sbuf(st, "gt1", [128, NB, 8], F32)
        a2 = sbuf(st, "a2", [128, NB, 8], F32)
        sh2 = sbuf(st, "sh2", [128, NB, 8], F32)
        gt2 = sbuf(st, "gt2", [128, NB, 8], F32)
        g1 = sbuf(st, "g1", [128, 8], F32)
        g2 = sbuf(st, "g2", [128, 8], F32)

        G(lambda e: e.iota(iota_f[:], pattern=[[1, 128]], base=0, channel_multiplier=0,
                           allow_small_or_imprecise_dtypes=True), w=["iota"])
        G(lambda e: e.iota(pid_f[:], pattern=[[0, 1]], base=0, channel_multiplier=1,
                           allow_small_or_imprecise_dtypes=True), w=["pid"])
        V(lambda e: e.tensor_scalar(ident_f[:], iota_f[:], pid_f[:, 0:1], None, ALU.is_equal),
          ["iota", "pid"], ["ident_f"])
        V(lambda e: e.tensor_copy(ident_b[:], ident_f[:]), ["ident_f"], ["ident_b"])
        V(lambda e: e.tensor_copy(iota_b[:], iota_f[:]), ["iota"], ["iota_b"])
        V(lambda e: e.memset(ones_f[:], 1.0), w=["ones_f"])
        V(lambda e: e.memset(bones_f[:], 0.0), w=["bones_f"])
        V(lambda e: e.memset(bones_f[0:64, 0:64], 1.0), ["bones_f"], ["bones_f"])
        V(lambda e: e.memset(bones_f[64:128, 64:128], 1.0), ["bones_f"], ["bones_f"])
        V(lambda e: e.memset(epsc[:], EPS), w=["epsc"])
        P.dma("sp", g1[:], g1_d[:, :], writes=["g1"])
        P.dma("sp", g2[:], g2_d[:, :], writes=["g2"])
        for (src, dst, key) in ((uT_d, uTb_s, "uTb_s"), (v_d, vb_s, "vb_s")):
            sv = src.rearrange("(p a) e -> p (a e)", p=128)
            dv = dst.rearrange("(p a) e -> p (a e)", p=128)
            for pc in range(16):
                cs = slice(pc * 8192, (pc + 1) * 8192)
                P.dma("pool", dv[:, cs], sv[:, cs], writes=[key])

        def rsqrt(out, src, scale, rk, wk):
            np_ = out.shape[0]
            A(lambda e: e.activation(out, src, AF.Sqrt, bias=epsc[0:np_, 0:1], scale=scale),
              list(rk) + ["epsc"], wk)
            V(lambda e: e.reciprocal(out, out), wk, wk)

        with ExitStack() as s0:
            cT = sbuf(s0, "cT", [128, 8 * NB], F32)
            scs = sbuf(s0, "scs", [128, 8 * NB], F32)
            bada = sbuf(s0, "bada", [128, 48], F32)
            mod = sbuf(s0, "mod", [128, 48, NB], F32)
            stg = sbuf(s0, "stg", [128, 2, 8, 1024], F32)
            P.dma("sp", cT[:], cT_d[:, :], writes=["cT"])
            P.dma("sp", bada[:], bada_d[:, :], writes=["bada"])
            A(lambda e: e.activation(scs[:], cT[:], AF.Silu), ["cT"], ["scs"])
            rot = Rot(list(range(8)))
            for fp in range(6):
                sl = fp % 2
                P.dma("sp", stg[:, sl], wada_d[:, fp * 1024:(fp + 1) * 1024].rearrange(
                    "(k p) f -> p k f", p=128), writes=["stg%d" % sl])
                bk, bkk = rot.get()

                def mm(e, sl=sl, bk=bk):
                    last = None
                    for fc in range(8):
                        for k in range(8):
                            last = e.matmul(bk[:, fc * NB:(fc + 1) * NB],
                                            stg[:, sl, k, fc * 128:(fc + 1) * 128],
                                            scs[:, k * NB:(k + 1) * NB],
                                            start=(k == 0), stop=(k == 7))
                    return last
                T(mm, ["stg%d" % sl, "scs"], [bkk])
                for b in range(NB):
                    V(lambda e, b=b, bk=bk, fp=fp: e.tensor_tensor(
                        mod[:, fp * 8:(fp + 1) * 8, b],
                        bk[:, 0:8 * NB].rearrange("p (f b) -> p f b", b=NB)[:, :, b],
                        bada[:, fp * 8:(fp + 1) * 8], ALU.add), [bkk, "bada"], ["mod"])
            for b in range(NB):
                V(lambda e, b=b: e.scalar_tensor_tensor(a1[:, b, :], mod[:, 8:16, b], 1.0, g1[:],
                                                        ALU.add, ALU.mult), ["mod", "g1"], ["a1"])
                V(lambda e, b=b: e.scalar_tensor_tensor(a2[:, b, :], mod[:, 32:40, b], 1.0, g2[:],
                                                        ALU.add, ALU.mult), ["mod", "g2"], ["a2"])
                V(lambda e, b=b: e.tensor_copy(sh1[:, b, :], mod[:, 0:8, b]), ["mod"], ["sh1"])
                V(lambda e, b=b: e.tensor_copy(gt1[:, b, :], mod[:, 16:24, b]), ["mod"], ["gt1"])
                V(lambda e, b=b: e.tensor_copy(sh2[:, b, :], mod[:, 24:32, b]), ["mod"], ["sh2"])
                V(lambda e, b=b: e.tensor_copy(gt2[:, b, :], mod[:, 40:48, b]), ["mod"], ["gt2"])

        mark("adaln")
        P.barrier()

        def gate_bcast(dst, gt, b, gkey, dkey, rot):
            for half in range(2):
                bk, bkk = rot.get()
                for kk in range(4):
                    k = half * 4 + kk
                    V(lambda e, k=k: e.tensor_scalar(rep[:], ones_f[:], gt[:, b, k:k + 1], None,
                                                     ALU.mult), ["ones_f", gkey], ["rep"])
                    T(lambda e, kk=kk, bk=bk: e.matmul(bk[:, kk * 128:(kk + 1) * 128], rep[:],
                                                       ident_f[:], start=True, stop=True),
                      ["rep", "ident_f"], [bkk])
                V(lambda e, half=half, bk=bk: e.tensor_copy(dst[:, half * 512:(half + 1) * 512],
                                                            bk[:]), [bkk], [dkey])

        rep = sbuf(st, "rep", [128, 128], F32)

        with ExitStack() as s1:
            w_in = sbuf(s1, "w_in", [128, 8, NCOL], BF16)
            w_out = sbuf(s1, "w_out", [128, 8, D], BF16)
            kT = sbuf(s1, "kT", [128, 4, S], BF16)
            vsb = sbuf(s1, "vsb", [128, NT, 8, 65], BF16)
            kiT = sbuf(s1, "kiT", [64, S], BF16)
            idx = sbuf(s1, "idx", [128, max(S, NCOL)], F32)
            mbs = sbuf(s1, "mbs", [128, 2, S], BF16)
            xts = sbuf(s1, "xts", [128, 2, D], F32)
            Dg = sbuf(s1, "Dg", [128, 2, 128], F32)
            gt1_bc = xts[:, 1]
            hT = sbuf(s1, "hT", [128, 8, 128], BF16)
            h2T = hT
            mixTs = sbuf(s1, "mixTs", [128, 2, 8, 128], BF16)
            ubuf = sbuf(s1, "ubuf", [128, 4, 158], F32)
            yc = sbuf(s1, "yc", [128, 4, 128], F32)
            t2 = yc
            mean = sbuf(s1, "mean", [128, 128], F32)
            var = sbuf(s1, "var", [128, 128], F32)
            r3 = mean
            sq = sbuf(s1, "sq", [128, 512], F32)
            ysq = sq[:].rearrange("p (c t) -> p c t", c=4)
            rl = sq[:].rearrange("p (o t) -> p o t", o=1)
            zc = yc
            rq = sbuf(s1, "rq", [128, 512], F32)
            print("phase1 sbuf bytes free:", nc.sbuf_bytes_remaining)
            qn = sq
            qpads = sbuf(s1, "qpads", [128, 2, 8, 128], BF16)
            qiT = sbuf(s1, "qiT", [64, 4, 128], BF16)
            Eb = sbuf(s1, "Eb", [128, 2, 512], BF16)
            attn = sbuf(s1, "attn", [128, 512], F32)
            biasTb = sbuf(s1, "biasTb", [128, 2, 8, 128], BF16)
            biasfar = sbuf(s1, "biasfar", [128, 8], F32)
            sm = sbuf(s1, "sm", [128, 16], F32)
            wi = sbuf(s1, "wi", [128, 4], F32)
            bis = sbuf(s1, "bis", [128, 8], F32)
            pw2 = sbuf(s1, "pw2", [128, 32], F32)
            bws = sbuf(s1, "bws", [128, 32], F32)
            qg = sbuf(s1, "qg", [128, 1], F32)
            kg = sbuf(s1, "kg", [128, 1], F32)
            cw = sbuf(s1, "cw", [128, 4, 31], F32)
            cb = sbuf(s1, "cb", [128, 4], F32)
            clg = sbuf(s1, "clg", [128, 4], F32)
            clb = sbuf(s1, "clb", [128, 4], F32)
            goc = sbuf(s1, "goc", [128, 4], F32)
            goa = sbuf(s1, "goa", [128, 4], F32)

            for nm, t, dsrc in (("qg", qg, qg_d), ("kg", kg, kg_d), ("cb", cb, cb_d),
                                ("clg", clg, clg_d), ("clb", clb, clb_d), ("goc", goc, goc_d),
                                ("goa", goa, goa_d), ("biasfar", biasfar, biasfar_d)):
                P.dma("sp", t[:], dsrc[:, :], writes=[nm])
            P.dma("sp", cw[:], cw_d[:, :].rearrange("p (c j) -> p c j", j=31), writes=["cw"])
            V(lambda e: e.tensor_scalar(qg[:], qg[:], 0.125, None, ALU.mult), ["qg"], ["qg"])
            for blk in range(2):
                P.dma("sp", idx[:, 0:1024], biasT_d[:, blk * 1024:(blk + 1) * 1024], writes=["idx"])
                for h in range(8):
                    V(lambda e, h=h, blk=blk: e.tensor_scalar(
                        biasTb[:, blk, h, :], idx[:, h * 128:(h + 1) * 128],
                        biasfar[:, h:h + 1], None, ALU.subtract), ["idx", "biasfar"], ["biasTb"])
            V(lambda e: e.memset(qpads[:], 0.0), w=["qpad0", "qpad1"])
            for k_ in range(32):
                V(lambda e, k_=k_: e.memset(pw2[:, k_:k_ + 1], 2.0 ** (-k_)), ["pw2"] if k_ else [], ["pw2"])
            V(lambda e: e.memset(vsb[:], 1.0), w=["vsb%d" % j for j in range(NT)])

            for k in range(8):
                P.dma("sp", idx[:, 0:NCOL], win_d[k * 128:(k + 1) * 128, :], writes=["idx"])
                if k % 2 == 0:
                    G(lambda e, k=k: e.tensor_copy(w_in[:, k, :], idx[:, 0:NCOL]), ["idx"], ["w_in"])
                else:
                    A(lambda e, k=k: e.copy(w_in[:, k, :], idx[:, 0:NCOL]), ["idx"], ["w_in"])
            mark("setup1")
            P.barrier()
            rot = Rot(list(range(6)))
            tb_eps = -(2.0 ** -20)

            def norm_to_T(src, ssq_col, r_col, ak, shk, b, dstT, dkey, skey, dsl):
                dks = ["%s%d" % (dkey, k) for k in range(8)]
                A(lambda e: e.activation(dstT[:].rearrange("p k t -> p (k t)"), src, AF.Square,
                                         accum_out=sm[:, ssq_col:ssq_col + 1]),
                  [skey], dks + ["sm%d" % ssq_col])
                rsqrt(sm[:, r_col:r_col + 1], sm[:, ssq_col:ssq_col + 1], 1.0 / D,
                      ["sm%d" % ssq_col], ["sm%d" % r_col])
                V(lambda e: e.tensor_scalar(Dg[:, dsl, :], ident_f[:], sm[:, r_col:r_col + 1], None, ALU.mult),
                  ["ident_f", "sm%d" % r_col], ["Dg%d" % dsl])
                for half in range(2):
                    bk, bkk = rot.get()

                    def tr(e, half=half, bk=bk):
                        last = None
                        for kk in range(4):
                            k = half * 4 + kk
                            last = e.matmul(bk[:, kk * 128:(kk + 1) * 128], src[:, k * 128:(k + 1) * 128],
                                            Dg[:, dsl, :], start=True, stop=True)
                        return last
                    T(tr, [skey, "Dg%d" % dsl], [bkk])
                    for kk in range(4):
                        k = half * 4 + kk
                        V(lambda e, k=k, kk=kk, bk=bk: e.tensor_scalar(
                            dstT[:, k, :], bk[:, kk * 128:(kk + 1) * 128],
                            ak[:, b, k:k + 1], shk[:, b, k:k + 1], ALU.mult, ALU.add),
                          [bkk, "a1", "a2", "sh1", "sh2"], ["%s%d" % (dkey, k)])

            hTk = ["hT%d" % k for k in range(8)]
            h2Tk = ["hT%d" % k for k in range(8)]
            mixk = ["mix%d" % k for k in range(8)]

            for b in range(NB):
                gate_bcast(gt1_bc, gt1, b, "gt1", "xt1", rot)
                for k in range(8):
                    P.dma("sp", idx[:, 0:D], wout_d[k * 128:(k + 1) * 128, :], writes=["idx"])
                    (V if k % 2 == 0 else G)(lambda e, k=k: e.tensor_tensor(
                        w_out[:, k, :], idx[:, 0:D], gt1_bc, ALU.mult), ["idx", "xt1"], ["w_out"])
                mark("wout%d" % b)
                def front(b, i):
                    t0 = i * 128
                    r0 = b * S + t0
                    NK = t0 + 128
                    NKB = i + 1
                    ps_ = i % 2
                    vk = "vsb%d" % i
                    kTk = "kT%d" % i
                    kik = "kiT%d" % i
                    xt = xts[:, ps_]
                    xtk = "xt%d" % ps_
                    mb = mbs[:, ps_]
                    mbk = "mb%d" % ps_
                    qpad = qpads[:, ps_]
                    qpk = "qpad%d" % ps_
                    mixT = mixTs[:, ps_]
                    mixk = ["mix%d_%d" % (ps_, k) for k in range(8)]
                    P.dma("sp", xt, x_d[r0:r0 + 128, :], writes=[xtk])
                    norm_to_T(xt, 0, 1, a1, sh1, b, hT, "hT", xtk, 0)
                    def proj_fm(bk, col0, ncols, ncolblk, M):
                        def f(e):
                            last = None
                            for j in range(ncolblk):
                                for k in range(8):
                                    last = e.matmul(bk[0:M, j * 128:(j + 1) * 128],
                                                    w_in[:, k, col0 + j * M:col0 + (j + 1) * M],
                                                    hT[:, k, :], start=(k == 0), stop=(k == 7))
                            return last
                        return f

                    def qknorm(bsrc, bsrck, gain, gk, is_q):
                        A(lambda e: e.activation(sq[:], bsrc[:], AF.Square), [bsrck], ["sq", "rl0", "rl1"])
                        bn, bnk = rot.get()
                        T(lambda e: e.matmul(bn[:], bones_f[:], sq[:], start=True, stop=True),
                          ["sq", "bones_f"], [bnk])
                        rsqrt(rq[:], bn[:], 1.0 / 64, [bnk], ["rq"])
                        V(lambda e: e.scalar_tensor_tensor(qn[:], bsrc[:], gain[:, 0:1], rq[:],
                                                           ALU.mult, ALU.mult), [bsrck, gk, "rq", "sq"], ["sq"])
                        if is_q:
                            for eh in range(2):
                                ps = slice(eh * 64, (eh + 1) * 64)
                                G(lambda e, ps=ps, eh=eh: e.tensor_copy(
                                    qpad[ps].rearrange("p (c two) t -> p c two t", two=2)[:, :, eh, :],
                                    qn[ps, :].rearrange("p (c t) -> p c t", c=4)), ["sq"], [qpk])
                        else:
                            G(lambda e: e.tensor_copy(kT[:, :, t0:t0 + 128],
                                                      qn[:].rearrange("p (c t) -> p c t", c=4)),
                              ["sq"], [kTk])
                    bcv, bcvk = rot.get()
                    T(proj_fm(bcv, 0, 512, 4, 128), hTk + ["w_in"], [bcvk])
                    bcg, bcgk = rot.get()
                    T(proj_fm(bcg, 512, 512, 4, 128), hTk + ["w_in"], [bcgk])
                    A(lambda e, bcg=bcg: e.activation(ubuf[:, :, 30:158], bcg[:].rearrange("p (c t) -> p c t", c=4),
                                                      AF.Sigmoid), [bcgk], ["ubuf_c"])
                    if i == 0:
                        G(lambda e: e.memset(ubuf[:, :, 0:30], 0.0), w=["ubuf_h"])
                    V(lambda e, bcv=bcv: e.tensor_tensor(ubuf[:, :, 30:158],
                                                         bcv[:].rearrange("p (c t) -> p c t", c=4),
                                                         ubuf[:, :, 30:158], ALU.mult), [bcvk, "ubuf_c"], ["ubuf_c"])
                    bq, bqk = rot.get()
                    T(proj_fm(bq, 1024, 512, 4, 128), hTk + ["w_in"], [bqk])
                    qknorm(bq, bqk, qg, "qg", True)
                    bkk_, bkkk = rot.get()
                    T(proj_fm(bkk_, 1536, 512, 4, 128), hTk + ["w_in"], [bkkk])
                    qknorm(bkk_, bkkk, kg, "kg", False)
                    bv, bvk = rot.get()

                    def projv(e, bv=bv):
                        last = None
                        for k in range(8):
                            last = e.matmul(bv[:, 0:512], hT[:, k, :], w_in[:, k, 2048:2560],
                                            start=(k == 0), stop=(k == 7))
                        return last
                    T(projv, hTk + ["w_in"], [bvk])
                    A(lambda e, bv=bv: e.copy(vsb[:, i, :, 0:64], bv[:].rearrange("p (h d) -> p h d", h=8)),
                      [bvk], [vk])
                    bi, bik = rot.get()

                    bi2, bi2k = rot.get()

                    def proji2(e, bi=bi, bi2=bi2):
                        last = None
                        for j in range(4):
                            for k in range(8):
                                last = e.matmul(bi[0:64, j * 128:(j + 1) * 128],
                                                w_in[:, k, 2560 + j * 64:2560 + (j + 1) * 64],
                                                hT[:, k, :], start=(k == 0), stop=(k == 7))
                        for k in range(8):
                            last = e.matmul(bi2[0:64, 0:128], w_in[:, k, 2816:2880], hT[:, k, :],
                                            start=(k == 0), stop=(k == 7))
                        for k in range(8):
                            last = e.matmul(bi2[:, 128:132], hT[:, k, :], w_in[:, k, 2880:2884],
                                            start=(k == 0), stop=(k == 7))
                        return last
                    T(proji2, hTk + ["w_in"], [bik, bi2k])

                    V(lambda e, bi=bi: e.tensor_copy(qiT[:], bi[0:64, :].rearrange("p (h t) -> p h t", h=4)),
                      [bik], ["qiT"])
                    V(lambda e, bi2=bi2: e.tensor_copy(kiT[:, t0:t0 + 128], bi2[0:64, 0:128]), [bi2k], [kik])
                    V(lambda e, bi2=bi2: e.tensor_copy(wi[:, 0:4], bi2[:, 128:132]), [bi2k], ["wi"])
                    kik_all = ["kiT%d" % j for j in range(NKB)]
                    G(lambda e: e.iota(idx[:, 0:NK], pattern=[[1, NK]], base=0, channel_multiplier=0,
                                       allow_small_or_imprecise_dtypes=True), w=["idx"])
                    A(lambda e: e.mul(idx[:, 0:NK], idx[:, 0:NK], tb_eps), ["idx"], ["idx"])
                    nrl = 0
                    rl2 = sq[:].rearrange("p (o t) -> p o t", o=2)
                    nrl = 0
                    for c0 in range(0, NK, 256):
                        cn = min(256, NK - c0)
                        for h in range(4):
                            bx, bxk = rot.get()
                            T(lambda e, bx=bx, h=h, c0=c0, cn=cn: e.matmul(
                                bx[:, 0:cn], qiT[:, h, :], kiT[:, c0:c0 + cn], start=True, stop=True),
                              ["qiT"] + kik_all, [bxk])
                            sl = nrl % 2
                            A(lambda e, bx=bx, h=h, cn=cn, sl=sl: e.activation(
                                rl2[:, sl, 0:cn], bx[:, 0:cn], AF.Relu),
                              [bxk], ["rl%d" % sl] + (["sq"] if nrl < 2 else []))
                            nrl += 1
                            V(lambda e, h=h, c0=c0, cn=cn, sl=sl: e.scalar_tensor_tensor(
                                idx[:, c0:c0 + cn], rl2[:, sl, 0:cn], wi[:, h:h + 1], idx[:, c0:c0 + cn],
                                ALU.mult, ALU.add), ["rl%d" % sl, "wi", "idx"], ["idx"])
                    mark("index%d" % i)
                    need_search = NK > NSEL
                    if need_search:
                        V(lambda e: e.tensor_reduce(bis[:, 0:1], idx[:, 0:NK], AX.X, ALU.max), ["idx"], ["bis"])
                        V(lambda e: e.tensor_reduce(bis[:, 1:2], idx[:, 0:NK], AX.X, ALU.min), ["idx"], ["bis"])
                        V(lambda e: e.tensor_tensor(bis[:, 2:3], bis[:, 0:1], bis[:, 1:2], ALU.subtract),
                          ["bis"], ["bis"])
                    else:
                        V(lambda e: e.memset(bis[:, 1:2], -1e29), w=["bis"])
                    V(lambda e: e.memset(idx[0:64, t0 + 64:t0 + 128], -1e30), ["idx"], ["idx"])
                    if need_search:
                        V(lambda e: e.tensor_scalar(bws[:, 0:n_iter + 1], pw2[:, 0:n_iter + 1], bis[:, 2:3], None,
                                                    ALU.mult), ["bis", "pw2"], ["bws"])
                        V(lambda e: e.tensor_tensor(bis[:, 3:4], bis[:, 1:2], bws[:, 1:2], ALU.add),
                          ["bis", "bws"], ["mid0"])
                        mcol = (3, 6)
                        for it in range(1, n_iter + 1):
                            mc = mcol[(it - 1) % 2]
                            mn = mcol[it % 2]
                            mck = "mid%d" % ((it - 1) % 2)
                            mnk = "mid%d" % (it % 2)
                            V(lambda e, mc=mc: e.tensor_scalar(mb[:, 0:NK], idx[:, 0:NK], bis[:, mc:mc + 1], None,
                                                               ALU.is_ge, ALU.add, accum_out=bis[:, 4:5]),
                              ["idx", mck], [mbk, "cnt"])
                            V(lambda e, it=it: e.scalar_tensor_tensor(
                                bis[:, 5:6], bis[:, 4:5], NSEL - 0.5, bws[:, it:it + 1],
                                ALU.is_ge, ALU.mult), ["cnt", "bws"], ["tmpb"])
                            if it < n_iter:
                                V(lambda e, it=it, mc=mc, mn=mn: e.scalar_tensor_tensor(
                                    bis[:, mn:mn + 1], bis[:, mc:mc + 1], bws[:, it + 1:it + 2], bis[:, 5:6],
                                    ALU.subtract, ALU.add), [mck, "tmpb", "bws"], [mnk])
                            V(lambda e, mc=mc: e.copy_predicated(bis[:, 1:2], bis[:, 5:6].bitcast(U32),
                                                                 bis[:, mc:mc + 1]),
                              [mck, "tmpb", "bis"], ["bis"])
                    V(lambda e: e.tensor_scalar(mb[:, 0:NK], idx[:, 0:NK], bis[:, 1:2], NEG,
                                                ALU.is_lt, ALU.mult), ["idx", "bis"], [mbk])

                    mark("bisect%d" % i)
                def convblk(b, i):
                    t0 = i * 128
                    r0 = b * S + t0
                    NK = t0 + 128
                    NKB = i + 1
                    ps_ = i % 2
                    vk = "vsb%d" % i
                    kTk = "kT%d" % i
                    kik = "kiT%d" % i
                    xt = xts[:, ps_]
                    xtk = "xt%d" % ps_
                    mb = mbs[:, ps_]
                    mbk = "mb%d" % ps_
                    qpad = qpads[:, ps_]
                    qpk = "qpad%d" % ps_
                    mixT = mixTs[:, ps_]
                    mixk = ["mix%d_%d" % (ps_, k) for k in range(8)]
                    for j in range(31):
                        for c in range(4):
                            if j == 0:
                                V(lambda e, c=c: e.tensor_scalar(yc[:, c, :], ubuf[:, c, 0:128],
                                                                 cw[:, c, 0:1], cb[:, c:c + 1],
                                                                 ALU.mult, ALU.add),
                                  ["ubuf_h", "ubuf_c", "cw", "cb"], ["yc%d" % c])
                            else:
                                V(lambda e, c=c, j=j: e.scalar_tensor_tensor(
                                    yc[:, c, :], ubuf[:, c, j:j + 128], cw[:, c, j:j + 1], yc[:, c, :],
                                    ALU.mult, ALU.add),
                                  ["ubuf_h", "ubuf_c", "cw"], ["yc%d" % c])
                    yck = ["yc%d" % c for c in range(4)]
                    ysqk = ["sq"]
                    if i + 1 < NT:
                        G(lambda e: e.tensor_copy(ubuf[:, :, 0:30], ubuf[:, :, 128:158]),
                          ["ubuf_c"], ["ubuf_h"])
                    G(lambda e: e.tensor_tensor(ysq[:], yc[:], yc[:], ALU.mult), yck, ysqk + ["rl0", "rl1"])
                    bs, bsk = rot.get()

                    def lnstat(e, bs=bs):
                        last = None
                        for c in range(4):
                            last = e.matmul(bs[:, 0:128], ones_f[:], yc[:, c, :], start=(c == 0), stop=(c == 3))
                        for c in range(4):
                            last = e.matmul(bs[:, 128:256], ones_f[:], ysq[:, c, :], start=(c == 0), stop=(c == 3))
                        return last
                    T(lnstat, yck + ysqk + ["ones_f"], [bsk])
                    V(lambda e, bs=bs: e.tensor_scalar(mean[:], bs[:, 0:128], 1.0 / 512, None, ALU.mult),
                      [bsk], ["mean"])
                    V(lambda e: e.tensor_tensor(var[:], mean[:], mean[:], ALU.mult), ["mean"], ["var"])
                    V(lambda e, bs=bs: e.scalar_tensor_tensor(var[:], bs[:, 128:256], 1.0 / 512, var[:],
                                                              ALU.mult, ALU.subtract), [bsk, "var"], ["var"])
                    rsqrt(var[:], var[:], 1.0, ["var"], ["var"])
                    V(lambda e: e.tensor_tensor(t2[:], yc[:], mean[:].unsqueeze(1).to_broadcast([128, 4, 128]),
                                                ALU.subtract), yck + ["mean"], yck)
                    V(lambda e: e.tensor_tensor(t2[:], t2[:], var[:].unsqueeze(1).to_broadcast([128, 4, 128]),
                                                ALU.mult), yck + ["var"], yck)
                    for c in range(4):
                        V(lambda e, c=c: e.tensor_scalar(t2[:, c, :], t2[:, c, :], clg[:, c:c + 1], clb[:, c:c + 1],
                                                         ALU.mult, ALU.add), ["yc%d" % c, "clg", "clb"], ["yc%d" % c])
                    A(lambda e: e.activation(zc[:], t2[:], AF.Silu), yck, yck)
                    zck = yck
                    G(lambda e: e.tensor_tensor(ysq[:], zc[:], zc[:], ALU.mult), zck, ysqk)
                    bs2, bs2k = rot.get()

                    def rmstat(e, bs2=bs2):
                        last = None
                        for c in range(4):
                            last = e.matmul(bs2[:, 0:128], ones_f[:], ysq[:, c, :], start=(c == 0), stop=(c == 3))
                        return last
                    T(rmstat, ysqk + ["ones_f"], [bs2k])
                    rsqrt(r3[:], bs2[:, 0:128], 1.0 / 512, [bs2k, "mean"], ["mean"])
                    for c in range(4):
                        V(lambda e, c=c: e.scalar_tensor_tensor(mixT[:, c, :], zc[:, c, :], goc[:, c:c + 1],
                                                                r3[:], ALU.mult, ALU.mult),
                          ["yc%d" % c, "goc", "mean"], [mixk[c]])


                def attloop(b, i):
                    t0 = i * 128
                    r0 = b * S + t0
                    NK = t0 + 128
                    NKB = i + 1
                    ps_ = i % 2
                    vk = "vsb%d" % i
                    kTk = "kT%d" % i
                    kik = "kiT%d" % i
                    xt = xts[:, ps_]
                    xtk = "xt%d" % ps_
                    mb = mbs[:, ps_]
                    mbk = "mb%d" % ps_
                    qpad = qpads[:, ps_]
                    qpk = "qpad%d" % ps_
                    mixT = mixTs[:, ps_]
                    mixk = ["mix%d_%d" % (ps_, k) for k in range(8)]
                    pvA, pvAk = banks[6], "bk6"
                    pvB, pvBk = banks[7], "bk7"
                    kT_all = ["kT%d" % j for j in range(NKB)]
                    v_all = ["vsb%d" % j for j in range(NKB)]
                    items = [(h, g0) for h in range(8) for g0 in range(0, NKB, 4)]

                    def emit_lg(n):
                        h, g0 = items[n]
                        c = h // 2
                        blks = list(range(g0, min(g0 + 4, NKB)))
                        bl, blk_ = rot.get()

                        def lg(e, bl=bl, blks=blks, h=h, c=c):
                            last = None
                            for n_, kb in enumerate(blks):
                                o = bl[:, n_ * 128:(n_ + 1) * 128]
                                e.matmul(o, kT[:, c, kb * 128:(kb + 1) * 128], qpad[:, h, :],
                                         start=True, stop=False)
                                if kb >= i - 1:
                                    e.matmul(o, ident_b[:], biasTb[:, kb - (i - 1), h, :],
                                             start=False, stop=False)
                                last = e.matmul(o, mb[:, kb * 128:(kb + 1) * 128], ident_b[:],
                                                start=False, stop=True)
                            return last
                        T(lg, kT_all + [qpk, mbk, "ident_b", "biasTb"], [blk_])
                        return bl, blk_, blks

                    nxt = emit_lg(0)
                    for n in range(len(items)):
                        bl, blk_, blks = nxt
                        if n + 1 < len(items):
                            nxt = emit_lg(n + 1)
                        h, g0 = items[n]
                        pv, pvk = (pvA, pvAk) if h < 4 else (pvB, pvBk)
                        hh = h % 4
                        nb_ = len(blks)
                        sl = n % 2
                        A(lambda e, bl=bl, nb_=nb_, sl=sl, h=h: e.activation(
                            Eb[:, sl, 0:nb_ * 128], bl[:, 0:nb_ * 128], AF.Exp,
                            bias=biasfar[:, h:h + 1], scale=1.0), [blk_, "biasfar"], ["Eb%d" % sl])

                        def pvm(e, pv=pv, blks=blks, sl=sl, h=h, hh=hh):
                            last = None
                            for n_, kb in enumerate(blks):
                                last = e.matmul(pv[:, hh * 65:(hh + 1) * 65],
                                                Eb[:, sl, n_ * 128:(n_ + 1) * 128], vsb[:, kb, h, :],
                                                start=(kb == 0), stop=(kb == NKB - 1))
                            return last
                        T(pvm, ["Eb%d" % sl] + v_all, [pvk])
                    return pvA, pvAk, pvB, pvBk

                def backrest(b, i, pvA, pvAk, pvB, pvBk):
                    t0 = i * 128
                    r0 = b * S + t0
                    NK = t0 + 128
                    NKB = i + 1
                    ps_ = i % 2
                    vk = "vsb%d" % i
                    kTk = "kT%d" % i
                    kik = "kiT%d" % i
                    xt = xts[:, ps_]
                    xtk = "xt%d" % ps_
                    mb = mbs[:, ps_]
                    mbk = "mb%d" % ps_
                    qpad = qpads[:, ps_]
                    qpk = "qpad%d" % ps_
                    mixT = mixTs[:, ps_]
                    mixk = ["mix%d_%d" % (ps_, k) for k in range(8)]
                    for hf, (pv, pvk) in enumerate(((pvA, pvAk), (pvB, pvBk))):
                        pv3 = pv[:, 0:260].rearrange("p (h d) -> p h d", d=65)
                        V(lambda e, pv3=pv3, hf=hf: e.reciprocal(sm[:, 8 + hf * 4:12 + hf * 4], pv3[:, :, 64]),
                          [pvk], ["smr%d" % hf])
                        V(lambda e, pv3=pv3, hf=hf: e.tensor_tensor(
                            attn[:, hf * 256:(hf + 1) * 256].rearrange("p (h d) -> p h d", d=64),
                            pv3[:, :, 0:64],
                            sm[:, 8 + hf * 4:12 + hf * 4].unsqueeze(2).to_broadcast([128, 4, 64]),
                            ALU.mult), [pvk, "smr%d" % hf], ["attn%d" % hf])
                    A(lambda e: e.activation(mixT[:, 4:8, :].rearrange("p c t -> p (c t)"), attn[:], AF.Square,
                                             accum_out=sm[:, 2:3]),
                      ["attn0", "attn1"], mixk[4:8] + ["sm2"])
                    rsqrt(sm[:, 3:4], sm[:, 2:3], 1.0 / 512, ["sm2"], ["sm3"])
                    V(lambda e: e.tensor_scalar(Dg[:, 1, :], ident_f[:], sm[:, 3:4], None, ALU.mult),
                      ["ident_f", "sm3"], ["Dg1"])
                    bt, btk = rot.get()

                    def atr(e, bt=bt):
                        last = None
                        for c in range(4):
                            last = e.matmul(bt[:, c * 128:(c + 1) * 128], attn[:, c * 128:(c + 1) * 128],
                                            Dg[:, 1, :], start=True, stop=True)
                        return last
                    T(atr, ["attn0", "attn1", "Dg1"], [btk])
                    for c in range(4):
                        V(lambda e, c=c, bt=bt: e.tensor_scalar(mixT[:, 4 + c, :], bt[:, c * 128:(c + 1) * 128],
                                                                goa[:, c:c + 1], None, ALU.mult),
                          [btk, "goa"], [mixk[4 + c]])

                    for half in range(2):
                        bo, bok = rot.get()

                        def op_(e, bo=bo, half=half):
                            last = None
                            for k in range(8):
                                last = e.matmul(bo[:], mixT[:, k, :], w_out[:, k, half * 512:(half + 1) * 512],
                                                start=(k == 0), stop=(k == 7))
                            return last
                        T(op_, mixk + ["w_out"], [bok])
                        hs = slice(half * 512, (half + 1) * 512)
                        V(lambda e, bo=bo, hs=hs: e.tensor_tensor(xt[:, hs], bo[:], xt[:, hs], ALU.add),
                          [bok, xtk], [xtk])
                    P.dma("sp", out_d[r0:r0 + 128, :], xt, reads=[xtk],
                          writes=["out_g%d" % (r0 // TG)], semkey="st_x1")
                    norm_to_T(xt, 4, 5, a2, sh2, b, h2T, "hT", xtk, 1)
                    P.dma("sp", h2T_s[r0:r0 + 128, :], h2T[:].rearrange("p k t -> p (k t)"), reads=h2Tk,
                          writes=["h2s%d" % (r0 // 128)], semkey="st_h2")


                front(b, 0)
                convblk(b, 0)
                for i in range(NT):
                    if i + 1 < NT:
                        front(b, i + 1)
                    pvs = attloop(b, i)
                    if i + 1 < NT:
                        convblk(b, i + 1)
                    backrest(b, i, *pvs)

        if phase1_only:
            P.final_wait("sp", ["out_g%d" % g for g in range(NG)])
            return nc
        P.barrier()
        with ExitStack() as s2:
            w_pq = sbuf(s2, "w_pq", [128, 8, 2048], BF16)
            with ExitStack() as s2a:
                stg = sbuf(s2a, "stgq", [128, 2, 2048], F32)
                for k in range(8):
                    sl = k % 2
                    P.dma("sp", stg[:, sl, :], wpq_d[k * 128:(k + 1) * 128, :], writes=["stgq%d" % sl])
                    if k % 2 == 0:
                        G(lambda e, k=k, sl=sl: e.tensor_copy(w_pq[:, k, :], stg[:, sl, :]),
                          ["stgq%d" % sl], ["w_pq"])
                    else:
                        A(lambda e, k=k, sl=sl: e.copy(w_pq[:, k, :], stg[:, sl, :]),
                          ["stgq%d" % sl], ["w_pq"])
            P.barrier()
            k1T = sbuf(s2, "k1T", [128, 8, 128], F32)
            k2T = sbuf(s2, "k2T", [128, 8, 128], F32)
            gt2_bc = sbuf(s2, "gt2_bc", [128, NB, D], F32)
            h2 = sbuf(s2, "h2", [128, 2, 8, TG], BF16)
            qqT = sbuf(s2, "qqT", [128, 16, 128], F32)
            sc = sbuf(s2, "sc", [128, 16, 128], F32)
            mx = sbuf(s2, "mx", [128, 16, 16], F32)
            ix = sbuf(s2, "ix", [128, 16, 16], U32)
            ixf = sbuf(s2, "ixf", [128, 16, 16], F32)
            cand2 = sbuf(s2, "cand2", [128, 8, 256], F32)
            best = sbuf(s2, "best", [128, 8, 16], F32)
            pos = sbuf(s2, "pos", [128, 8, 16], U32)
            pab_u = sbuf(s2, "pab_u", [128, 2, 8, 16], U32)
            pa = sbuf(s2, "pa", [128, 8, 16], F32)
            pb = sbuf(s2, "pb", [128, 8, 16], F32)
            IJg = sbuf(s2, "IJg", [128, 3, 128], F32)
            ssum = sbuf(s2, "ssum", [128, 16], F32)
            IJgT = sbuf(s2, "IJgT", [128, 2, 3, TG], BF16)
            OHI = sbuf(s2, "OHI", [128, 2, 8, 128], BF16)
            OHJ = sbuf(s2, "OHJ", [128, 2, 8, 128], BF16)
            OHJg = sbuf(s2, "OHJg", [128, 2, 8, 128], BF16)
            W = sbuf(s2, "W", [128, TG, 128], BF16)
            ub = sbuf(s2, "ub", [128, 2, 8, 512], BF16)
            vbuf = sbuf(s2, "vbuf", [128, 2, 4, D], BF16)
            gel = sbuf(s2, "gel", [128, 2, TG], BF16)
            Z = sbuf(s2, "Z", [128, 3, TG], BF16)
            iota16 = iota_f[:, 0:16]
            sc2 = qqT
            cand = sc[:].rearrange("p m n -> p (m n)").rearrange("p (h q) -> p h q", h=8)
            oh = cand2[:].rearrange("p h (a b) -> p h a b", b=16)
            res = qqT[:].rearrange("p m n -> p (m n)")[:, 0:D]
            xr = cand2[:].rearrange("p h q -> p (h q)")[:, 0:D]

            P.dma("sp", k1T[:], k1T_d[:, :].rearrange("p (h n) -> p h n", h=8), writes=["k1T"])
            P.dma("sp", k2T[:], k2T_d[:, :].rearrange("p (h n) -> p h n", h=8), writes=["k2T"])
            rotw = Rot([4, 5, 6, 7])
            for b in range(NB):
                class _D:
                    pass
                gate_bcast(gt2_bc[:, b, :], gt2, b, "gt2", "gt2_bc", rotw)

            def gating_gen(g):
                gb = g % 2
                r0 = g * TG
                for sub in range(2):
                    rr = r0 + sub * 128
                    P.dma("sp", h2[:, gb, :, sub * 128:(sub + 1) * 128],
                          h2T_s[rr:rr + 128, :].rearrange("p (k t) -> p k t", k=8),
                          reads=["h2s%d" % (rr // 128)], writes=["h2_%d_%d" % (gb, sub)])
                    yield
                for sub in range(2):
                    ts_ = slice(sub * 128, (sub + 1) * 128)
                    for m4 in range(4):
                        bk, bkk = rotw.get()

                        def qm(e, bk=bk, m4=m4, ts_=ts_):
                            last = None
                            for mm_ in range(4):
                                m = m4 * 4 + mm_
                                for k in range(8):
                                    last = e.matmul(bk[:, mm_ * 128:(mm_ + 1) * 128],
                                                    w_pq[:, k, m * 128:(m + 1) * 128], h2[:, gb, k, ts_],
                                                    start=(k == 0), stop=(k == 7))
                            return last
                        T(qm, ["w_pq", "h2_%d_%d" % (gb, sub)], [bkk])
                        yield
                        A(lambda e, bk=bk, m4=m4: e.copy(
                            qqT[:, m4 * 4:(m4 + 1) * 4, :].rearrange("p m t -> p (m t)"), bk[:]),
                          [bkk], ["A"])
                        yield
                    for m4 in range(4):
                        bk, bkk = rotw.get()

                        def sm_(e, bk=bk, m4=m4):
                            last = None
                            for mm_ in range(4):
                                m = m4 * 4 + mm_
                                kt = k1T if m % 2 == 0 else k2T
                                last = e.matmul(bk[:, mm_ * 128:(mm_ + 1) * 128], qqT[:, m, :],
                                                kt[:, m // 2, :], start=True, stop=True)
                            return last
                        T(sm_, ["A", "k1T", "k2T"], [bkk])
                        yield
                        A(lambda e, bk=bk, m4=m4: e.copy(
                            sc[:, m4 * 4:(m4 + 1) * 4, :].rearrange("p m t -> p (m t)"), bk[:]),
                          [bkk], ["B"])
                        yield
                    sck = ["B"]
                    for m in range(16):
                        V(lambda e, m=m: e.max(mx[:, m, 0:8], sc[:, m, :]), sck, ["mx%d" % m])
                        yield
                    for m in range(16):
                        V(lambda e, m=m: e.max_index(ix[:, m, 0:8], mx[:, m, 0:8], sc[:, m, :]),
                          sck + ["mx%d" % m], ["ix%d" % m])
                        yield
                    for m in range(16):
                        V(lambda e, m=m: e.match_replace(sc2[:, m, :], mx[:, m, 0:8], sc[:, m, :], -1e30),
                          sck + ["mx%d" % m], ["A"])
                        yield
                    for m in range(16):
                        V(lambda e, m=m: e.max(mx[:, m, 8:16], sc2[:, m, :]), ["A"], ["mx%d" % m])
                        yield
                    for m in range(16):
                        V(lambda e, m=m: e.max_index(ix[:, m, 8:16], mx[:, m, 8:16], sc2[:, m, :]),
                          ["A", "mx%d" % m], ["ix%d" % m])
                        yield
                    mxk = ["mx%d" % m for m in range(16)]
                    ixk = ["ix%d" % m for m in range(16)]
                    V(lambda e: e.tensor_copy(ixf[:], ix[:]), ixk, ["ixf"])
                    yield
                    mxv = mx[:].rearrange("p (h two) a -> p h two a", two=2)
                    ixv = ixf[:].rearrange("p (h two) a -> p h two a", two=2)
                    V(lambda e: e.tensor_tensor(
                        cand.rearrange("p h (a b) -> p h a b", b=16),
                        mxv[:, :, 0, :].unsqueeze(3).to_broadcast([128, 8, 16, 16]),
                        mxv[:, :, 1, :].unsqueeze(2).to_broadcast([128, 8, 16, 16]), ALU.add),
                      mxk + ixk, ["B"])
                    yield
                    for h in range(8):
                        V(lambda e, h=h: e.max(best[:, h, 0:8], cand[:, h, :]), ["B"], ["best%d" % h])
                        yield
                    for h in range(8):
                        V(lambda e, h=h: e.max_index(pos[:, h, 0:8], best[:, h, 0:8], cand[:, h, :]),
                          ["B", "best%d" % h], ["pos%d" % h])
                        yield
                    for h in range(8):
                        V(lambda e, h=h: e.match_replace(cand2[:, h, :], best[:, h, 0:8], cand[:, h, :], -1e30),
                          ["B", "best%d" % h], ["C"])
                        yield
                    for h in range(8):
                        V(lambda e, h=h: e.max(best[:, h, 8:16], cand2[:, h, :]), ["C"], ["best%d" % h])
                        yield
                    for h in range(8):
                        V(lambda e, h=h: e.max_index(pos[:, h, 8:16], best[:, h, 8:16], cand2[:, h, :]),
                          ["C", "best%d" % h], ["pos%d" % h])
                        yield
                    bestk = ["best%d" % h for h in range(8)]
                    posk = ["pos%d" % h for h in range(8)]
                    gsl = IJg[:, 2, :].rearrange("p (h k) -> p h k", k=16)
                    V(lambda e: e.tensor_tensor(gsl, best[:], best[:, :, 0:1].to_broadcast([128, 8, 16]),
                                                ALU.subtract), bestk, ["gate"])
                    yield
                    A(lambda e: e.activation(gsl, gsl, AF.Exp), ["gate"], ["gate"])
                    yield
                    V(lambda e: e.tensor_reduce(ssum[:, 0:8], gsl, AX.X, ALU.add), ["gate"], ["ssum"])
                    yield
                    V(lambda e: e.reciprocal(ssum[:, 8:16], ssum[:, 0:8]), ["ssum"], ["ssum"])
                    yield
                    V(lambda e: e.tensor_tensor(gsl, gsl, ssum[:, 8:16].unsqueeze(2).to_broadcast([128, 8, 16]),
                                                ALU.mult), ["gate", "ssum"], ["gate"])
                    yield
                    V(lambda e: e.tensor_single_scalar(pab_u[:, 0], pos[:], 4, ALU.logical_shift_right),
                      posk, ["pab_u"])
                    yield
                    V(lambda e: e.tensor_single_scalar(pab_u[:, 1], pos[:], 15, ALU.bitwise_and),
                      posk + ["pab_u"], ["pab_u"])
                    yield
                    V(lambda e: e.tensor_copy(pa[:], pab_u[:, 0]), ["pab_u"], ["pa"])
                    yield
                    V(lambda e: e.tensor_copy(pb[:], pab_u[:, 1]), ["pab_u"], ["pb"])
                    yield
                    i16 = iota16.unsqueeze(1).unsqueeze(1).to_broadcast([128, 8, 16, 16])
                    for which, (pp, ppk) in enumerate(((pa, "pa"), (pb, "pb"))):
                        V(lambda e, pp=pp: e.tensor_tensor(oh, pp[:].unsqueeze(3).to_broadcast([128, 8, 16, 16]),
                                                           i16, ALU.is_equal), [ppk, "iota"], ["C"])
                        yield
                        V(lambda e, which=which: e.tensor_tensor(
                            oh, oh, ixv[:, :, which, :].unsqueeze(2).to_broadcast([128, 8, 16, 16]),
                            ALU.mult), ["C", "ixf"], ["C"])
                        yield
                        V(lambda e, which=which: e.tensor_reduce(
                            IJg[:, which, :].rearrange("p (h k) -> p h k", k=16), oh, AX.X, ALU.add),
                          ["C"], ["IJ%d" % which])
                        yield
                    bk, bkk = rotw.get()

                    def ijt(e, bk=bk):
                        last = None
                        for w_ in range(3):
                            last = e.transpose(bk[:, w_ * 128:(w_ + 1) * 128], IJg[:, w_, :], ident_f[:])
                        return last
                    T(ijt, ["IJ0", "IJ1", "gate", "ident_f"], [bkk])
                    yield
                    A(lambda e, bk=bk, ts_=ts_: e.copy(IJgT[:, gb, :, ts_], bk[:, 0:384].rearrange("p (w t) -> p w t", w=3)),
                      [bkk], ["IJgT%d_%d" % (gb, sub)])
                    yield

            def wbuild(g):
                gb = g % 2
                for sub in range(2):
                    for q in range(16):
                        tq = sub * 128 + q * 8
                        sl = q % 2
                        ik = "IJgT%d_%d" % (gb, sub)
                        iob = iota_b[:].unsqueeze(1).to_broadcast([128, 8, 128])
                        V(lambda e, sl=sl, tq=tq: e.tensor_tensor(
                            OHI[:, sl], iob, IJgT[:, gb, 0, tq:tq + 8].unsqueeze(2).to_broadcast([128, 8, 128]),
                            ALU.is_equal), [ik, "iota_b"], ["OHI%d" % sl])
                        V(lambda e, sl=sl, tq=tq: e.tensor_tensor(
                            OHJ[:, sl], iob, IJgT[:, gb, 1, tq:tq + 8].unsqueeze(2).to_broadcast([128, 8, 128]),
                            ALU.is_equal), [ik, "iota_b"], ["OHJ%d" % sl])
                        V(lambda e, sl=sl, tq=tq: e.tensor_tensor(
                            OHJg[:, sl], OHJ[:, sl],
                            IJgT[:, gb, 2, tq:tq + 8].unsqueeze(2).to_broadcast([128, 8, 128]),
                            ALU.mult), [ik, "OHJ%d" % sl], ["OHJg%d" % sl])
                        for q4 in range(2):
                            bk, bkk = rotw.get()

                            def wm(e, bk=bk, sl=sl, q4=q4):
                                last = None
                                for tt in range(4):
                                    t_ = q4 * 4 + tt
                                    last = e.matmul(bk[:, tt * 128:(tt + 1) * 128], OHJg[:, sl, t_, :],
                                                    OHI[:, sl, t_, :], start=True, stop=True)
                                return last
                            T(wm, ["OHJg%d" % sl, "OHI%d" % sl], [bkk])
                            A(lambda e, bk=bk, tq=tq, q4=q4: e.copy(
                                W[:, tq + q4 * 4:tq + q4 * 4 + 4, :].rearrange("p t i -> p (t i)"), bk[:]),
                              [bkk], ["W"])

            gq = gating_gen(0)
            for _ in gq:
                pass
            for g in range(NG):
                r0 = g * TG
                b = r0 // S
                gb = g % 2
                h2k = ["h2_%d_0" % gb, "h2_%d_1" % gb]
                wbuild(g)
                gq = gating_gen(g + 1) if g + 1 < NG else iter(())
                LA = 2
                GI = 5
                pend = []
                for ci in range(NEXP_CH + LA):
                    if ci < NEXP_CH:
                        cg, cc = divmod(ci, 4)
                        sl = cg % 2
                        if cc == 0:
                            e0 = cg * 512
                            P.dma("sp", ub[:, sl], uTb_s[:, e0:e0 + 512].rearrange("(k p) e -> p k e", p=128),
                                  reads=["uTb_s"], writes=["ub%d" % sl])
                            P.dma("sp", vbuf[:, sl], vb_s[e0:e0 + 512, :].rearrange("(c j) d -> j c d", j=128),
                                  reads=["vb_s"], writes=["vbuf%d" % sl])
                        by, byk = rotw.get()

                        def ym(e, by=by, sl=sl, cc=cc, gb=gb):
                            last = None
                            for k in range(8):
                                last = e.matmul(by[:, 0:TG], ub[:, sl, k, cc * 128:(cc + 1) * 128], h2[:, gb, k, :],
                                                start=(k == 0), stop=(k == 7))
                            return last
                        T(ym, ["ub%d" % sl] + h2k, [byk])
                        gs = ci % 2
                        zs = ci % 3
                        A(lambda e, by=by, gs=gs: e.activation(gel[:, gs, :], by[:, 0:TG], AF.Gelu_apprx_tanh),
                          [byk], ["gel%d" % gs])
                        V(lambda e, zs=zs, gs=gs, ci=ci: e.tensor_tensor(Z[:, zs, :], gel[:, gs, :], W[:, :, ci],
                                                                         ALU.mult),
                          ["gel%d" % gs, "W"], ["Z%d" % zs])

                        def vm(e, zs=zs, sl=sl, cc=cc, ci=ci):
                            last = None
                            for sub in range(2):
                                for half in range(2):
                                    last = e.matmul(banks[sub * 2 + half][:],
                                                    Z[:, zs, sub * 128:(sub + 1) * 128],
                                                    vbuf[:, sl, cc, half * 512:(half + 1) * 512],
                                                    start=(ci == 0), stop=(ci == NEXP_CH - 1))
                            return last
                        pend.append((vm, ["Z%d" % zs, "vbuf%d" % sl]))
                    if ci >= LA:
                        vm_, rk_ = pend.pop(0)
                        T(vm_, rk_, ["bk0", "bk1", "bk2", "bk3"])
                    for _ in range(GI):
                        next(gq, None)
                for _ in gq:
                    pass
                for sub in range(2):
                    rr = r0 + sub * 128
                    P.dma("sp", xr, out_d[rr:rr + 128, :], reads=["out_g%d" % g], writes=["C"])
                    for half in range(2):
                        hs = slice(half * 512, (half + 1) * 512)
                        V(lambda e, sub=sub, half=half, hs=hs: e.tensor_tensor(
                            res[:, hs], banks[sub * 2 + half][:], gt2_bc[:, b, hs], ALU.mult),
                          ["bk%d" % (sub * 2 + half), "gt2_bc", "A"], ["A"])
                    G(lambda e: e.tensor_tensor(res, res, xr, ALU.add), ["A", "C"], ["A"])
                    P.dma("sp", out_d[rr:rr + 128, :], res, reads=["A"], writes=["out_f"], semkey="st_res")
            P.final_wait("sp", ["out_f"])
        P.emit()
        print("instructions:", P.n_instr, {e: len(P.ops[e]) for e in ENGS}, "dma sems:", len(P.dsem))
    return nc


def _t5_bucket_table():
    import jax
    import jax.numpy as jnp
    with jax.default_device(jax.devices("cpu")[0]):
        return _t5_bucket_table_impl(jnp)


def _t5_bucket_table_impl(jnp):
    rel = jnp.arange(-255, 128, dtype=jnp.int32)
    half = 16
    max_exact = 8
    ret = jnp.where(rel > 0, half, 0)
    n = jnp.abs(rel)
    nf = jnp.maximum(n, 1).astype(jnp.float32)
    large = max_exact + (jnp.log(nf / max_exact) / math.log(128 / max_exact)
                         * (half - max_exact)).astype(jnp.int32)
    large = jnp.minimum(large, half - 1)
    return np.asarray(ret + jnp.where(n < max_exact, n, large))


def fm(vec, nchunk):
    return np.ascontiguousarray(np.asarray(vec, np.float32).reshape(nchunk, 128).T)


def prep_shared(inp):
    f = lambda a: np.ascontiguousarray(np.asarray(a, np.float32))
    bt = _t5_bucket_table()
    rb = f(inp["rel_bias"])
    ii = np.arange(128)
    biasT = np.empty((128, 2, 8, 128), np.float32)
    for blk in range(2):
        rel = (blk - 1) * 128 + ii[:, None] - ii[None, :]
        biasT[:, blk] = np.transpose(rb[bt[rel + 255]], (0, 2, 1))
    far = rb[15]
    sh = {
        "w_ada": f(inp["w_ada"][0]),
        "b_ada": fm(inp["b_ada"][0], 48),
        "g1": fm(inp["g_norm1"][0], 8),
        "g2": fm(inp["g_norm2"][0], 8),
        "w_in": f(inp["w_in"][0]),
        "qg": np.ascontiguousarray(np.tile(f(inp["q_norm_g"][0]), 2)[:, None]),
        "kg": np.ascontiguousarray(np.tile(f(inp["k_norm_g"][0]), 2)[:, None]),
        "cw": np.ascontiguousarray(np.transpose(f(inp["conv_w"][0])[:, 0, :].reshape(31, 4, 128), (2, 1, 0)).reshape(128, 124)),
        "cb": fm(inp["conv_b"][0], 4),
        "clg": fm(inp["conv_ln_g"][0], 4),
        "clb": fm(inp["conv_ln_b"][0], 4),
        "goc": fm(inp["g_out_conv"][0], 4),
        "goa": fm(inp["g_out_attn"][0], 4),
        "biasT": np.ascontiguousarray(biasT.reshape(128, -1)),
        "biasfar": np.ascontiguousarray(np.broadcast_to(far[None, :], (128, 8))),
        "w_out": f(inp["w_out"][0]),
        "w_pq": f(inp["w_peer_q"][0]),
        "k1T": np.ascontiguousarray(np.transpose(f(inp["peer_k1"][0]), (2, 0, 1)).reshape(128, 1024)),
        "k2T": np.ascontiguousarray(np.transpose(f(inp["peer_k2"][0]), (2, 0, 1)).reshape(128, 1024)),
        "uT": np.ascontiguousarray(f(inp["peer_u"][0]).T),
        "vtab": f(inp["peer_v"][0]),
    }
    return sh


def prep_core(x, c, NB):
    S = x.shape[1]
    cT = np.ascontiguousarray(np.transpose(np.asarray(c, np.float32).reshape(NB, 8, 128), (2, 1, 0)).reshape(128, 8 * NB))
    return {"x": np.ascontiguousarray(np.asarray(x, np.float32).reshape(NB * S, D)), "cT": cT}


N_CORES = 8


def kernel(**inputs):
    x = np.asarray(inputs["x"])
    c = np.asarray(inputs["c"])
    B, S, _ = x.shape
    NB = B // N_CORES
    NSEL = min(256, S // 4)
    nc = build(S, NB, NSEL)
    sh = prep_shared(inputs)
    in_maps = []
    for i in range(N_CORES):
        m = dict(sh)
        m.update(prep_core(x[i * NB:(i + 1) * NB], c[i * NB:(i + 1) * NB], NB))
        in_maps.append(m)
    res = run_bass_kernel_spmd(nc, in_maps, core_ids=list(range(N_CORES)))
    out = np.concatenate([np.asarray(r["out"]).reshape(NB, S, D) for r in res.results], axis=0)
    return out.astype(np.float32)
```
